# Optimizing a Trainium2 kernel written in Bass

```python
import math
import jax, jax.numpy as jnp
from jax import lax
import numpy as np

D_MODEL = 2048
BATCH = 8
SEQ = 2048
DEPTH = 1

SSM_WIDTH = D_MODEL // 2
SSM_GROUP = 16
SSM_GROUPS = SSM_WIDTH // SSM_GROUP
SSM_STATE = 64
RET_WIDTH = D_MODEL - SSM_WIDTH
RET_HEADS = 8
RET_HEAD_DIM = RET_WIDTH // RET_HEADS
RET_CHUNK = 128
ROPE_BASE = 10000.0
N_GROUPS = 4
EXPERTS_PER_GROUP = 8
N_EXPERTS = N_GROUPS * EXPERTS_PER_GROUP
TOP_K = 2
D_EXPERT = D_MODEL // 2
MOE_BLOCK = 256
N_MOD = 6
NORM_EPS = 1e-6
GN_EPS = 1e-5
DT_MIN = 1e-3
DT_MAX = 1e-1

kernel_name = "hymba_s5_retnet_hiermoe_adaln"


def rms_norm(x, gain):
    xf = x.astype(jnp.float32)
    y = xf * lax.rsqrt(jnp.mean(xf * xf, axis=-1, keepdims=True) + NORM_EPS)
    return (y * gain.astype(jnp.float32)).astype(x.dtype)


def cmul(ar, ai, br, bi):
    return ar * br - ai * bi, ar * bi + ai * br


def s5_mixer(u, a_re, a_im, b_re, b_im, c_re, c_im, d_skip, log_dt, w_glu, beta):
    bn, L, _ = u.shape
    f32 = jnp.float32
    uf = u.astype(f32).reshape(bn, L, SSM_GROUPS, SSM_GROUP)
    a_re = a_re.astype(f32)
    a_im = a_im.astype(f32)
    dt = jnp.exp(log_dt.astype(f32))[:, None]
    mag = jnp.exp(dt * a_re)
    ab_re = mag * jnp.cos(dt * a_im)
    ab_im = mag * jnp.sin(dt * a_im)
    inv_abs2 = 1.0 / (a_re * a_re + a_im * a_im)
    n_re = ab_re - 1.0
    f_re = (n_re * a_re + ab_im * a_im) * inv_abs2
    f_im = (ab_im * a_re - n_re * a_im) * inv_abs2
    bb_re, bb_im = cmul(f_re[..., None], f_im[..., None],
                        b_re.astype(f32), b_im.astype(f32))
    bu_re = jnp.einsum('blgh,gph->blgp', uf, bb_re)
    bu_im = jnp.einsum('blgh,gph->blgp', uf, bb_im)
    a_seq_re = jnp.broadcast_to(ab_re, (1, L, SSM_GROUPS, SSM_STATE))
    a_seq_im = jnp.broadcast_to(ab_im, (1, L, SSM_GROUPS, SSM_STATE))

    def combine(e1, e2):
        ar1, ai1, br1, bi1 = e1
        ar2, ai2, br2, bi2 = e2
        ar, ai = cmul(ar2, ai2, ar1, ai1)
        xr, xi = cmul(ar2, ai2, br1, bi1)
        return ar, ai, xr + br2, xi + bi2

    _, _, s_re, s_im = lax.associative_scan(
        combine, (a_seq_re, a_seq_im, bu_re, bu_im), axis=1)
    y = (jnp.einsum('blgp,ghp->blgh', s_re, c_re.astype(f32))
         - jnp.einsum('blgp,ghp->blgh', s_im, c_im.astype(f32))
         + d_skip.astype(f32) * uf)
    y = y.reshape(bn, L, SSM_WIDTH)
    z = jax.nn.gelu(y)
    out = z * jax.nn.sigmoid(z @ w_glu.astype(f32))
    return rms_norm(out, beta)


def rope(x, L):
    half = RET_HEAD_DIM // 2
    inv_freq = 1.0 / (ROPE_BASE ** (jnp.arange(half, dtype=jnp.float32) * 2.0 / RET_HEAD_DIM))
    ang = jnp.arange(L, dtype=jnp.float32)[:, None] * inv_freq[None, :]
    cos = jnp.cos(ang)[None, :, None, :]
    sin = jnp.sin(ang)[None, :, None, :]
    x1, x2 = x[..., :half], x[..., half:]
    return jnp.concatenate([x1 * cos - x2 * sin, x2 * cos + x1 * sin], axis=-1)


def retention(q, k, v, g, beta):
    bn, L, _ = q.shape
    f32 = jnp.float32
    H, Dh, C = RET_HEADS, RET_HEAD_DIM, RET_CHUNK
    nc = L // C
    q = rope(q.astype(f32).reshape(bn, L, H, Dh), L)
    k = rope(k.astype(f32).reshape(bn, L, H, Dh), L) * (Dh ** -0.5)
    v = v.astype(f32).reshape(bn, L, H, Dh)
    gamma = 1.0 - jnp.exp2(-5.0 - jnp.arange(H, dtype=f32))
    log_g = jnp.log(gamma)
    idx = jnp.arange(C, dtype=f32)
    rel = idx[:, None] - idx[None, :]
    decay = jnp.where(rel >= 0, jnp.exp(log_g[:, None, None] * jnp.maximum(rel, 0.0)), 0.0)
    qc = q.reshape(bn, nc, C, H, Dh)
    kc = k.reshape(bn, nc, C, H, Dh)
    vc = v.reshape(bn, nc, C, H, Dh)
    s = jnp.einsum('bnchd,bnmhd->bnhcm', qc, kc) * decay[None, None]
    inner = jnp.einsum('bnhcm,bnmhe->bnche', s, vc)
    zeta = jnp.exp(log_g[:, None] * (C - 1.0 - idx)[None, :])
    kv = jnp.einsum('bnmhd,hm,bnmhe->bnhde', kc, zeta, vc)
    g_chunk = jnp.exp(log_g * C)[None, :, None, None]

    def step(r, kv_n):
        return r * g_chunk + kv_n, r

    _, r_prev = lax.scan(step, jnp.zeros((bn, H, Dh, Dh), f32), jnp.moveaxis(kv, 1, 0))
    r_prev = jnp.moveaxis(r_prev, 0, 1)
    xi = jnp.exp(log_g[None, :] * (idx + 1.0)[:, None])
    cross = jnp.einsum('bnchd,bnhde->bnche', qc, r_prev) * xi[None, None, :, :, None]
    y = (inner + cross).reshape(bn, L, H, Dh)
    mu = jnp.mean(y, axis=-1, keepdims=True)
    var = jnp.mean(jnp.square(y - mu), axis=-1, keepdims=True)
    y = ((y - mu) * lax.rsqrt(var + GN_EPS)).reshape(bn, L, RET_WIDTH) * beta.astype(f32)
    return jax.nn.silu(g.astype(f32)) * y


def hier_moe(h, w_rg, b_rg, w_re, b_re, w_gate, w_up, w_down):
    bn, L, D = h.shape
    T = bn * L
    f32 = jnp.float32
    hf = h.reshape(T, D)
    gl = (hf @ w_rg + b_rg).astype(f32)
    gp = jax.nn.softmax(gl, axis=-1)
    g_sel = jnp.argmax(gl, axis=-1)
    g_w = jnp.take_along_axis(gp, g_sel[:, None], axis=1)[:, 0]
    el = (hf @ w_re + b_re).astype(f32).reshape(T, N_GROUPS, EXPERTS_PER_GROUP)
    el_sel = jnp.take_along_axis(el, g_sel[:, None, None], axis=1)[:, 0]
    top_l, top_j = lax.top_k(el_sel, TOP_K)
    top_w = jax.nn.softmax(top_l, axis=-1) * g_w[:, None]
    eid = g_sel[:, None] * EXPERTS_PER_GROUP + top_j
    A = T * TOP_K
    e_flat = eid.reshape(A).astype(jnp.int32)
    w_flat = top_w.reshape(A)
    tok = jnp.arange(A, dtype=jnp.int32) // TOP_K
    order = jnp.argsort(e_flat)
    s_e = e_flat[order]
    s_tok = tok[order]
    s_w = w_flat[order]
    counts = jnp.bincount(e_flat, length=N_EXPERTS)
    padded = ((counts + MOE_BLOCK - 1) // MOE_BLOCK) * MOE_BLOCK
    off = jnp.cumsum(counts) - counts
    pend = jnp.cumsum(padded)
    poff = pend - padded
    dest = poff[s_e] + jnp.arange(A, dtype=jnp.int32) - off[s_e]
    nb = -(-A // MOE_BLOCK) + N_EXPERTS
    P = nb * MOE_BLOCK
    slot_tok = jnp.full((P,), T, jnp.int32).at[dest].set(s_tok)
    slot_w = jnp.zeros((P,), f32).at[dest].set(s_w)
    block_e = jnp.minimum(
        jnp.searchsorted(pend, jnp.arange(nb, dtype=jnp.int32) * MOE_BLOCK, side='right'),
        N_EXPERTS - 1)
    x_pad = jnp.concatenate([hf, jnp.zeros((1, D), hf.dtype)], axis=0)
    xs = x_pad[slot_tok].reshape(nb, MOE_BLOCK, D)

    def expert_block(args):
        xb, e = args
        return (jax.nn.silu(xb @ w_gate[e]) * (xb @ w_up[e])) @ w_down[e]

    ys = lax.map(expert_block, (xs, block_e)).reshape(P, D)
    ys = ys * slot_w[:, None].astype(ys.dtype)
    out = jax.ops.segment_sum(ys, slot_tok, num_segments=T + 1)[:T]
    return out.reshape(bn, L, D)


def setup_inputs(seed: int = 0) -> dict:
    key = jax.random.key(seed)
    ks = jax.random.split(key, 32)
    f32 = jnp.float32
    D, G, P, Hc = D_MODEL, SSM_GROUPS, SSM_STATE, SSM_GROUP
    nrm = lambda k, shape, s: jax.random.normal(k, shape, f32) * s
    gain = lambda k, shape: 1.0 + 0.02 * jax.random.normal(k, shape, f32)
    return {
        "x": jax.random.normal(ks[0], (BATCH, SEQ, D), f32),
        "c": jax.random.normal(ks[1], (BATCH, D), f32),
        "w_ada": nrm(ks[2], (DEPTH, D, N_MOD * D), D ** -0.5),
        "b_ada": nrm(ks[3], (DEPTH, N_MOD * D), 0.02),
        "g_norm1": gain(ks[4], (DEPTH, D)),
        "w_in": nrm(ks[5], (DEPTH, D, SSM_WIDTH + 4 * RET_WIDTH), D ** -0.5),
        "ssm_a_re": -0.5 + 0.01 * jax.random.normal(ks[6], (DEPTH, G, P), f32),
        "ssm_a_im": math.pi * jnp.arange(P, dtype=f32)[None, None, :]
                    + 0.01 * jax.random.normal(ks[7], (DEPTH, G, P), f32),
        "ssm_b_re": nrm(ks[8], (DEPTH, G, P, Hc), (2 * Hc) ** -0.5),
        "ssm_b_im": nrm(ks[9], (DEPTH, G, P, Hc), (2 * Hc) ** -0.5),
        "ssm_c_re": nrm(ks[10], (DEPTH, G, Hc, P), (2 * P) ** -0.5),
        "ssm_c_im": nrm(ks[11], (DEPTH, G, Hc, P), (2 * P) ** -0.5),
        "ssm_d": nrm(ks[12], (DEPTH, G, Hc), 1.0),
        "ssm_log_dt": jax.random.uniform(ks[13], (DEPTH, G), f32,
                                         math.log(DT_MIN), math.log(DT_MAX)),
        "w_glu": nrm(ks[14], (DEPTH, SSM_WIDTH, SSM_WIDTH), SSM_WIDTH ** -0.5),
        "beta_ssm": gain(ks[15], (DEPTH, SSM_WIDTH)),
        "beta_ret": gain(ks[16], (DEPTH, RET_WIDTH)),
        "w_out": nrm(ks[17], (DEPTH, D, D), D ** -0.5),
        "g_norm2": gain(ks[18], (DEPTH, D)),
        "w_router_group": nrm(ks[19], (DEPTH, D, N_GROUPS), D ** -0.5),
        "b_router_group": nrm(ks[20], (DEPTH, N_GROUPS), 0.01),
        "w_router_expert": nrm(ks[21], (DEPTH, D, N_EXPERTS), D ** -0.5),
        "b_router_expert": nrm(ks[22], (DEPTH, N_EXPERTS), 0.01),
        "w_gate": nrm(ks[23], (DEPTH, N_EXPERTS, D, D_EXPERT), D ** -0.5),
        "w_up": nrm(ks[24], (DEPTH, N_EXPERTS, D, D_EXPERT), D ** -0.5),
        "w_down": nrm(ks[25], (DEPTH, N_EXPERTS, D_EXPERT, D), D_EXPERT ** -0.5),
        "g_final": gain(ks[26], (D,)),
    }


def reference(x, c, w_ada, b_ada, g_norm1, w_in, ssm_a_re, ssm_a_im, ssm_b_re, ssm_b_im,
              ssm_c_re, ssm_c_im, ssm_d, ssm_log_dt, w_glu, beta_ssm, beta_ret, w_out,
              g_norm2, w_router_group, b_router_group, w_router_expert, b_router_expert,
              w_gate, w_up, w_down, g_final):
    splits = [SSM_WIDTH, SSM_WIDTH + RET_WIDTH, SSM_WIDTH + 2 * RET_WIDTH,
              SSM_WIDTH + 3 * RET_WIDTH]
    for l in range(DEPTH):
        mod = jax.nn.silu(c) @ w_ada[l] + b_ada[l]
        sh1, sc1, gt1, sh2, sc2, gt2 = jnp.split(mod, N_MOD, axis=-1)
        h = rms_norm(x, g_norm1[l]) * (1.0 + sc1[:, None, :]) + sh1[:, None, :]
        proj = h @ w_in[l]
        u, q, k, v, g = jnp.split(proj, splits, axis=-1)
        y_ssm = s5_mixer(u, ssm_a_re[l], ssm_a_im[l], ssm_b_re[l], ssm_b_im[l],
                         ssm_c_re[l], ssm_c_im[l], ssm_d[l], ssm_log_dt[l],
                         w_glu[l], beta_ssm[l])
        y_ret = retention(q, k, v, g, beta_ret[l])
        mixed = jnp.concatenate([y_ssm, y_ret], axis=-1).astype(x.dtype) @ w_out[l]
        x = x + gt1[:, None, :] * mixed
        h2 = rms_norm(x, g_norm2[l]) * (1.0 + sc2[:, None, :]) + sh2[:, None, :]
        y_moe = hier_moe(h2, w_router_group[l], b_router_group[l], w_router_expert[l],
                         b_router_expert[l], w_gate[l], w_up[l], w_down[l])
        x = x + gt2[:, None, :] * y_moe.astype(x.dtype)
    return rms_norm(x, g_final)
```

```python
import math
import os
from contextlib import ExitStack

import numpy as np
import concourse.bass as bass
import concourse.mybir as mybir
from concourse.bass_utils import run_bass_kernel_spmd

F32 = mybir.dt.float32
BF16 = mybir.dt.bfloat16
I32 = mybir.dt.int32
ALU = mybir.AluOpType
AF = mybir.ActivationFunctionType
AX = mybir.AxisListType

D = 2048
L = 2048
NCORES = 8
COMPUTE = ("pe", "act", "dve", "pool")
NDMA_SEMS = 28
SB_BASE = 16640
SB_LIMIT = 229376


class Prog:
    def __init__(self, nc):
        self.nc = nc
        self.ops = []

    def op(self, eng, fn, reads=(), writes=()):
        self.ops.append(dict(kind="c", eng=eng, fn=fn, reads=tuple(reads), writes=tuple(writes), bar=False))
        return len(self.ops) - 1

    def dma(self, q, fn, reads=(), writes=()):
        self.ops.append(dict(kind="d", eng=q, fn=fn, reads=tuple(reads), writes=tuple(writes), bar=False))
        return len(self.ops) - 1

    def barrier(self, fn):
        self.ops.append(dict(kind="c", eng="dve", fn=fn, reads=(), writes=(), bar=True))
        return len(self.ops) - 1

    def _analyze(self, final):
        ops = self.ops
        last_w, readers = {}, {}
        last_on = {}
        dmas_since = []
        pending_bar = {}
        for i, o in enumerate(ops):
            deps = set()
            raw = set()
            if o["bar"]:
                for e, j in last_on.items():
                    deps.add(j)
                deps.update(dmas_since)
                dmas_since = []
                for e in ("pe", "act", "dve", "pool", "sp"):
                    pending_bar[e] = i
                pending_bar.pop("dve", None)
                last_w, readers = {}, {}
            else:
                for b in o["reads"]:
                    if b in last_w:
                        deps.add(last_w[b])
                        raw.add(last_w[b])
                for b in o["writes"]:
                    if b in last_w:
                        deps.add(last_w[b])
                    deps.update(readers.get(b, ()))
                if o["eng"] in pending_bar:
                    deps.add(pending_bar.pop(o["eng"]))
            deps.discard(i)
            raw.discard(i)
            o["deps"] = deps
            o["raw"] = raw
            for b in o["reads"]:
                readers.setdefault(b, []).append(i)
            for b in o["writes"]:
                last_w[b] = i
                readers[b] = []
            if o["kind"] == "c":
                last_on[o["eng"]] = i
            else:
                dmas_since.append(i)
        for o in ops:
            o["signal"] = False
        for d in final:
            ops[d]["signal"] = True
        for i, o in enumerate(ops):
            for d in o["deps"]:
                p = ops[d]
                if p["kind"] == "c" and (p["eng"] != o["eng"] or o["kind"] == "d"
                                         or (d in o["raw"] and p["eng"] != "pe")):
                    p["signal"] = True
        cnt = {e: 0 for e in COMPUTE}
        dcnt = [0] * NDMA_SEMS
        nd = 0
        for o in ops:
            if o["kind"] == "c":
                if o["signal"]:
                    cnt[o["eng"]] += 1
                    o["sigval"] = cnt[o["eng"]]
            else:
                s = nd % NDMA_SEMS
                nd += 1
                o["dsem"] = s
                o["dprev"] = dcnt[s] * 16
                dcnt[s] += 1
                o["sigval"] = dcnt[s] * 16

    def emit(self, final):
        nc = self.nc
        self._analyze(final)
        ops = self.ops
        with ExitStack() as es:
            esem = {e: es.enter_context(nc.semaphore("s_" + e)) for e in COMPUTE}
            dsem = [es.enter_context(nc.semaphore("d_%d" % i)) for i in range(NDMA_SEMS)]
            block = es.enter_context(nc.Block())

            def run(engname, engobj):
                seen = {}

                def wait(key, sem, val):
                    if seen.get(key, 0) >= val:
                        return
                    seen[key] = val
                    engobj.wait_ge(sem, val)

                def wait_on(p):
                    if p["kind"] == "c":
                        wait(("c", p["eng"]), esem[p["eng"]], p["sigval"])
                    else:
                        wait(("d", p["dsem"]), dsem[p["dsem"]], p["sigval"])

                for i, o in enumerate(ops):
                    if o["eng"] != engname:
                        continue
                    for d in sorted(o["deps"]):
                        p = ops[d]
                        if p["kind"] == "c" and p["eng"] == engname and o["kind"] == "c":
                            if engname == "pe" or d not in o["raw"]:
                                continue
                        wait_on(p)
                    if o["kind"] == "d":
                        if o["dprev"] > 0:
                            wait(("d", o["dsem"]), dsem[o["dsem"]], o["dprev"])
                        o["fn"](engobj).then_inc(dsem[o["dsem"]], 16)
                    else:
                        ins = o["fn"](engobj)
                        if o["signal"]:
                            ins.then_inc(esem[engname], 1)
                if engname == "sp":
                    for d in final:
                        wait_on(ops[d])

            block.sync(lambda e: run("sp", e))
            block.scalar(lambda e: run("act", e))
            block.vector(lambda e: run("dve", e))
            block.gpsimd(lambda e: run("pool", e))
            block.tensor(lambda e: run("pe", e))


class Arena:
    def __init__(self, nc):
        self.nc = nc
        self.off = SB_BASE
        self.n = 0

    def alloc(self, name, shape, dt):
        nb = {F32: 4, BF16: 2, I32: 4}[dt]
        per = nb
        for s in shape[1:]:
            per *= s
        per = (per + 31) // 32 * 32
        assert self.off + per <= SB_LIMIT, (name, self.off, per)
        self.n += 1
        t = self.nc.alloc_sbuf_tensor_at("%s_%d" % (name, self.n), list(shape), dt, offset=self.off)
        self.off += per
        return t

    def mark(self):
        return self.off

    def release(self, m):
        self.off = m


def build_nc(dbg=None):
    nc = bass.Bass("TRN2", target_bir_lowering=False)
    P = Prog(nc)
    AR = Arena(nc)
    es = ExitStack()
    fin = []

    def din(name, shape, dt=F32):
        return nc.dram_tensor(name, list(shape), dt, kind="ExternalInput").ap()

    def dscr(name, shape, dt=F32):
        return nc.dram_tensor(name, list(shape), dt, kind="Internal").ap()

    def dout(name, shape, dt=F32):
        return nc.dram_tensor(name, list(shape), dt, kind="ExternalOutput").ap()

    x_d = din("x", [L, D])
    cT_d = din("cT", [128, 16])
    wada_d = din("w_ada", [D, 6 * D])
    bada_d = din("b_ada", [1, 6 * D])
    g1_d = din("g_norm1", [1, D])
    g2_d = din("g_norm2", [1, D])
    gf_d = din("g_final", [1, D])
    win_d = din("w_in_t", [40, 128, 16, 128])
    wglu_d = din("w_glu", [1024, 1024])
    wout_d = din("w_out", [D, D])
    betaT_d = din("betaT", [128, 16])
    dT_d = din("dT", [128, 8])
    wr_d = din("wr", [D, 36])
    br_d = din("br", [1, 36])
    NE = 32 if dbg is None or dbg in ("moe", "final") else 1
    wg_d = din("w_gate", [NE * 2048, 1024])
    wu_d = din("w_up", [NE * 2048, 1024])
    wd_d = din("w_down", [NE * 1024, 2048])
    slpar_d = din("sl_par", [128, 3, 64])
    slV_d = din("sl_V", [128, 2, 64, 16])
    slC_d = din("sl_C", [128, 64, 16])
    plpar_d = din("pl_par", [128, 3, 32])
    plC_d = din("pl_C", [128, 2, 32, 16])
    tlpar_d = din("tl_par", [128, 3, 512])
    tlB_d = din("tl_B", [128, 2, 512])
    cst_d = din("cst32", [128, 5, 128])
    rope_d = din("rope", [128, 2, L])
    decay_d = din("decayT", [128, 8, 128])
    xi_d = din("xiT", [128, 8, 128])
    col_d = din("colc", [128, 24])
    mask4_d = din("mask4", [128, 4, 32])
    rt_d = din("rtc", [128, 16 + 64 + 32])

    out_d = dout("out", [L, D])
    mod_d = dscr("mod_d", [1, 6 * D])
    uT_d = dscr("uT_d", [8, 128, L], BF16)
    acc_d = dscr("acc_d", [L + 1, D])
    h2_d = dscr("h2_d", [L + 1, D], BF16)
    slot_d = dscr("slot_d", [8192, 1])
    ti_d = dscr("ti_d", [L + 1, 4])

    dbg_outs = {}

    def dbg_out(name, shape, dt=F32):
        dbg_outs[name] = dout("dbg_" + name, shape, dt)
        return dbg_outs[name]

    with es:
        ps = [es.enter_context(nc.psum_tensor("ps%d" % i, [128, 512], F32)) for i in range(8)]
        PSK = ["ps%d" % i for i in range(8)]

        def V(fn, r=(), w=()):
            return P.op("dve", fn, r, w)

        def A(fn, r=(), w=()):
            return P.op("act", fn, r, w)

        def G(fn, r=(), w=()):
            return P.op("pool", fn, r, w)

        def T(fn, r=(), w=()):
            return P.op("pe", fn, r, w)

        def DS(fn, r=(), w=()):
            return P.dma("sp", fn, r, w)

        def DG(fn, r=(), w=()):
            return P.dma("pool", fn, r, w)

        def phase_barrier(tag):
            sc = barc
            P.barrier(lambda e: e.memset(sc[:], 0.0))

        def dump(name, t, shape, dt, keys):
            fin.append(DS(lambda e: e.dma_start(out=dbg_out(name, shape, dt), in_=t[:]), r=keys))
            P.emit(fin)
            return nc, dbg_outs

        barc = AR.alloc("barc", [128, 8], F32)
        cst = AR.alloc("cst", [128, 5, 128], F32)
        colc = AR.alloc("colc", [128, 24], F32)
        id16 = AR.alloc("id16", [128, 128], BF16)
        ones16 = AR.alloc("ones16", [128, 128], BF16)
        DS(lambda e: e.dma_start(out=cst[:], in_=cst_d), w=["cst"])
        DS(lambda e: e.dma_start(out=colc[:], in_=col_d), w=["colc"])
        V(lambda e: e.tensor_copy(id16[:], cst[:, 0, :]), r=["cst"], w=["id16"])
        V(lambda e: e.tensor_copy(ones16[:], cst[:, 4, :]), r=["cst"], w=["ones16"])
        ident32 = cst[:, 0, :]
        perm32 = cst[:, 1, :]
        onesdiv32 = cst[:, 2, :]
        tri32 = cst[:, 3, :]
        ones32 = cst[:, 4, :]
        SGN, SGNC, PAR, NPAR, EPS6, EPS5, PIDX, HALFPI = [colc[:, i:i + 1] for i in range(8)]
        base_mark = AR.mark()

        m0 = AR.mark()
        cT = AR.alloc("cT", [128, 16], F32)
        scT = AR.alloc("scT", [128, 16], F32)
        bada = AR.alloc("bada", [1, 6 * D], F32)
        wa = [AR.alloc("wa%d" % i, [128, 16, 512], F32) for i in range(2)]
        modrow = [AR.alloc("modrow%d" % i, [1, 512], F32) for i in range(2)]
        DS(lambda e: e.dma_start(out=cT[:], in_=cT_d), w=["cT"])
        DS(lambda e: e.dma_start(out=bada[:], in_=bada_d), w=["bada"])
        A(lambda e: e.activation(scT[:], cT[:], AF.Silu), r=["cT"], w=["scT"])
        wada_v = wada_d.rearrange("(k p) n -> p k n", p=128)
        for nb in range(24):
            b = nb % 2
            DS(lambda e, b=b, nb=nb: e.dma_start(out=wa[b][:], in_=wada_v[:, :, nb * 512:(nb + 1) * 512]), w=["wa%d" % b])
            for k in range(16):
                T(lambda e, b=b, k=k: e.matmul(ps[b][0:1, :], scT[:, k:k + 1], wa[b][:, k, :], start=(k == 0), stop=(k == 15)),
                  r=["scT", "wa%d" % b], w=[PSK[b]])
            V(lambda e, b=b, nb=nb: e.tensor_tensor(modrow[b][:], ps[b][0:1, :], bada[0:1, nb * 512:(nb + 1) * 512], ALU.add),
              r=[PSK[b], "bada"], w=["modrow%d" % b])
            DS(lambda e, b=b, nb=nb: e.dma_start(out=mod_d[0:1, nb * 512:(nb + 1) * 512], in_=modrow[b][:]),
               r=["modrow%d" % b], w=["mod_d"])
        if dbg == "mod":
            t = AR.alloc("dbgm", [1, 6 * D], F32)
            DS(lambda e: e.dma_start(out=t[:], in_=mod_d), r=["mod_d"], w=["dbgm"])
            fin.append(DS(lambda e: e.dma_start(out=dbg_out("mod", [1, 6 * D]), in_=t[:]), r=["dbgm"]))
            P.emit(fin)
            return nc, dbg_outs
        phase_barrier("p0")
        AR.release(m0)

        def mod_bcast(dst, idx, key):
            return DS(lambda e: e.dma_start(out=dst[:], in_=mod_d[0:1, idx * D:(idx + 1) * D].partition_broadcast(128)),
                      r=["mod_d"], w=[key])

        yretT = AR.alloc("yretT", [128, 8, L], BF16)
        mh = AR.mark()
        hT = AR.alloc("hT", [128, 16, L], BF16)
        m1 = AR.mark()
        A1b = AR.alloc("A1b", [128, D], F32)
        B1b = AR.alloc("B1b", [128, D], F32)
        g1b = AR.alloc("g1b", [128, D], F32)
        xb = [AR.alloc("xb%d" % i, [128, D], F32) for i in range(2)]
        junk = AR.alloc("junk", [128, D], F32)
        hf = AR.alloc("hf", [128, D], F32)
        h16 = [AR.alloc("h16_%d" % i, [128, D], BF16) for i in range(2)]
        ss = AR.alloc("ss", [128, 16], F32)
        rs = AR.alloc("rs", [128, 16], F32)
        mod_bcast(A1b, 1, "A1b")
        mod_bcast(B1b, 0, "B1b")
        DS(lambda e: e.dma_start(out=g1b[:], in_=g1_d.partition_broadcast(128)), w=["g1b"])
        V(lambda e: e.scalar_tensor_tensor(out=A1b[:], in0=A1b[:], scalar=1.0, in1=g1b[:], op0=ALU.add, op1=ALU.mult),
          r=["A1b", "g1b"], w=["A1b"])
        V(lambda e: e.memset(ss[:], 0.0), w=["ss"])

        def rms_rstd_g(junk, ss, rs, xt_ap, xkey, col, eps_ap, n):
            A(lambda e: e.activation(junk[:], xt_ap, AF.Square, accum_out=ss[:, col:col + 1]), r=[xkey, "ss"], w=["junk", "ss"])
            A(lambda e: e.activation(rs[:, col:col + 1], ss[:, col:col + 1], AF.Sqrt, bias=eps_ap, scale=1.0 / n),
              r=["ss", "colc"], w=["rs"])
            V(lambda e: e.reciprocal(rs[:, col:col + 1], rs[:, col:col + 1]), r=["rs"], w=["rs"])

        def rms_rstd(xt_ap, xkey, col, eps_ap, n):
            A(lambda e: e.activation(junk[:], xt_ap, AF.Square, accum_out=ss[:, col:col + 1]), r=[xkey, "ss"], w=["junk", "ss"])
            A(lambda e: e.activation(rs[:, col:col + 1], ss[:, col:col + 1], AF.Sqrt, bias=eps_ap, scale=1.0 / n),
              r=["ss", "colc"], w=["rs"])
            V(lambda e: e.reciprocal(rs[:, col:col + 1], rs[:, col:col + 1]), r=["rs"], w=["rs"])

        for tt in range(16):
            b = tt % 2
            DS(lambda e, b=b, tt=tt: e.dma_start(out=xb[b][:], in_=x_d[tt * 128:(tt + 1) * 128, :]), w=["xb%d" % b])
            rms_rstd_g(junk, ss, rs, xb[b][:], "xb%d" % b, tt, EPS6, D)
            V(lambda e, b=b, tt=tt: e.scalar_tensor_tensor(out=hf[:], in0=xb[b][:], scalar=rs[:, tt:tt + 1], in1=A1b[:],
                                                           op0=ALU.mult, op1=ALU.mult), r=["xb%d" % b, "rs", "A1b"], w=["hf"])
            V(lambda e, b=b: e.tensor_tensor(h16[b][:], hf[:], B1b[:], ALU.add), r=["hf", "B1b"], w=["h16_%d" % b])
            for kg in range(4):
                pb = 2 + (tt * 4 + kg) % 4
                for kk in range(4):
                    k = kg * 4 + kk
                    T(lambda e, pb=pb, kk=kk, k=k, b=b: e.matmul(ps[pb][:, kk * 128:(kk + 1) * 128], h16[b][:, k * 128:(k + 1) * 128],
                                                                 id16[:], start=True, stop=True),
                      r=["h16_%d" % b, "id16"], w=[PSK[pb]])
                ev = A if kg % 2 == 0 else V
                if kg % 2 == 0:
                    A(lambda e, pb=pb, kg=kg, tt=tt: e.copy(hT[:, kg * 4:(kg + 1) * 4, tt * 128:(tt + 1) * 128],
                                                           ps[pb][:].rearrange("p (a c) -> p a c", a=4)),
                      r=[PSK[pb]], w=["hT%d" % tt])
                else:
                    V(lambda e, pb=pb, kg=kg, tt=tt: e.tensor_copy(hT[:, kg * 4:(kg + 1) * 4, tt * 128:(tt + 1) * 128],
                                                                  ps[pb][:].rearrange("p (a c) -> p a c", a=4)),
                      r=[PSK[pb]], w=["hT%d" % tt])
        if dbg == "hT":
            t = AR.alloc("dbgh", [128, 16, L], F32)
            V(lambda e: e.tensor_copy(t[:], hT[:]), r=["hT%d" % i for i in range(16)], w=["dbgh"])
            fin.append(DS(lambda e: e.dma_start(out=dbg_out("hT", [128, 16, L]), in_=t[:]), r=["dbgh"]))
            P.emit(fin)
            return nc, dbg_outs
        phase_barrier("p1")
        AR.release(m1)

        m2 = AR.mark()
        win = [AR.alloc("win%d" % i, [128, 16, 128], BF16) for i in range(6)]
        wctr = [0]

        def load_win(tile_idx):
            i = wctr[0] % 6
            wctr[0] += 1
            DG(lambda e, i=i, tile_idx=tile_idx: e.dma_start(out=win[i][:], in_=win_d[tile_idx]), w=["win%d" % i])
            return i

        HTK = ["hT%d" % i for i in range(16)]
        pctr = [0]

        def next_ps():
            pctr[0] += 1
            return pctr[0] % 8

        def inproj_block(wi, tb, pb):
            for k in range(16):
                T(lambda e, wi=wi, tb=tb, pb=pb, k=k: e.matmul(ps[pb][:], win[wi][:, k, :], hT[:, k, tb * 512:(tb + 1) * 512],
                                                               start=(k == 0), stop=(k == 15)),
                  r=["win%d" % wi] + HTK[tb * 4:tb * 4 + 4], w=[PSK[pb]])

        ut = [AR.alloc("ut%d" % i, [128, L], BF16) for i in range(2)]
        for j in range(8):
            wi = load_win(j)
            ub = j % 2
            for tb in range(4):
                pb = next_ps()
                inproj_block(wi, tb, pb)
                ev = A if tb % 2 == 0 else V
                if tb % 2 == 0:
                    A(lambda e, pb=pb, ub=ub, tb=tb: e.copy(ut[ub][:, tb * 512:(tb + 1) * 512], ps[pb][:]), r=[PSK[pb]], w=["ut%d" % ub])
                else:
                    V(lambda e, pb=pb, ub=ub, tb=tb: e.tensor_copy(ut[ub][:, tb * 512:(tb + 1) * 512], ps[pb][:]), r=[PSK[pb]], w=["ut%d" % ub])
            DS(lambda e, ub=ub, j=j: e.dma_start(out=uT_d[j], in_=ut[ub][:]), r=["ut%d" % ub], w=["uT_d"])
        if dbg == "uT":
            t = AR.alloc("dbgu", [128, 8, L], BF16)
            t2 = AR.alloc("dbgu2", [128, 8, L], F32)
            DS(lambda e: e.dma_start(out=t[:], in_=uT_d.rearrange("j p t -> p j t")), r=["uT_d"], w=["dbgu"])
            V(lambda e: e.tensor_copy(t2[:], t[:]), r=["dbgu"], w=["dbgu2"])
            fin.append(DS(lambda e: e.dma_start(out=dbg_out("uT", [128, 8, L]), in_=t2[:]), r=["dbgu2"]))
            P.emit(fin)
            return nc, dbg_outs

        rope = AR.alloc("rope", [128, 2, L], F32)
        decT = AR.alloc("decT", [128, 8, 128], F32)
        xiT = AR.alloc("xiT", [128, 8, 128], F32)
        betaT = AR.alloc("betaT", [128, 16], F32)
        DS(lambda e: e.dma_start(out=rope[:], in_=rope_d), w=["rope"])
        DS(lambda e: e.dma_start(out=decT[:], in_=decay_d), w=["decT"])
        DS(lambda e: e.dma_start(out=xiT[:], in_=xi_d), w=["xiT"])
        DS(lambda e: e.dma_start(out=betaT[:], in_=betaT_d), w=["betaT"])
        qr = AR.alloc("qr", [128, L], BF16)
        kr = AR.alloc("kr", [128, L], BF16)
        qxi = AR.alloc("qxi", [128, L], BF16)
        vtok = AR.alloc("vtok", [128, 16, 128], BF16)
        kz = AR.alloc("kz", [128, 16, 128], BF16)
        gs = AR.alloc("gs", [128, 512], F32)
        qf = [AR.alloc("qf%d" % i, [128, 512], F32) for i in range(2)]
        t1 = [AR.alloc("t1_%d" % i, [128, 512], F32) for i in range(2)]
        t2 = [AR.alloc("t2_%d" % i, [128, 512], F32) for i in range(2)]
        vT16 = AR.alloc("vT16", [128, 512], BF16)
        Sd = [AR.alloc("Sd%d" % i, [128, 128], BF16) for i in range(2)]
        Rf = AR.alloc("Rf", [128, 128], F32)
        Rb = [AR.alloc("Rb%d" % i, [128, 128], BF16) for i in range(2)]
        yh = AR.alloc("yh", [128, 512], F32)
        ysq = AR.alloc("ysq", [128, 512], F32)
        mean = AR.alloc("mean", [128, 512], F32)
        var = AR.alloc("var", [128, 512], F32)
        yc = AR.alloc("yc", [128, 512], F32)
        gC = [float((1.0 - 2.0 ** (-5.0 - h)) ** 128) for h in range(8)]

        for hh in range(8):
            wq, wk, wv, wgt = [load_win(8 + s * 8 + hh) for s in range(4)]
            V(lambda e: e.memset(Rf[:], 0.0), w=["Rf"])
            for tb in range(4):
                sl = slice(tb * 512, (tb + 1) * 512)
                for which, wi, dst, dkey in (("q", wq, qr, "qr"), ("k", wk, kr, "kr")):
                    b2 = 0 if which == "q" else 1
                    pb = next_ps()
                    inproj_block(wi, tb, pb)
                    A(lambda e, pb=pb, b2=b2: e.copy(qf[b2][:], ps[pb][:]), r=[PSK[pb]], w=["qf%d" % b2])
                    pb2 = next_ps()
                    T(lambda e, pb2=pb2, b2=b2: e.matmul(ps[pb2][:], perm32, qf[b2][:], start=True, stop=True),
                      r=["cst", "qf%d" % b2], w=[PSK[pb2]])
                    G(lambda e, b2=b2, sl=sl: e.tensor_tensor(t1[b2][:], qf[b2][:], rope[:, 0, sl], ALU.mult),
                      r=["qf%d" % b2, "rope"], w=["t1_%d" % b2])
                    V(lambda e, pb2=pb2, b2=b2, sl=sl: e.tensor_tensor(t2[b2][:], ps[pb2][:], rope[:, 1, sl], ALU.mult),
                      r=[PSK[pb2], "rope"], w=["t2_%d" % b2])
                    V(lambda e, b2=b2: e.tensor_tensor(t1[b2][:], t1[b2][:], t2[b2][:], ALU.add),
                      r=["t1_%d" % b2, "t2_%d" % b2], w=["t1_%d" % b2])
                    A(lambda e, b2=b2, dst=dst, sl=sl: e.copy(dst[:, sl], t1[b2][:]), r=["t1_%d" % b2], w=[dkey])
                    if which == "q":
                        V(lambda e, hh=hh, sl=sl: e.tensor_tensor(qxi[:, sl].rearrange("p (a c) -> p a c", a=4),
                                                                  t1[0][:].rearrange("p (a c) -> p a c", a=4),
                                                                  xiT[:, hh, :].unsqueeze(1).to_broadcast([128, 4, 128]), ALU.mult),
                          r=["t1_0", "xiT"], w=["qxi"])
                pb = next_ps()
                inproj_block(wv, tb, pb)
                V(lambda e, pb=pb: e.tensor_copy(vT16[:], ps[pb][:]), r=[PSK[pb]], w=["vT16"])
                pb = next_ps()
                for c in range(4):
                    T(lambda e, pb=pb, c=c: e.matmul(ps[pb][:, c * 128:(c + 1) * 128], vT16[:, c * 128:(c + 1) * 128], id16[:],
                                                     start=True, stop=True), r=["vT16", "id16"], w=[PSK[pb]])
                A(lambda e, pb=pb, tb=tb: e.copy(vtok[:, tb * 4:(tb + 1) * 4, :], ps[pb][:].rearrange("p (a c) -> p a c", a=4)),
                  r=[PSK[pb]], w=["vtok"])
                pb = next_ps()
                for c in range(4):
                    T(lambda e, pb=pb, c=c, tb=tb: e.matmul(ps[pb][:, c * 128:(c + 1) * 128], kr[:, tb * 512 + c * 128: tb * 512 + (c + 1) * 128],
                                                            id16[:], start=True, stop=True), r=["kr", "id16"], w=[PSK[pb]])
                V(lambda e, pb=pb, tb=tb, hh=hh: e.tensor_scalar(kz[:, tb * 4:(tb + 1) * 4, :], ps[pb][:].rearrange("p (a c) -> p a c", a=4),
                                                                 colc[:, 8 + hh:9 + hh], None, ALU.mult),
                  r=[PSK[pb], "colc"], w=["kz"])
                pb = next_ps()
                inproj_block(wgt, tb, pb)
                A(lambda e, pb=pb: e.activation(gs[:], ps[pb][:], AF.Silu), r=[PSK[pb]], w=["gs"])
                pbo = next_ps()
                for c in range(4):
                    n = tb * 4 + c
                    cs = slice(n * 128, (n + 1) * 128)
                    pS = next_ps()
                    while pS == pbo:
                        pS = next_ps()
                    T(lambda e, pS=pS, cs=cs: e.matmul(ps[pS][:, 0:128], kr[:, cs], qr[:, cs], start=True, stop=True),
                      r=["kr", "qr"], w=[PSK[pS]])
                    T(lambda e, pS=pS, n=n: e.matmul(ps[pS][:, 128:256], kz[:, n, :], vtok[:, n, :], start=True, stop=True),
                      r=["kz", "vtok"], w=[PSK[pS]])
                    sb_i = n % 2
                    V(lambda e, pS=pS, sb_i=sb_i, hh=hh: e.tensor_tensor(Sd[sb_i][:], ps[pS][:, 0:128], decT[:, hh, :], ALU.mult),
                      r=[PSK[pS], "decT"], w=["Sd%d" % sb_i])
                    T(lambda e, pbo=pbo, c=c, n=n, sb_i=sb_i: e.matmul(ps[pbo][:, c * 128:(c + 1) * 128], vtok[:, n, :], Sd[sb_i][:],
                                                                      start=True, stop=(n == 0)),
                      r=["vtok", "Sd%d" % sb_i], w=[PSK[pbo]])
                    if n > 0:
                        T(lambda e, pbo=pbo, c=c, cs=cs, n=n: e.matmul(ps[pbo][:, c * 128:(c + 1) * 128], Rb[n % 2][:], qxi[:, cs],
                                                                      start=False, stop=True),
                          r=["Rb%d" % (n % 2), "qxi"], w=[PSK[pbo]])
                    V(lambda e, pS=pS, hh=hh: e.scalar_tensor_tensor(out=Rf[:], in0=Rf[:], scalar=gC[hh], in1=ps[pS][:, 128:256],
                                                                     op0=ALU.mult, op1=ALU.add), r=["Rf", PSK[pS]], w=["Rf"])
                    A(lambda e, n=n: e.copy(Rb[(n + 1) % 2][:], Rf[:]), r=["Rf"], w=["Rb%d" % ((n + 1) % 2)])
                A(lambda e, pbo=pbo: e.copy(yh[:], ps[pbo][:]), r=[PSK[pbo]], w=["yh"])
                A(lambda e: e.activation(ysq[:], yh[:], AF.Square), r=["yh"], w=["ysq"])
                pm = next_ps()
                pq = next_ps()
                T(lambda e, pm=pm: e.matmul(ps[pm][:], onesdiv32, yh[:], start=True, stop=True), r=["cst", "yh"], w=[PSK[pm]])
                T(lambda e, pq=pq: e.matmul(ps[pq][:], onesdiv32, ysq[:], start=True, stop=True), r=["cst", "ysq"], w=[PSK[pq]])
                A(lambda e, pm=pm: e.copy(mean[:], ps[pm][:]), r=[PSK[pm]], w=["mean"])
                V(lambda e: e.tensor_tensor(var[:], mean[:], mean[:], ALU.mult), r=["mean"], w=["var"])
                V(lambda e, pq=pq: e.tensor_tensor(var[:], ps[pq][:], var[:], ALU.subtract), r=[PSK[pq], "var"], w=["var"])
                A(lambda e: e.activation(var[:], var[:], AF.Sqrt, bias=EPS5, scale=1.0), r=["var", "colc"], w=["var"])
                V(lambda e: e.reciprocal(var[:], var[:]), r=["var"], w=["var"])
                V(lambda e: e.tensor_tensor(yc[:], yh[:], mean[:], ALU.subtract), r=["yh", "mean"], w=["yc"])
                V(lambda e: e.tensor_tensor(yc[:], yc[:], var[:], ALU.mult), r=["yc", "var"], w=["yc"])
                V(lambda e, hh=hh, sl=sl: e.scalar_tensor_tensor(out=yretT[:, hh, sl], in0=yc[:], scalar=betaT[:, 8 + hh:9 + hh], in1=gs[:],
                                                                 op0=ALU.mult, op1=ALU.mult), r=["yc", "betaT", "gs"], w=["yretT"])
        if dbg == "yret":
            fin.append(DS(lambda e: e.dma_start(out=dbg_out("yret", [128, 8, L], BF16), in_=yretT[:]), r=["yretT"]))
            P.emit(fin)
            return nc, dbg_outs
        phase_barrier("p2")
        AR.release(mh)
        TWO_PI = 2.0 * math.pi
        TOP, BOT, NTOP, NBOT = [colc[:, i:i + 1] for i in range(16, 20)]
        zT = AR.alloc("zT", [128, 8, L], BF16)
        BD = AR.alloc("BD", [128, 8, 8, 128], BF16)
        HC = AR.alloc("HC", [128, 32, 8, 2, 32], BF16)
        ArB = AR.alloc("ArB", [128, 2, 32], F32)
        AiB = AR.alloc("AiB", [128, 2, 32], F32)
        mask4 = AR.alloc("mask4", [128, 4, 32], F32)
        dT = AR.alloc("dT", [128, 8], F32)
        betaS = AR.alloc("betaS", [128, 16], F32)
        DS(lambda e: e.dma_start(out=mask4[:], in_=mask4_d), w=["mask4"])
        DS(lambda e: e.dma_start(out=dT[:], in_=dT_d), w=["dT"])
        DS(lambda e: e.dma_start(out=betaS[:], in_=betaT_d), w=["betaS"])
        ms = AR.mark()
        uid = [0]

        def tmp(shape, dt=F32):
            uid[0] += 1
            return AR.alloc("tmp%d" % uid[0], shape, dt), "tmp%d" % uid[0]

        def disc(par, pk, N, ks):
            K = len(ks)
            dt_, dtk = tmp([128, N]); lr, lrk = tmp([128, N]); li, lik = tmp([128, N])
            A(lambda e: e.activation(dt_[:], par[:, 2, :], AF.Exp), r=[pk], w=[dtk])
            V(lambda e: e.tensor_tensor(lr[:], dt_[:], par[:, 0, :], ALU.mult), r=[dtk, pk], w=[lrk])
            V(lambda e: e.tensor_tensor(li[:], dt_[:], par[:, 1, :], ALU.mult), r=[dtk, pk], w=[lik])
            MAG, mk = tmp([128, K, N]); ANG, ak = tmp([128, K, N]); TF, tfk = tmp([128, K, N]); TI, tik = tmp([128, K, N], I32)
            SN, snk = tmp([128, K, N]); CSN, csk = tmp([128, K, N])
            for i, k in enumerate(ks):
                A(lambda e, i=i, k=k: e.activation(MAG[:, i, :], lr[:], AF.Exp, scale=float(k)), r=[lrk], w=[mk])
                V(lambda e, i=i, k=k: e.tensor_scalar(ANG[:, i, :], li[:], float(k), None, ALU.mult), r=[lik], w=[ak])
            for shift, OUT, ok in ((0.0, SN, snk), (math.pi / 2, CSN, csk)):
                V(lambda e, shift=shift: e.tensor_scalar(TF[:], ANG[:], shift, 1.0 / TWO_PI, ALU.add, ALU.mult), r=[ak], w=[tfk])
                V(lambda e: e.tensor_copy(TI[:], TF[:]), r=[tfk], w=[tik])
                V(lambda e: e.tensor_copy(TF[:], TI[:]), r=[tik], w=[tfk])
                V(lambda e: e.scalar_tensor_tensor(out=TF[:], in0=TF[:], scalar=-TWO_PI, in1=ANG[:], op0=ALU.mult, op1=ALU.add),
                  r=[tfk, ak], w=[tfk])
                V(lambda e, shift=shift: e.tensor_scalar(TF[:], TF[:], shift, 3.1415925, ALU.add, ALU.min), r=[tfk], w=[tfk])
                V(lambda e: e.tensor_scalar(TF[:], TF[:], -3.1415925, None, ALU.max), r=[tfk], w=[tfk])
                A(lambda e, OUT=OUT: e.activation(OUT[:], TF[:], AF.Sin), r=[tfk], w=[ok])
            V(lambda e: e.tensor_tensor(CSN[:], CSN[:], MAG[:], ALU.mult), r=[csk, mk], w=[csk])
            V(lambda e: e.tensor_tensor(SN[:], SN[:], MAG[:], ALU.mult), r=[snk, mk], w=[snk])
            return (CSN, csk), (SN, snk)

        def zoh(par, pk, N, E1re, E1im, ek):
            nre, nk = tmp([128, N]); inv, ik = tmp([128, N]); fre, frk = tmp([128, N]); fim, fik = tmp([128, N]); tq, tqk = tmp([128, N])
            V(lambda e: e.tensor_scalar(nre[:], E1re, -1.0, None, ALU.add), r=ek, w=[nk])
            V(lambda e: e.tensor_tensor(inv[:], par[:, 0, :], par[:, 0, :], ALU.mult), r=[pk], w=[ik])
            V(lambda e: e.tensor_tensor(tq[:], par[:, 1, :], par[:, 1, :], ALU.mult), r=[pk], w=[tqk])
            V(lambda e: e.tensor_tensor(inv[:], inv[:], tq[:], ALU.add), r=[ik, tqk], w=[ik])
            V(lambda e: e.reciprocal(inv[:], inv[:]), r=[ik], w=[ik])
            V(lambda e: e.tensor_tensor(fre[:], nre[:], par[:, 0, :], ALU.mult), r=[nk, pk], w=[frk])
            V(lambda e: e.tensor_tensor(tq[:], E1im, par[:, 1, :], ALU.mult), r=ek + [pk, tqk], w=[tqk])
            V(lambda e: e.tensor_tensor(fre[:], fre[:], tq[:], ALU.add), r=[frk, tqk], w=[frk])
            V(lambda e: e.tensor_tensor(fre[:], fre[:], inv[:], ALU.mult), r=[frk, ik], w=[frk])
            V(lambda e: e.tensor_tensor(fim[:], E1im, par[:, 0, :], ALU.mult), r=ek + [pk], w=[fik])
            V(lambda e: e.tensor_tensor(tq[:], nre[:], par[:, 1, :], ALU.mult), r=[nk, pk, tqk], w=[tqk])
            V(lambda e: e.tensor_tensor(fim[:], fim[:], tq[:], ALU.subtract), r=[fik, tqk], w=[fik])
            V(lambda e: e.tensor_tensor(fim[:], fim[:], inv[:], ALU.mult), r=[fik, ik], w=[fik])
            return (fre, frk), (fim, fik)

        def _sl():
            slpar = AR.alloc("slpar", [128, 3, 64], F32)
            slV = AR.alloc("slV", [128, 2, 64, 16], F32)
            slC = AR.alloc("slC", [128, 64, 16], F32)
            DS(lambda e: e.dma_start(out=slpar[:], in_=slpar_d), w=["slpar"])
            DS(lambda e: e.dma_start(out=slV[:], in_=slV_d), w=["slV"])
            DS(lambda e: e.dma_start(out=slC[:], in_=slC_d), w=["slC"])
            (Ere, erk), (Eim, eik) = disc(slpar, "slpar", 64, list(range(8)))
            (fre, frk), (fim, fik) = zoh(slpar, "slpar", 64, Ere[:, 1, :], Eim[:, 1, :], [erk, eik])
            U1, u1k = tmp([128, 64, 16]); U2, u2k = tmp([128, 64, 16]); tA, tAk = tmp([128, 64, 16]); tB, tBk = tmp([128, 64, 16])
            PSb, psbk = tmp([128, 8, 64, 16], BF16); CSb, csbk = tmp([128, 64, 16], BF16)
            bc = lambda ap: ap.unsqueeze(2).to_broadcast([128, 64, 16])
            V(lambda e: e.tensor_tensor(tA[:], slV[:, 0], bc(fre[:]), ALU.mult), r=["slV", frk], w=[tAk])
            V(lambda e: e.tensor_tensor(tB[:], slV[:, 1], bc(fim[:]), ALU.mult), r=["slV", fik], w=[tBk])
            V(lambda e: e.scalar_tensor_tensor(out=U1[:], in0=tB[:], scalar=SGN, in1=tA[:], op0=ALU.mult, op1=ALU.add), r=[tAk, tBk, "colc"], w=[u1k])
            V(lambda e: e.tensor_tensor(tA[:], slV[:, 1], bc(fre[:]), ALU.mult), r=["slV", frk], w=[tAk])
            V(lambda e: e.tensor_tensor(tB[:], slV[:, 0], bc(fim[:]), ALU.mult), r=["slV", fik], w=[tBk])
            V(lambda e: e.scalar_tensor_tensor(out=U2[:], in0=tB[:], scalar=SGNC, in1=tA[:], op0=ALU.mult, op1=ALU.add), r=[tAk, tBk, "colc"], w=[u2k])
            for j in range(8):
                V(lambda e, j=j: e.tensor_tensor(tA[:], U1[:], bc(Ere[:, j, :]), ALU.mult), r=[u1k, erk], w=[tAk])
                V(lambda e, j=j: e.tensor_tensor(tB[:], U2[:], bc(Eim[:, j, :]), ALU.mult), r=[u2k, eik], w=[tBk])
                V(lambda e, j=j: e.scalar_tensor_tensor(out=PSb[:, j], in0=tB[:], scalar=SGN, in1=tA[:], op0=ALU.mult, op1=ALU.add),
                  r=[tAk, tBk, "colc"], w=[psbk])
            V(lambda e: e.tensor_scalar(CSb[:], slC[:], SGNC, None, ALU.mult), r=["slC", "colc"], w=[csbk])
            for jt in range(8):
                pb = next_ps()
                for pr in range(4):
                    g0 = 2 * (4 * jt + pr)
                    for j in range(8):
                        kw = dict(tile_position=(0, 96)) if pr == 3 else {}
                        T(lambda e, pb=pb, pr=pr, j=j, g0=g0, kw=kw: e.matmul(ps[pb][32 * pr:32 * pr + 32, j * 32:(j + 1) * 32],
                                                                            PSb[:, j, g0:g0 + 2, :], CSb[:, g0:g0 + 2, :],
                                                                            start=True, stop=True, **kw),
                          r=[psbk, csbk], w=[PSK[pb]])
                for pr in range(4):
                    V(lambda e, pb=pb, pr=pr, jt=jt: e.tensor_tensor(BD[:, jt, :, 32 * pr:32 * pr + 32],
                                                                     ps[pb][:, 0:256].rearrange("p (j c) -> p j c", j=8),
                                                                     mask4[:, pr, :].unsqueeze(1).to_broadcast([128, 8, 32]), ALU.mult),
                      r=[PSK[pb], "mask4"], w=["BD"])

        _sl()
        if dbg == "ssm_BD":
            return dump("BD", BD, [128, 8, 8, 128], BF16, ["BD"])
        phase_barrier("sl")
        AR.release(ms)

        def _pl():
            plpar = AR.alloc("plpar", [128, 3, 32], F32)
            plC = AR.alloc("plC", [128, 2, 32, 16], F32)
            DS(lambda e: e.dma_start(out=plpar[:], in_=plpar_d), w=["plpar"])
            DS(lambda e: e.dma_start(out=plC[:], in_=plC_d), w=["plC"])
            (Ere, erk), (Eim, eik) = disc(plpar, "plpar", 32, list(range(1, 9)))
            qa, qak = tmp([128, 32, 16]); qb, qbk = tmp([128, 32, 16]); Qre, qrk = tmp([128, 32, 16]); Qim, qik = tmp([128, 32, 16])
            bc2 = lambda ap: ap.unsqueeze(2).to_broadcast([128, 32, 16])
            for t in range(8):
                V(lambda e, t=t: e.tensor_tensor(qa[:], plC[:, 0], bc2(Ere[:, t, :]), ALU.mult), r=["plC", erk], w=[qak])
                V(lambda e, t=t: e.tensor_tensor(qb[:], plC[:, 1], bc2(Eim[:, t, :]), ALU.mult), r=["plC", eik], w=[qbk])
                V(lambda e: e.tensor_tensor(Qre[:], qa[:], qb[:], ALU.subtract), r=[qak, qbk], w=[qrk])
                V(lambda e, t=t: e.tensor_tensor(qa[:], plC[:, 0], bc2(Eim[:, t, :]), ALU.mult), r=["plC", eik], w=[qak])
                V(lambda e, t=t: e.tensor_tensor(qb[:], plC[:, 1], bc2(Ere[:, t, :]), ALU.mult), r=["plC", erk], w=[qbk])
                V(lambda e: e.tensor_tensor(Qim[:], qa[:], qb[:], ALU.add), r=[qak, qbk], w=[qik])
                V(lambda e, t=t: e.tensor_scalar(HC[:, :, t, 0, 0:16], Qre[:], TOP, None, ALU.mult), r=[qrk, "colc"], w=["HC"])
                V(lambda e, t=t: e.tensor_scalar(HC[:, :, t, 0, 16:32], Qre[:], BOT, None, ALU.mult), r=[qrk, "colc"], w=["HC"])
                V(lambda e, t=t: e.tensor_scalar(HC[:, :, t, 1, 0:16], Qim[:], NTOP, None, ALU.mult), r=[qik, "colc"], w=["HC"])
                V(lambda e, t=t: e.tensor_scalar(HC[:, :, t, 1, 16:32], Qim[:], NBOT, None, ALU.mult), r=[qik, "colc"], w=["HC"])
            for ri in range(2):
                V(lambda e, ri=ri: e.tensor_copy(ArB[:, ri, :], Ere[:, 7, :]), r=[erk], w=["ArB"])
                V(lambda e, ri=ri: e.tensor_copy(AiB[:, ri, :], Eim[:, 7, :]), r=[eik], w=["AiB"])

        _pl()
        if dbg == "ssm_HC":
            return dump("HC", HC, [128, 32, 8, 2, 32], BF16, ["HC"])
        phase_barrier("pl")
        AR.release(ms)

        PTre = AR.alloc("PTre", [128, 8, 512], F32)
        PTim = AR.alloc("PTim", [128, 8, 512], F32)
        mt2 = AR.mark()
        tlpar = AR.alloc("tlpar", [128, 3, 512], F32)
        tlB = AR.alloc("tlB", [128, 2, 512], F32)
        Bbre = AR.alloc("Bbre", [128, 512], F32)
        Bbim = AR.alloc("Bbim", [128, 512], F32)
        ta = AR.alloc("tla", [128, 512], F32)
        tb_ = AR.alloc("tlb", [128, 512], F32)
        DS(lambda e: e.dma_start(out=tlpar[:], in_=tlpar_d), w=["tlpar"])
        DS(lambda e: e.dma_start(out=tlB[:], in_=tlB_d), w=["tlB"])
        mt = AR.mark()

        def _tl0():
            (Ere, erk), (Eim, eik) = disc(tlpar, "tlpar", 512, [1])
            (fre, frk), (fim, fik) = zoh(tlpar, "tlpar", 512, Ere[:, 0, :], Eim[:, 0, :], [erk, eik])
            V(lambda e: e.tensor_tensor(ta[:], fre[:], tlB[:, 0, :], ALU.mult), r=[frk, "tlB"], w=["tla"])
            V(lambda e: e.tensor_tensor(tb_[:], fim[:], tlB[:, 1, :], ALU.mult), r=[fik, "tlB"], w=["tlb"])
            V(lambda e: e.tensor_tensor(Bbre[:], ta[:], tb_[:], ALU.subtract), r=["tla", "tlb"], w=["Bbre"])
            V(lambda e: e.tensor_tensor(ta[:], fre[:], tlB[:, 1, :], ALU.mult), r=[frk, "tlB"], w=["tla"])
            V(lambda e: e.tensor_tensor(tb_[:], fim[:], tlB[:, 0, :], ALU.mult), r=[fik, "tlB"], w=["tlb"])
            V(lambda e: e.tensor_tensor(Bbim[:], ta[:], tb_[:], ALU.add), r=["tla", "tlb"], w=["Bbim"])

        def _tltau(tau):
            (Ere, erk), (Eim, eik) = disc(tlpar, "tlpar", 512, [7 - tau])
            V(lambda e: e.tensor_tensor(ta[:], Ere[:, 0, :], Bbre[:], ALU.mult), r=[erk, "Bbre"], w=["tla"])
            V(lambda e: e.tensor_tensor(tb_[:], Eim[:, 0, :], Bbim[:], ALU.mult), r=[eik, "Bbim"], w=["tlb"])
            V(lambda e: e.tensor_tensor(PTre[:, tau, :], ta[:], tb_[:], ALU.subtract), r=["tla", "tlb"], w=["PTre"])
            V(lambda e: e.tensor_tensor(ta[:], Ere[:, 0, :], Bbim[:], ALU.mult), r=[erk, "Bbim"], w=["tla"])
            V(lambda e: e.tensor_tensor(tb_[:], Eim[:, 0, :], Bbre[:], ALU.mult), r=[eik, "Bbre"], w=["tlb"])
            V(lambda e: e.tensor_tensor(PTim[:, tau, :], ta[:], tb_[:], ALU.add), r=["tla", "tlb"], w=["PTim"])

        _tl0()
        for tau in range(8):
            phase_barrier("tl%d" % tau)
            AR.release(mt)
            _tltau(tau)
        if dbg == "ssm_PT":
            fin.append(DS(lambda e: e.dma_start(out=dbg_out("PTim", [128, 8, 512], F32), in_=PTim[:]), r=["PTim"]))
            return dump("PTre", PTre, [128, 8, 512], F32, ["PTre"])
        phase_barrier("tl")
        AR.release(mt2)
        XS = AR.alloc("XS", [128, 2, 256, 32], BF16)

        GT = [AR.alloc("GT%d" % i, [128, 8, 2, 128], BF16) for i in range(2)]
        ut3 = [AR.alloc("ut3_%d" % i, [128, L], BF16) for i in range(2)]
        for j in range(8):
            b = j % 2
            DS(lambda e, b=b, j=j: e.dma_start(out=ut3[b][:], in_=uT_d[j]), r=["uT_d"], w=["ut%d" % b])
            for ri, PT, ptk in ((0, PTre, "PTre"), (1, PTim, "PTim")):
                for gg, colsel in ((0, NPAR), (1, PAR)):
                    V(lambda e, b=b, j=j, ri=ri, gg=gg, PT=PT, colsel=colsel: e.tensor_scalar(
                        GT[b][:, :, ri, gg * 64:(gg + 1) * 64], PT[:, :, j * 64:(j + 1) * 64], colsel, None, ALU.mult),
                      r=[ptk, "colc"], w=["GT%d" % b])
            for pr in range(4):
                pb = next_ps()
                for ri in range(2):
                    for tau in range(8):
                        kw = dict(tile_position=(96, 0)) if pr == 3 else {}
                        T(lambda e, pb=pb, b=b, pr=pr, ri=ri, tau=tau, kw=kw: e.matmul(
                            ps[pb][:, ri * 256:(ri + 1) * 256], GT[b][32 * pr:32 * pr + 32, tau, ri, :], ut3[b][32 * pr:32 * pr + 32, tau::8],
                            start=(tau == 0), stop=(tau == 7), **kw),
                          r=["GT%d" % b, "ut%d" % b], w=[PSK[pb]])
                ev = A if pr % 2 == 0 else V
                if pr % 2 == 0:
                    A(lambda e, pb=pb, j=j, pr=pr: e.copy(XS[:, :, :, 4 * j + pr], ps[pb][:].rearrange("p (r n) -> p r n", r=2)),
                      r=[PSK[pb]], w=["XS"])
                else:
                    V(lambda e, pb=pb, j=j, pr=pr: e.tensor_copy(XS[:, :, :, 4 * j + pr], ps[pb][:].rearrange("p (r n) -> p r n", r=2)),
                      r=[PSK[pb]], w=["XS"])

        if dbg == "ssm_X":
            return dump("XS", XS, [128, 2, 256, 32], BF16, ["XS"])
        st = AR.alloc("st", [128, 2, 32], F32)
        T13 = AR.alloc("T13", [128, 2, 32], F32)
        T24 = AR.alloc("T24", [128, 2, 32], F32)
        V(lambda e: e.tensor_copy(st[:], XS[:, :, 0, :]), r=["XS"], w=["st"])
        for n in range(1, 255):
            V(lambda e: e.tensor_tensor(T13[:], st[:], ArB[:], ALU.mult), r=["st", "ArB"], w=["T13"])
            V(lambda e: e.tensor_tensor(T24[:], st[:], AiB[:], ALU.mult), r=["st", "AiB"], w=["T24"])
            V(lambda e: e.tensor_tensor(st[:, 0, :], T13[:, 0, :], T24[:, 1, :], ALU.subtract), r=["T13", "T24"], w=["st"])
            V(lambda e: e.tensor_tensor(st[:, 1, :], T13[:, 1, :], T24[:, 0, :], ALU.add), r=["T13", "T24"], w=["st"])
            V(lambda e, n=n: e.tensor_tensor(st[:], st[:], XS[:, :, n, :], ALU.add), r=["st", "XS"], w=["st"])
            V(lambda e, n=n: e.tensor_copy(XS[:, :, n, :], st[:]), r=["st"], w=["XS"])

        if dbg == "ssm_S":
            return dump("XS", XS, [128, 2, 256, 32], BF16, ["XS"])
        ytmp = [AR.alloc("ytmp%d" % i, [128, 256], F32) for i in range(2)]
        yin = [AR.alloc("yin%d" % i, [128, 256], F32) for i in range(2)]
        ysg = [AR.alloc("ysg%d" % i, [128, 256], F32) for i in range(2)]
        for j in range(8):
            b = j % 2
            DS(lambda e, b=b, j=j: e.dma_start(out=ut3[b][:], in_=uT_d[j]), r=["uT_d"], w=["ut%d" % b])
            for t in range(8):
                pb = next_ps()
                q = t % 2
                for tau in range(t + 1):
                    T(lambda e, pb=pb, b=b, j=j, t=t, tau=tau: e.matmul(ps[pb][:, 0:256], BD[:, j, t - tau, :], ut3[b][:, tau::8],
                                                                       start=(tau == 0), stop=False),
                      r=["BD", "ut%d" % b], w=[PSK[pb]])
                for pr in range(4):
                    for ri in range(2):
                        kw = dict(tile_position=(0, 96)) if pr == 3 else {}
                        last = (pr == 3 and ri == 1)
                        T(lambda e, pb=pb, j=j, t=t, pr=pr, ri=ri, kw=kw, last=last: e.matmul(
                            ps[pb][32 * pr:32 * pr + 32, 1:256], HC[:, 4 * j + pr, t, ri, :], XS[:, ri, 0:255, 4 * j + pr],
                            start=False, stop=last, **kw),
                          r=["HC", "XS"], w=[PSK[pb]])
                V(lambda e, pb=pb, b=b, j=j, t=t, q=q: e.scalar_tensor_tensor(out=ytmp[q][:], in0=ut3[b][:, t::8], scalar=dT[:, j:j + 1],
                                                                              in1=ps[pb][:, 0:256], op0=ALU.mult, op1=ALU.add),
                  r=["ut%d" % b, "dT", PSK[pb]], w=["ytmp%d" % q])
                G(lambda e, q=q: e.tensor_tensor(yin[q][:], ytmp[q][:], ytmp[q][:], ALU.mult), r=["ytmp%d" % q], w=["yin%d" % q])
                G(lambda e, q=q: e.tensor_scalar(yin[q][:], yin[q][:], 0.044715, 1.0, ALU.mult, ALU.add), r=["yin%d" % q], w=["yin%d" % q])
                G(lambda e, q=q: e.tensor_tensor(yin[q][:], yin[q][:], ytmp[q][:], ALU.mult), r=["yin%d" % q, "ytmp%d" % q], w=["yin%d" % q])
                A(lambda e, q=q: e.activation(ysg[q][:], yin[q][:], AF.Sigmoid, scale=1.5957691216057308), r=["yin%d" % q], w=["ysg%d" % q])
                V(lambda e, q=q, j=j, t=t: e.tensor_tensor(zT[:, j, t::8], ytmp[q][:], ysg[q][:], ALU.mult),
                  r=["ytmp%d" % q, "ysg%d" % q], w=["zT"])
        if dbg == "zT":
            fin.append(DS(lambda e: e.dma_start(out=dbg_out("zT", [128, 8, L], BF16), in_=zT[:]), r=["zT"]))
            P.emit(fin)
            return nc, dbg_outs
        phase_barrier("p3a")
        AR.release(ms)

        wglu = AR.alloc("wglu", [128, 8, 1024], BF16)
        DG(lambda e: e.dma_start(out=wglu[:], in_=wglu_d.rearrange("(k p) n -> p k n", p=128)), w=["wglu"])
        oT = AR.alloc("oT", [128, 8, 512], BF16)
        sig = [AR.alloc("sig%d" % i, [128, 512], F32) for i in range(2)]
        sq16 = [AR.alloc("sq16_%d" % i, [128, 512], BF16) for i in range(2)]
        rstd = AR.alloc("rstd", [128, 512], F32)
        for tb in range(4):
            sl = slice(tb * 512, (tb + 1) * 512)
            pq = next_ps()
            for ft in range(8):
                pb = next_ps()
                while pb == pq:
                    pb = next_ps()
                q = ft % 2
                for k in range(8):
                    T(lambda e, pb=pb, k=k, ft=ft, sl=sl: e.matmul(ps[pb][:], wglu[:, k, ft * 128:(ft + 1) * 128], zT[:, k, sl],
                                                                  start=(k == 0), stop=(k == 7)), r=["wglu", "zT"], w=[PSK[pb]])
                A(lambda e, pb=pb, q=q: e.activation(sig[q][:], ps[pb][:], AF.Sigmoid), r=[PSK[pb]], w=["sig%d" % q])
                V(lambda e, q=q, ft=ft, sl=sl: e.tensor_tensor(sig[q][:], sig[q][:], zT[:, ft, sl], ALU.mult), r=["sig%d" % q, "zT"], w=["sig%d" % q])
                A(lambda e, q=q, ft=ft: e.copy(oT[:, ft, :], sig[q][:]), r=["sig%d" % q], w=["oT"])
                G(lambda e, q=q: e.tensor_tensor(sq16[q][:], sig[q][:], sig[q][:], ALU.mult), r=["sig%d" % q], w=["sq16_%d" % q])
                T(lambda e, pq=pq, q=q, ft=ft: e.matmul(ps[pq][:], ones16[:], sq16[q][:], start=(ft == 0), stop=(ft == 7)),
                  r=["ones16", "sq16_%d" % q], w=[PSK[pq]])
            A(lambda e, pq=pq: e.activation(rstd[:], ps[pq][:], AF.Sqrt, bias=EPS6, scale=1.0 / 1024), r=[PSK[pq], "colc"], w=["rstd"])
            V(lambda e: e.reciprocal(rstd[:], rstd[:]), r=["rstd"], w=["rstd"])
            for ft in range(8):
                V(lambda e, ft=ft, sl=sl: e.scalar_tensor_tensor(out=zT[:, ft, sl], in0=oT[:, ft, :], scalar=betaS[:, ft:ft + 1], in1=rstd[:],
                                                                 op0=ALU.mult, op1=ALU.mult), r=["oT", "betaS", "rstd"], w=["zT"])
        if dbg == "yssm":
            fin.append(DS(lambda e: e.dma_start(out=dbg_out("yssm", [128, 8, L], BF16), in_=zT[:]), r=["zT"]))
            P.emit(fin)
            return nc, dbg_outs
        phase_barrier("p3")
        AR.release(ms)
        m4 = AR.mark()
        gt1b = AR.alloc("gt1b", [128, D], F32)
        wo = [AR.alloc("wo%d" % i, [128, 16, 512], BF16) for i in range(2)]
        xt = [AR.alloc("xt%d" % i, [128, 512], F32) for i in range(3)]
        x1t = [AR.alloc("x1t%d" % i, [128, 512], F32) for i in range(3)]
        zrow = AR.alloc("zrow", [1, D], F32)
        zrow16 = AR.alloc("zrow16", [1, D], BF16)
        fillt = AR.alloc("fillt", [128, 64], F32)
        mod_bcast(gt1b, 2, "gt1b")
        V(lambda e: e.memset(zrow[:], 0.0), w=["zrow"])
        V(lambda e: e.memset(zrow16[:], 0.0), w=["zrow16"])
        V(lambda e: e.memset(fillt[:], 2048.0), w=["fillt"])
        DS(lambda e: e.dma_start(out=acc_d[L:L + 1, :], in_=zrow[:]), r=["zrow"], w=["acc_pad"])
        DS(lambda e: e.dma_start(out=h2_d[L:L + 1, :], in_=zrow16[:]), r=["zrow16"], w=["h2_pad"])
        DS(lambda e: e.dma_start(out=ti_d[L:L + 1, :], in_=zrow[0:1, 0:4]), r=["zrow"], w=["ti_pad"])
        DS(lambda e: e.dma_start(out=slot_d.rearrange("(p j) o -> p (j o)", p=128), in_=fillt[:]), r=["fillt"], w=["slot_d"])
        wout_v = wout_d.rearrange("(k p) n -> p k n", p=128)
        cnt4 = 0
        for cb in range(4):
            wb = cb % 2
            DG(lambda e, wb=wb, cb=cb: e.dma_start(out=wo[wb][:], in_=wout_v[:, :, cb * 512:(cb + 1) * 512]), w=["wo%d" % wb])
            for tt in range(16):
                pb = next_ps()
                q = cnt4 % 3
                cnt4 += 1
                for k in range(16):
                    src = zT if k < 8 else yretT
                    skey = "zT" if k < 8 else "yretT"
                    T(lambda e, pb=pb, k=k, tt=tt, wb=wb, src=src: e.matmul(ps[pb][:], src[:, k % 8, tt * 128:(tt + 1) * 128], wo[wb][:, k, :],
                                                                           start=(k == 0), stop=(k == 15)),
                      r=[skey, "wo%d" % wb], w=[PSK[pb]])
                DS(lambda e, q=q, tt=tt, cb=cb: e.dma_start(out=xt[q][:], in_=x_d[tt * 128:(tt + 1) * 128, cb * 512:(cb + 1) * 512]), w=["xt%d" % q])
                V(lambda e, pb=pb, q=q, cb=cb: e.tensor_tensor(x1t[q][:], ps[pb][:], gt1b[:, cb * 512:(cb + 1) * 512], ALU.mult),
                  r=[PSK[pb], "gt1b"], w=["x1t%d" % q])
                G(lambda e, q=q: e.tensor_tensor(x1t[q][:], x1t[q][:], xt[q][:], ALU.add), r=["x1t%d" % q, "xt%d" % q], w=["x1t%d" % q])
                DS(lambda e, q=q, tt=tt, cb=cb: e.dma_start(out=acc_d[tt * 128:(tt + 1) * 128, cb * 512:(cb + 1) * 512], in_=x1t[q][:]),
                   r=["x1t%d" % q], w=["acc%d" % tt])
        phase_barrier("p4")
        AR.release(base_mark)

        blke = AR.alloc("blke", [128, 64], F32)
        rtc = AR.alloc("rtc", [128, 112], F32)
        m5 = AR.mark()
        A2b = AR.alloc("A2b", [128, D], F32)
        B2b = AR.alloc("B2b", [128, D], F32)
        g2b = AR.alloc("g2b", [128, D], F32)
        wr = AR.alloc("wr", [128, 16, 36], F32)
        brb = AR.alloc("brb", [128, 36], F32)
        LG = AR.alloc("LG", [128, 16, 36], F32)
        ss5 = AR.alloc("ss", [128, 16], F32)
        rs5 = AR.alloc("rs", [128, 16], F32)
        junk5 = AR.alloc("junk", [128, D], F32)
        xb5 = [AR.alloc("xb%d" % i, [128, D], F32) for i in range(2)]
        h2f = AR.alloc("h2f", [128, D], F32)
        h2b = [AR.alloc("h2b%d" % i, [128, D], BF16) for i in range(2)]
        h2T = AR.alloc("h2T", [128, 16, 128], F32)
        mod_bcast(A2b, 4, "A2b")
        mod_bcast(B2b, 3, "B2b")
        DS(lambda e: e.dma_start(out=g2b[:], in_=g2_d.partition_broadcast(128)), w=["g2b"])
        DS(lambda e: e.dma_start(out=wr[:], in_=wr_d.rearrange("(k p) n -> p k n", p=128)), w=["wr"])
        DS(lambda e: e.dma_start(out=brb[:], in_=br_d.partition_broadcast(128)), w=["brb"])
        V(lambda e: e.scalar_tensor_tensor(out=A2b[:], in0=A2b[:], scalar=1.0, in1=g2b[:], op0=ALU.add, op1=ALU.mult),
          r=["A2b", "g2b"], w=["A2b"])
        V(lambda e: e.memset(ss5[:], 0.0), w=["ss"])
        for tt in range(16):
            b = tt % 2
            DS(lambda e, b=b, tt=tt: e.dma_start(out=xb5[b][:], in_=acc_d[tt * 128:(tt + 1) * 128, :]), w=["xb%d" % b])
            rms_rstd_g(junk5, ss5, rs5, xb5[b][:], "xb%d" % b, tt, EPS6, D)
            V(lambda e, b=b, tt=tt: e.scalar_tensor_tensor(out=h2f[:], in0=xb5[b][:], scalar=rs5[:, tt:tt + 1], in1=A2b[:],
                                                           op0=ALU.mult, op1=ALU.mult), r=["xb%d" % b, "rs", "A2b"], w=["h2f"])
            V(lambda e: e.tensor_tensor(h2f[:], h2f[:], B2b[:], ALU.add), r=["h2f", "B2b"], w=["h2f"])
            A(lambda e, b=b: e.copy(h2b[b][:], h2f[:]), r=["h2f"], w=["h2b%d" % b])
            DS(lambda e, b=b, tt=tt: e.dma_start(out=h2_d[tt * 128:(tt + 1) * 128, :], in_=h2b[b][:]), r=["h2b%d" % b], w=["h2_d"])
            for kg in range(4):
                pb = next_ps()
                for kk in range(4):
                    k = kg * 4 + kk
                    T(lambda e, pb=pb, kk=kk, k=k: e.matmul(ps[pb][:, kk * 128:(kk + 1) * 128], h2f[:, k * 128:(k + 1) * 128], ident32,
                                                          start=True, stop=True), r=["h2f", "cst"], w=[PSK[pb]])
                if kg % 2 == 0:
                    A(lambda e, pb=pb, kg=kg: e.copy(h2T[:, kg * 4:(kg + 1) * 4, :], ps[pb][:].rearrange("p (a c) -> p a c", a=4)),
                      r=[PSK[pb]], w=["h2T"])
                else:
                    V(lambda e, pb=pb, kg=kg: e.tensor_copy(h2T[:, kg * 4:(kg + 1) * 4, :], ps[pb][:].rearrange("p (a c) -> p a c", a=4)),
                      r=[PSK[pb]], w=["h2T"])
            pb = next_ps()
            for k in range(16):
                T(lambda e, pb=pb, k=k: e.matmul(ps[pb][:, 0:36], h2T[:, k, :], wr[:, k, :], start=(k == 0), stop=(k == 15)),
                  r=["h2T", "wr"], w=[PSK[pb]])
            V(lambda e, pb=pb, tt=tt: e.tensor_tensor(LG[:, tt, :], ps[pb][:, 0:36], brb[:], ALU.add), r=[PSK[pb], "brb"], w=["LG"])
        if dbg == "LG":
            return dump("LG", LG, [128, 16, 36], F32, ["LG"])

        DS(lambda e: e.dma_start(out=rtc[:], in_=rt_d), w=["rtc"])
        tokid = rtc[:, 0:16]
        blkthr = rtc[:, 16:80]
        iota32 = rtc[:, 80:112]
        rk = [0]

        def rt(shape, dt=F32):
            rk[0] += 1
            return AR.alloc("rt%d" % rk[0], shape, dt), "rt%d" % rk[0]

        gl = LG[:, :, 0:4]
        gmax, gmaxk = rt([128, 16]); ohg, ohgk = rt([128, 16, 4]); ge, gek = rt([128, 16, 4]); gw, gwk = rt([128, 16])
        b3 = lambda ap, n: ap.unsqueeze(2).to_broadcast([128, 16, n])
        V(lambda e: e.tensor_reduce(out=gmax[:], in_=gl, axis=AX.X, op=ALU.max), r=["LG"], w=[gmaxk])
        V(lambda e: e.tensor_tensor(ohg[:], gl, b3(gmax[:], 4), ALU.is_equal), r=["LG", gmaxk], w=[ohgk])
        V(lambda e: e.tensor_tensor(ge[:], gl, b3(gmax[:], 4), ALU.subtract), r=["LG", gmaxk], w=[gek])
        A(lambda e: e.activation(ge[:], ge[:], AF.Exp), r=[gek], w=[gek])
        V(lambda e: e.tensor_reduce(out=gw[:], in_=ge[:], axis=AX.X, op=ALU.add), r=[gek], w=[gwk])
        V(lambda e: e.reciprocal(gw[:], gw[:]), r=[gwk], w=[gwk])
        els, elsk = rt([128, 16, 8]); etmp, etk = rt([128, 16, 8])
        V(lambda e: e.tensor_tensor(els[:], LG[:, :, 4:12], b3(ohg[:, :, 0], 8), ALU.mult), r=["LG", ohgk], w=[elsk])
        for g in range(1, 4):
            V(lambda e, g=g: e.tensor_tensor(etmp[:], LG[:, :, 4 + 8 * g:12 + 8 * g], b3(ohg[:, :, g], 8), ALU.mult), r=["LG", ohgk], w=[etk])
            V(lambda e: e.tensor_tensor(els[:], els[:], etmp[:], ALU.add), r=[elsk, etk], w=[elsk])
        mx1, mx1k = rt([128, 16]); oh1, oh1k = rt([128, 16, 8]); el2, el2k = rt([128, 16, 8]); mx2, mx2k = rt([128, 16]); oh2, oh2k = rt([128, 16, 8])
        V(lambda e: e.tensor_reduce(out=mx1[:], in_=els[:], axis=AX.X, op=ALU.max), r=[elsk], w=[mx1k])
        V(lambda e: e.tensor_tensor(oh1[:], els[:], b3(mx1[:], 8), ALU.is_equal), r=[elsk, mx1k], w=[oh1k])
        V(lambda e: e.scalar_tensor_tensor(out=el2[:], in0=oh1[:], scalar=-1.0e30, in1=els[:], op0=ALU.mult, op1=ALU.add), r=[oh1k, elsk], w=[el2k])
        V(lambda e: e.tensor_reduce(out=mx2[:], in_=el2[:], axis=AX.X, op=ALU.max), r=[el2k], w=[mx2k])
        V(lambda e: e.tensor_tensor(oh2[:], el2[:], b3(mx2[:], 8), ALU.is_equal), r=[el2k, mx2k], w=[oh2k])
        ee, eek = rt([128, 16]); w1, w1k = rt([128, 16]); w2, w2k = rt([128, 16])
        V(lambda e: e.tensor_tensor(ee[:], mx2[:], mx1[:], ALU.subtract), r=[mx1k, mx2k], w=[eek])
        A(lambda e: e.activation(ee[:], ee[:], AF.Exp), r=[eek], w=[eek])
        V(lambda e: e.tensor_scalar(w1[:], ee[:], 1.0, None, ALU.add), r=[eek], w=[w1k])
        V(lambda e: e.reciprocal(w1[:], w1[:]), r=[w1k], w=[w1k])
        V(lambda e: e.tensor_tensor(w2[:], ee[:], w1[:], ALU.mult), r=[eek, w1k], w=[w2k])
        V(lambda e: e.tensor_tensor(w1[:], w1[:], gw[:], ALU.mult), r=[w1k, gwk], w=[w1k])
        V(lambda e: e.tensor_tensor(w2[:], w2[:], gw[:], ALU.mult), r=[w2k, gwk], w=[w2k])
        gsel, gselk = rt([128, 16]); j1, j1k = rt([128, 16]); j2, j2k = rt([128, 16]); itmp, itk = rt([128, 16, 8])
        ib = lambda n: iota32[:, 0:n].unsqueeze(1).to_broadcast([128, 16, n])
        V(lambda e: e.tensor_tensor(itmp[:, :, 0:4], ohg[:], ib(4), ALU.mult), r=[ohgk, "rtc"], w=[itk])
        V(lambda e: e.tensor_reduce(out=gsel[:], in_=itmp[:, :, 0:4], axis=AX.X, op=ALU.add), r=[itk], w=[gselk])
        V(lambda e: e.tensor_tensor(itmp[:], oh1[:], ib(8), ALU.mult), r=[oh1k, "rtc"], w=[itk])
        V(lambda e: e.tensor_reduce(out=j1[:], in_=itmp[:], axis=AX.X, op=ALU.add), r=[itk], w=[j1k])
        V(lambda e: e.tensor_tensor(itmp[:], oh2[:], ib(8), ALU.mult), r=[oh2k, "rtc"], w=[itk])
        V(lambda e: e.tensor_reduce(out=j2[:], in_=itmp[:], axis=AX.X, op=ALU.add), r=[itk], w=[j2k])
        TIt, tik = rt([128, 16, 4])
        V(lambda e: e.tensor_copy(TIt[:, :, 0], w1[:]), r=[w1k], w=[tik])
        V(lambda e: e.tensor_copy(TIt[:, :, 2], w2[:]), r=[w2k], w=[tik])
        V(lambda e: e.scalar_tensor_tensor(out=TIt[:, :, 1], in0=gsel[:], scalar=8.0, in1=j1[:], op0=ALU.mult, op1=ALU.add), r=[gselk, j1k], w=[tik])
        V(lambda e: e.scalar_tensor_tensor(out=TIt[:, :, 3], in0=gsel[:], scalar=8.0, in1=j2[:], op0=ALU.mult, op1=ALU.add), r=[gselk, j2k], w=[tik])
        DS(lambda e: e.dma_start(out=ti_d[0:L, :].rearrange("(t p) c -> p t c", p=128), in_=TIt[:]), r=[tik], w=["ti_d"])
        OH1, OH1k = rt([128, 16, 32]); OH2, OH2k = rt([128, 16, 32]); Mt, Mk = rt([128, 16, 32])
        i32b = iota32.unsqueeze(1).to_broadcast([128, 16, 32])
        V(lambda e: e.tensor_tensor(OH1[:], i32b, b3(TIt[:, :, 1], 32), ALU.is_equal), r=["rtc", tik], w=[OH1k])
        V(lambda e: e.tensor_tensor(OH2[:], i32b, b3(TIt[:, :, 3], 32), ALU.is_equal), r=["rtc", tik], w=[OH2k])
        V(lambda e: e.tensor_tensor(Mt[:], OH1[:], OH2[:], ALU.add), r=[OH1k, OH2k], w=[Mk])
        pc = next_ps()
        pr_ = next_ps()
        T(lambda e, pc=pc: e.matmul(ps[pc][:], ones32, Mt[:].rearrange("p a b -> p (a b)"), start=True, stop=True), r=["cst", Mk], w=[PSK[pc]])
        T(lambda e, pr_=pr_: e.matmul(ps[pr_][:], tri32, Mt[:].rearrange("p a b -> p (a b)"), start=True, stop=True), r=["cst", Mk], w=[PSK[pr_]])
        tot, totk = rt([128, 16, 32]); dest, destk = rt([128, 16, 32]); texc, texck = rt([128, 16, 32])
        A(lambda e, pc=pc: e.copy(tot[:].rearrange("p a b -> p (a b)"), ps[pc][:]), r=[PSK[pc]], w=[totk])
        V(lambda e, pr_=pr_: e.tensor_copy(dest[:].rearrange("p a b -> p (a b)"), ps[pr_][:]), r=[PSK[pr_]], w=[destk])
        V(lambda e: e.memset(texc[:, 0, :], 0.0), w=[texck])
        for tt in range(1, 16):
            V(lambda e, tt=tt: e.tensor_tensor(texc[:, tt, :], texc[:, tt - 1, :], tot[:, tt - 1, :], ALU.add), r=[texck, totk], w=[texck])
        cntt, cntk = rt([128, 32]); nbi, nbik = rt([128, 32], I32); padd, padk = rt([128, 32]); pend, pendk = rt([128, 32]); poff, poffk = rt([128, 32])
        onesr, onesk = rt([128, 32])
        V(lambda e: e.tensor_tensor(cntt[:], texc[:, 15, :], tot[:, 15, :], ALU.add), r=[texck, totk], w=[cntk])
        V(lambda e: e.tensor_scalar(cntt[:], cntt[:], 1.0 / 128, 0.49609375, ALU.mult, ALU.add), r=[cntk], w=[cntk])
        V(lambda e: e.tensor_copy(nbi[:], cntt[:]), r=[cntk], w=[nbik])
        V(lambda e: e.tensor_copy(padd[:], nbi[:]), r=[nbik], w=[padk])
        V(lambda e: e.tensor_scalar(padd[:], padd[:], 128.0, None, ALU.mult), r=[padk], w=[padk])
        V(lambda e: e.memset(onesr[:], 1.0), w=[onesk])
        V(lambda e: e.tensor_tensor_scan(pend[:], onesr[:], padd[:], 0.0, ALU.mult, ALU.add), r=[onesk, padk], w=[pendk])
        V(lambda e: e.tensor_tensor(poff[:], pend[:], padd[:], ALU.subtract), r=[pendk, padk], w=[poffk])
        V(lambda e: e.tensor_tensor(dest[:], dest[:], texc[:], ALU.add), r=[destk, texck], w=[destk])
        V(lambda e: e.tensor_tensor(dest[:], dest[:], poff[:, :].unsqueeze(1).to_broadcast([128, 16, 32]), ALU.add), r=[destk, poffk], w=[destk])
        d12, d12k = rt([128, 2, 16]); d12i, d12ik = rt([128, 2, 16], I32)
        V(lambda e: e.tensor_tensor(OH1[:], OH1[:], dest[:], ALU.mult), r=[OH1k, destk], w=[OH1k])
        V(lambda e: e.tensor_reduce(out=d12[:, 0, :], in_=OH1[:], axis=AX.X, op=ALU.add), r=[OH1k], w=[d12k])
        V(lambda e: e.tensor_tensor(OH2[:], OH2[:], dest[:], ALU.mult), r=[OH2k, destk], w=[OH2k])
        V(lambda e: e.tensor_reduce(out=d12[:, 1, :], in_=OH2[:], axis=AX.X, op=ALU.add), r=[OH2k], w=[d12k])
        V(lambda e: e.tensor_copy(d12i[:], d12[:]), r=[d12k], w=[d12ik])
        cmpt, cmpk = rt([128, 64, 32])
        V(lambda e: e.tensor_tensor(cmpt[:], pend[:, :].unsqueeze(1).to_broadcast([128, 64, 32]), blkthr.unsqueeze(2).to_broadcast([128, 64, 32]), ALU.is_le),
          r=[pendk, "rtc"], w=[cmpk])
        V(lambda e: e.tensor_reduce(out=blke[:], in_=cmpt[:], axis=AX.X, op=ALU.add), r=[cmpk], w=["blke"])
        V(lambda e: e.tensor_scalar(blke[:], blke[:], 31.0, None, ALU.min), r=["blke"], w=["blke"])
        for tt in range(16):
            for a_ in range(2):
                DG(lambda e, tt=tt, a_=a_: e.indirect_dma_start(out=slot_d, out_offset=bass.IndirectOffsetOnAxis(ap=d12i[:, a_, tt:tt + 1], axis=0),
                                                               in_=tokid[:, tt:tt + 1], in_offset=None),
                   r=[d12ik, "rtc", "slot_d"], w=["slot_d"])
        if dbg == "route":
            t_ = AR.alloc("dbgslot", [128, 64], F32)
            DS(lambda e: e.dma_start(out=t_[:], in_=slot_d.rearrange("(p j) o -> p (j o)", p=128)), r=["slot_d"], w=["dbgslot"])
            fin.append(DS(lambda e: e.dma_start(out=dbg_out("slot", [128, 64]), in_=t_[:]), r=["dbgslot"]))
            fin.append(DS(lambda e: e.dma_start(out=dbg_out("blke", [128, 64]), in_=blke[:]), r=["blke"]))
            fin.append(DS(lambda e: e.dma_start(out=dbg_out("TI", [128, 16, 4]), in_=TIt[:]), r=[tik]))
            return dump("pend", pend, [128, 32], F32, [pendk])

        phase_barrier("p6")
        AR.release(m5)
        gt2b = AR.alloc("gt2b", [128, D], F32)
        mod_bcast(gt2b, 5, "gt2b")
        wgS = AR.alloc("wgS", [128, 16, 1024], BF16)
        wuS = AR.alloc("wuS", [128, 16, 1024], BF16)
        wdS = AR.alloc("wdS", [128, 8, 2048], BF16)
        xs = [AR.alloc("xs%d" % i, [128, D], BF16) for i in range(2)]
        xsT = AR.alloc("xsT", [128, 16, 128], BF16)
        sg = AR.alloc("sg", [128, 1024], F32)
        a16 = AR.alloc("a16", [128, 1024], BF16)
        actT = AR.alloc("actT", [128, 8, 128], BF16)
        yb = AR.alloc("yb", [128, D], F32)
        stok = [AR.alloc("stok%d" % i, [128, 1], F32) for i in range(2)]
        idx = [AR.alloc("idx%d" % i, [128, 1], I32) for i in range(2)]
        tinf = [AR.alloc("tinf%d" % i, [128, 4], F32) for i in range(2)]
        wsl = [AR.alloc("wsl%d" % i, [128, 2], F32) for i in range(2)]
        offf = AR.alloc("offf", [128, 2], F32)
        offs16f = AR.alloc("offs16f", [128, 16], F32)
        offs8f = AR.alloc("offs8f", [128, 8], F32)
        offs16i = [AR.alloc("offs16i%d" % i, [128, 16], I32) for i in range(2)]
        offs8i = [AR.alloc("offs8i%d" % i, [128, 8], I32) for i in range(2)]
        NB = 64

        def moe_loads(b):
            q = b % 2
            DS(lambda e, q=q, b=b: e.dma_start(out=stok[q][:], in_=slot_d[b * 128:(b + 1) * 128, :]), r=["slot_d"], w=["stok%d" % q])
            V(lambda e, q=q: e.tensor_copy(idx[q][:], stok[q][:]), r=["stok%d" % q], w=["idx%d" % q])
            DG(lambda e, q=q: e.indirect_dma_start(out=tinf[q][:], out_offset=None, in_=ti_d,
                                                   in_offset=bass.IndirectOffsetOnAxis(ap=idx[q][:, :], axis=0)),
               r=["idx%d" % q, "ti_d", "ti_pad"], w=["tinf%d" % q])
            DG(lambda e, q=q: e.indirect_dma_start(out=xs[q][:], out_offset=None, in_=h2_d,
                                                   in_offset=bass.IndirectOffsetOnAxis(ap=idx[q][:, :], axis=0)),
               r=["idx%d" % q, "h2_d", "h2_pad"], w=["xs%d" % q])
            V(lambda e, b=b: e.scalar_tensor_tensor(out=offf[:, 0:1], in0=blke[:, b:b + 1], scalar=128.0, in1=PIDX, op0=ALU.mult, op1=ALU.add),
              r=["blke", "colc"], w=["offf"])
            V(lambda e: e.tensor_scalar(offf[:, 1:2], offf[:, 0:1], 8.0, None, ALU.mult), r=["offf"], w=["offf1"])
            V(lambda e: e.tensor_scalar(offf[:, 0:1], offf[:, 0:1], 16.0, None, ALU.mult), r=["offf", "offf1"], w=["offf"])
            V(lambda e: e.tensor_scalar(offs16f[:], rtc[:, 80:96], offf[:, 0:1], None, ALU.add), r=["rtc", "offf"], w=["offs16f"])
            V(lambda e: e.tensor_scalar(offs8f[:], rtc[:, 80:88], offf[:, 1:2], None, ALU.add), r=["rtc", "offf1"], w=["offs8f"])
            V(lambda e, q=q: e.tensor_copy(offs16i[q][:], offs16f[:]), r=["offs16f"], w=["offs16i%d" % q])
            V(lambda e, q=q: e.tensor_copy(offs8i[q][:], offs8f[:]), r=["offs8f"], w=["offs8i%d" % q])
            for kk in range(16):
                for wS, w_d, key in ((wgS, wg_d, "wgS"), (wuS, wu_d, "wuS")):
                    DG(lambda e, q=q, wS=wS, w_d=w_d, kk=kk: e.indirect_dma_start(
                        out=wS[:, kk, :], out_offset=None, in_=w_d, in_offset=bass.IndirectOffsetOnAxis(ap=offs16i[q][:, kk:kk + 1], axis=0)),
                       r=["offs16i%d" % q], w=["%s%d" % (key, kk)])
            for ff in range(8):
                DG(lambda e, q=q, ff=ff: e.indirect_dma_start(
                    out=wdS[:, ff, :], out_offset=None, in_=wd_d, in_offset=bass.IndirectOffsetOnAxis(ap=offs8i[q][:, ff:ff + 1], axis=0)),
                   r=["offs8i%d" % q], w=["wdS%d" % ff])

        moe_loads(0)
        for b in range(NB):
            q = b % 2
            V(lambda e, q=q, b=b: e.tensor_scalar(wsl[q][:, 0:1], tinf[q][:, 1:2], blke[:, b:b + 1], tinf[q][:, 0:1], ALU.is_equal, ALU.mult),
              r=["tinf%d" % q, "blke"], w=["wsl%d" % q])
            V(lambda e, q=q, b=b: e.tensor_scalar(wsl[q][:, 1:2], tinf[q][:, 3:4], blke[:, b:b + 1], tinf[q][:, 2:3], ALU.is_equal, ALU.mult),
              r=["tinf%d" % q, "blke"], w=["wsl%d" % q])
            V(lambda e, q=q: e.tensor_tensor(wsl[q][:, 0:1], wsl[q][:, 0:1], wsl[q][:, 1:2], ALU.add), r=["wsl%d" % q], w=["wsl%d" % q])
            for kg in range(4):
                pb = 4 + kg % 2
                for kk in range(4):
                    k = kg * 4 + kk
                    T(lambda e, pb=pb, kk=kk, k=k, q=q: e.matmul(ps[pb][:, kk * 128:(kk + 1) * 128], xs[q][:, k::16], id16[:], start=True, stop=True),
                      r=["xs%d" % q, "id16"], w=[PSK[pb]])
                if kg % 2 == 0:
                    A(lambda e, pb=pb, kg=kg: e.copy(xsT[:, kg * 4:(kg + 1) * 4, :], ps[pb][:].rearrange("p (a c) -> p a c", a=4)), r=[PSK[pb]], w=["xsT"])
                else:
                    V(lambda e, pb=pb, kg=kg: e.tensor_copy(xsT[:, kg * 4:(kg + 1) * 4, :], ps[pb][:].rearrange("p (a c) -> p a c", a=4)), r=[PSK[pb]], w=["xsT"])
            for kk in range(16):
                for wi_, (wS, key) in enumerate(((wgS, "wgS"), (wuS, "wuS"))):
                    for hf_ in range(2):
                        pb = wi_ * 2 + hf_
                        T(lambda e, pb=pb, kk=kk, wS=wS, hf_=hf_: e.matmul(ps[pb][:], xsT[:, kk, :], wS[:, kk, hf_ * 512:(hf_ + 1) * 512],
                                                                          start=(kk == 0), stop=(kk == 15)),
                          r=["xsT", "%s%d" % (key, kk)], w=[PSK[pb]])
            for hf_ in range(2):
                A(lambda e, hf_=hf_: e.activation(sg[:, hf_ * 512:(hf_ + 1) * 512], ps[hf_][:], AF.Silu), r=[PSK[hf_]], w=["sg"])
                V(lambda e, hf_=hf_: e.tensor_tensor(a16[:, hf_ * 512:(hf_ + 1) * 512], sg[:, hf_ * 512:(hf_ + 1) * 512], ps[2 + hf_][:], ALU.mult),
                  r=["sg", PSK[2 + hf_]], w=["a16"])
            for fg in range(2):
                pb = 4 + fg
                for fi in range(4):
                    ff = fg * 4 + fi
                    T(lambda e, pb=pb, fi=fi, ff=ff: e.matmul(ps[pb][:, fi * 128:(fi + 1) * 128], a16[:, ff::8], id16[:], start=True, stop=True),
                      r=["a16", "id16"], w=[PSK[pb]])
                if fg == 0:
                    A(lambda e, pb=pb, fg=fg: e.copy(actT[:, fg * 4:(fg + 1) * 4, :], ps[pb][:].rearrange("p (a c) -> p a c", a=4)), r=[PSK[pb]], w=["actT"])
                else:
                    V(lambda e, pb=pb, fg=fg: e.tensor_copy(actT[:, fg * 4:(fg + 1) * 4, :], ps[pb][:].rearrange("p (a c) -> p a c", a=4)), r=[PSK[pb]], w=["actT"])
            for db in range(4):
                pb = 6 + db % 2
                for ff in range(8):
                    T(lambda e, pb=pb, ff=ff, db=db: e.matmul(ps[pb][:], actT[:, ff, :], wdS[:, ff, db * 512:(db + 1) * 512],
                                                            start=(ff == 0), stop=(ff == 7)), r=["actT", "wdS%d" % ff], w=[PSK[pb]])
                V(lambda e, pb=pb, db=db, q=q: e.scalar_tensor_tensor(out=yb[:, db * 512:(db + 1) * 512], in0=ps[pb][:], scalar=wsl[q][:, 0:1],
                                                                     in1=gt2b[:, db * 512:(db + 1) * 512], op0=ALU.mult, op1=ALU.mult),
                  r=[PSK[pb], "wsl%d" % q, "gt2b"], w=["yb"])
            if b + 1 < NB:
                moe_loads(b + 1)
            DG(lambda e, q=q: e.indirect_dma_start(out=acc_d, out_offset=bass.IndirectOffsetOnAxis(ap=idx[q][:, :], axis=0), in_=yb[:],
                                                   in_offset=None, compute_op=ALU.add),
               r=["yb", "idx%d" % q, "acc_pad"] + ["acc%d" % i for i in range(16)], w=["accs"])
        phase_barrier("p7")
        AR.release(base_mark)

        gfb = AR.alloc("gfb", [128, D], F32)
        ss8v = AR.alloc("ss8", [128, 16], F32)
        rs8v = AR.alloc("rs8", [128, 16], F32)
        junk8v = AR.alloc("junk8", [128, D], F32)
        xb8v = [AR.alloc("xf%d" % i, [128, D], F32) for i in range(2)]
        ob = [AR.alloc("ob%d" % i, [128, D], F32) for i in range(2)]
        DS(lambda e: e.dma_start(out=gfb[:], in_=gf_d.partition_broadcast(128)), w=["gfb"])
        V(lambda e: e.memset(ss8v[:], 0.0), w=["ss"])
        for tt in range(16):
            b = tt % 2
            DS(lambda e, b=b, tt=tt: e.dma_start(out=xb8v[b][:], in_=acc_d[tt * 128:(tt + 1) * 128, :]), w=["xf%d" % b])
            rms_rstd_g(junk8v, ss8v, rs8v, xb8v[b][:], "xf%d" % b, tt, EPS6, D)
            V(lambda e, b=b, tt=tt: e.scalar_tensor_tensor(out=ob[b][:], in0=xb8v[b][:], scalar=rs8v[:, tt:tt + 1], in1=gfb[:],
                                                           op0=ALU.mult, op1=ALU.mult), r=["xf%d" % b, "rs", "gfb"], w=["ob%d" % b])
            fin.append(DS(lambda e, b=b, tt=tt: e.dma_start(out=out_d[tt * 128:(tt + 1) * 128, :], in_=ob[b][:]), r=["ob%d" % b]))
        P.emit(fin)
        return nc, dbg_outs


def host_consts():
    c = {}
    I = np.eye(128, dtype=np.float32)
    perm = np.zeros((128, 128), np.float32)
    for i in range(128):
        perm[(i + 64) % 128, i] = 1.0
    onesdiv = np.full((128, 128), 1.0 / 128, np.float32)
    tri = np.triu(np.ones((128, 128), np.float32), k=1)
    ones = np.ones((128, 128), np.float32)
    c["cst32"] = np.ascontiguousarray(np.stack([I, perm, onesdiv, tri, ones], axis=1))
    half = 64
    inv_freq = 1.0 / (10000.0 ** (np.arange(half, dtype=np.float32) * 2.0 / 128))
    ang = np.arange(L, dtype=np.float32)[:, None] * inv_freq[None, :]
    cos = np.cos(ang).astype(np.float32).T
    sin = np.sin(ang).astype(np.float32).T
    cosT = np.concatenate([cos, cos], axis=0)
    sinT = np.concatenate([-sin, sin], axis=0)
    c["rope"] = np.ascontiguousarray(np.stack([cosT, sinT], axis=1).astype(np.float32))
    H = 8
    gamma = 1.0 - np.exp2(-5.0 - np.arange(H, dtype=np.float32))
    log_g = np.log(gamma).astype(np.float32)
    idx = np.arange(128, dtype=np.float32)
    rel = idx[:, None] - idx[None, :]
    decay = np.where(rel >= 0, np.exp(log_g[:, None, None] * np.maximum(rel, 0.0)), 0.0)
    sc = 128.0 ** -0.5
    decT = (decay.transpose(2, 0, 1) * sc).astype(np.float32)
    c["decayT"] = np.ascontiguousarray(decT)
    zeta = np.exp(log_g[:, None] * (127.0 - idx)[None, :]).astype(np.float32)
    xi = np.exp(log_g[None, :] * (idx + 1.0)[:, None]).astype(np.float32)
    xiT = np.broadcast_to(xi.T[None, :, :], (128, 8, 128)).astype(np.float32)
    c["xiT"] = np.ascontiguousarray(xiT)
    col = np.zeros((128, 24), np.float32)
    p = np.arange(128)
    col[:, 0] = np.where(p < 64, -1.0, 1.0)
    col[:, 1] = np.where(p < 64, 1.0, -1.0)
    col[:, 2] = (p // 16) % 2
    col[:, 3] = 1.0 - col[:, 2]
    col[:, 4] = 1e-6
    col[:, 5] = 1e-5
    col[:, 6] = p
    col[:, 7] = math.pi / 2
    col[:, 8:16] = (zeta.T * sc)
    col[:, 16] = (p < 64)
    col[:, 17] = (p >= 64)
    col[:, 18] = -col[:, 16]
    col[:, 19] = -col[:, 17]
    c["colc"] = col
    q = np.arange(128)[:, None]
    cc = np.arange(32)[None, :]
    m32 = (((q % 32) // 16) == (cc // 16)).astype(np.float32)
    m4 = np.zeros((128, 4, 32), np.float32)
    for pr in range(4):
        m4[32 * pr:32 * pr + 32, pr, :] = m32[32 * pr:32 * pr + 32]
    c["mask4"] = m4
    rt = np.zeros((128, 16 + 64 + 32), np.float32)
    rt[:, 0:16] = np.arange(16)[None, :] * 128 + p[:, None]
    rt[:, 16:80] = (np.arange(64) * 128)[None, :]
    rt[:, 80:112] = np.arange(32)[None, :]
    c["rtc"] = rt
    return c


def host_layout(inp):
    f = lambda a: np.ascontiguousarray(np.asarray(a, dtype=np.float32))
    o = {}
    o["w_ada"] = f(inp["w_ada"][0])
    o["b_ada"] = f(inp["b_ada"][0][None, :])
    o["g_norm1"] = f(inp["g_norm1"][0][None, :])
    o["g_norm2"] = f(inp["g_norm2"][0][None, :])
    o["g_final"] = f(inp["g_final"][None, :])
    w_in = np.asarray(inp["w_in"][0])
    order = list(range(8))
    for s in range(4):
        for h in range(8):
            order.append(8 + s * 8 + h)
    wt = w_in.reshape(16, 128, 40, 128).transpose(2, 1, 0, 3)
    o["w_in_t"] = f(wt)
    o["w_glu"] = f(inp["w_glu"][0])
    o["w_out"] = f(inp["w_out"][0])
    beta = np.concatenate([np.asarray(inp["beta_ssm"][0]), np.asarray(inp["beta_ret"][0])])
    o["betaT"] = f(beta.reshape(16, 128).T)
    o["dT"] = f(np.asarray(inp["ssm_d"][0]).reshape(8, 128).T)
    o["wr"] = f(np.concatenate([np.asarray(inp["w_router_group"][0]), np.asarray(inp["w_router_expert"][0])], axis=1))
    o["br"] = f(np.concatenate([np.asarray(inp["b_router_group"][0]), np.asarray(inp["b_router_expert"][0])])[None, :])
    o["w_gate"] = f(inp["w_gate"][0]).reshape(32 * 2048, 1024)
    o["w_up"] = f(inp["w_up"][0]).reshape(32 * 2048, 1024)
    o["w_down"] = f(inp["w_down"][0]).reshape(32 * 1024, 2048)
    a_re = np.asarray(inp["ssm_a_re"][0]); a_im = np.asarray(inp["ssm_a_im"][0])
    ldt = np.asarray(inp["ssm_log_dt"][0])
    B_re = np.asarray(inp["ssm_b_re"][0]); B_im = np.asarray(inp["ssm_b_im"][0])
    C_re = np.asarray(inp["ssm_c_re"][0]); C_im = np.asarray(inp["ssm_c_im"][0])
    par = np.stack([a_re.T, a_im.T, np.broadcast_to(ldt[None, :], (64, 64))], axis=1)
    o["sl_par"] = f(np.concatenate([par, par], axis=0))
    Bre_p = B_re.transpose(1, 0, 2); Bim_p = B_im.transpose(1, 0, 2)
    V1 = np.concatenate([Bre_p, Bim_p], axis=0); V2 = np.concatenate([Bim_p, Bre_p], axis=0)
    o["sl_V"] = f(np.stack([V1, V2], axis=1))
    o["sl_C"] = f(np.concatenate([C_re.transpose(2, 0, 1), C_im.transpose(2, 0, 1)], axis=0))
    def pl(arr_gp):
        return arr_gp.reshape(32, 2, 64).transpose(1, 2, 0).reshape(128, 32)
    ldt_gp = np.broadcast_to(ldt[:, None], (64, 64))
    o["pl_par"] = f(np.stack([pl(a_re), pl(a_im), pl(ldt_gp)], axis=1))
    def plC(c_ghp):
        return c_ghp.reshape(32, 2, 16, 64).transpose(1, 3, 0, 2).reshape(128, 32, 16)
    o["pl_C"] = f(np.stack([plC(C_re), plC(C_im)], axis=1))
    def tl_gp(arr_gp):
        a = arr_gp.reshape(8, 8, 64)
        a = np.broadcast_to(a[:, :, None, :], (8, 8, 16, 64)).transpose(1, 2, 0, 3).reshape(128, 8 * 64)
        return a
    o["tl_par"] = f(np.stack([tl_gp(a_re), tl_gp(a_im), tl_gp(ldt_gp)], axis=1))
    def tl_B(b_gph):
        a = b_gph.reshape(8, 8, 64, 16).transpose(1, 3, 0, 2).reshape(128, 8 * 64)
        return a
    o["tl_B"] = f(np.stack([tl_B(B_re), tl_B(B_im)], axis=1))
    return o


_CACHE = {}


def kernel(**inputs):
    x = np.asarray(inputs["x"], dtype=np.float32)
    c = np.asarray(inputs["c"], dtype=np.float32)
    shared = host_layout(inputs)
    shared.update(host_consts())
    if "nc" not in _CACHE:
        _CACHE["nc"] = build_nc()[0]
    nc = _CACHE["nc"]
    in_maps = []
    for b in range(NCORES):
        m = dict(shared)
        m["x"] = np.ascontiguousarray(x[b])
        m["cT"] = np.ascontiguousarray(c[b].reshape(16, 128).T)
        in_maps.append(m)
    res = run_bass_kernel_spmd(nc, in_maps, core_ids=list(range(NCORES)))
    return np.stack([np.asarray(r["out"], dtype=np.float32) for r in res.results], axis=0)
```

```python
import math
import os
from contextlib import ExitStack

import numpy as np
import concourse.bass as bass
import concourse.mybir as mybir
from concourse.bass_utils import run_bass_kernel_spmd

F32 = mybir.dt.float32
BF16 = mybir.dt.bfloat16
I32 = mybir.dt.int32
ALU = mybir.AluOpType
AF = mybir.ActivationFunctionType
AX = mybir.AxisListType

D = 2048
L = 2048
NCORES = 8
COMPUTE = ("pe", "act", "dve", "pool")
NDMA_SEMS = 28
SB_BASE = 16640
SB_LIMIT = 229376


class Prog:
    def __init__(self, nc):
        self.nc = nc
        self.ops = []

    def op(self, eng, fn, reads=(), writes=()):
        self.ops.append(dict(kind="c", eng=eng, fn=fn, reads=tuple(reads), writes=tuple(writes), bar=False))
        return len(self.ops) - 1

    def dma(self, q, fn, reads=(), writes=()):
        self.ops.append(dict(kind="d", eng=q, fn=fn, reads=tuple(reads), writes=tuple(writes), bar=False))
        return len(self.ops) - 1

    def barrier(self, fn):
        self.ops.append(dict(kind="c", eng="dve", fn=fn, reads=(), writes=(), bar=True))
        return len(self.ops) - 1

    def _analyze(self, final):
        ops = self.ops
        last_w, readers = {}, {}
        last_on = {}
        dmas_since = []
        pending_bar = {}
        for i, o in enumerate(ops):
            deps = set()
            raw = set()
            if o["bar"]:
                for e, j in last_on.items():
                    deps.add(j)
                deps.update(dmas_since)
                dmas_since = []
                for e in ("pe", "act", "dve", "pool", "sp"):
                    pending_bar[e] = i
                pending_bar.pop("dve", None)
                last_w, readers = {}, {}
            else:
                for b in o["reads"]:
                    if b in last_w:
                        deps.add(last_w[b])
                        raw.add(last_w[b])
                for b in o["writes"]:
                    if b in last_w:
                        deps.add(last_w[b])
                    deps.update(readers.get(b, ()))
                if o["eng"] in pending_bar:
                    deps.add(pending_bar.pop(o["eng"]))
            deps.discard(i)
            raw.discard(i)
            o["deps"] = deps
            o["raw"] = raw
            for b in o["reads"]:
                readers.setdefault(b, []).append(i)
            for b in o["writes"]:
                last_w[b] = i
                readers[b] = []
            if o["kind"] == "c":
                last_on[o["eng"]] = i
            else:
                dmas_since.append(i)
        for o in ops:
            o["signal"] = False
        for d in final:
            ops[d]["signal"] = True
        for i, o in enumerate(ops):
            for d in o["deps"]:
                p = ops[d]
                if p["kind"] == "c" and (p["eng"] != o["eng"] or o["kind"] == "d"
                                         or (d in o["raw"] and p["eng"] != "pe")):
                    p["signal"] = True
        cnt = {e: 0 for e in COMPUTE}
        dcnt = [0] * NDMA_SEMS
        nd = 0
        for o in ops:
            if o["kind"] == "c":
                if o["signal"]:
                    cnt[o["eng"]] += 1
                    o["sigval"] = cnt[o["eng"]]
            else:
                s = nd % NDMA_SEMS
                nd += 1
                o["dsem"] = s
                o["dprev"] = dcnt[s] * 16
                dcnt[s] += 1
                o["sigval"] = dcnt[s] * 16

    def emit(self, final):
        nc = self.nc
        self._analyze(final)
        ops = self.ops
        with ExitStack() as es:
            esem = {e: es.enter_context(nc.semaphore("s_" + e)) for e in COMPUTE}
            dsem = [es.enter_context(nc.semaphore("d_%d" % i)) for i in range(NDMA_SEMS)]
            block = es.enter_context(nc.Block())

            def run(engname, engobj):
                seen = {}

                def wait(key, sem, val):
                    if seen.get(key, 0) >= val:
                        return
                    seen[key] = val
                    engobj.wait_ge(sem, val)

                def wait_on(p):
                    if p["kind"] == "c":
                        wait(("c", p["eng"]), esem[p["eng"]], p["sigval"])
                    else:
                        wait(("d", p["dsem"]), dsem[p["dsem"]], p["sigval"])

                for i, o in enumerate(ops):
                    if o["eng"] != engname:
                        continue
                    for d in sorted(o["deps"]):
                        p = ops[d]
                        if p["kind"] == "c" and p["eng"] == engname and o["kind"] == "c":
                            if engname == "pe" or d not in o["raw"]:
                                continue
                        wait_on(p)
                    if o["kind"] == "d":
                        if o["dprev"] > 0:
                            wait(("d", o["dsem"]), dsem[o["dsem"]], o["dprev"])
                        o["fn"](engobj).then_inc(dsem[o["dsem"]], 16)
                    else:
                        ins = o["fn"](engobj)
                        if o["signal"]:
                            ins.then_inc(esem[engname], 1)
                if engname == "sp":
                    for d in final:
                        wait_on(ops[d])

            block.sync(lambda e: run("sp", e))
            block.scalar(lambda e: run("act", e))
            block.vector(lambda e: run("dve", e))
            block.gpsimd(lambda e: run("pool", e))
            block.tensor(lambda e: run("pe", e))


class Arena:
    def __init__(self, nc):
        self.nc = nc
        self.off = SB_BASE
        self.n = 0

    def alloc(self, name, shape, dt):
        nb = {F32: 4, BF16: 2, I32: 4}[dt]
        per = nb
        for s in shape[1:]:
            per *= s
        per = (per + 31) // 32 * 32
        assert self.off + per <= SB_LIMIT, (name, self.off, per)
        self.n += 1
        t = self.nc.alloc_sbuf_tensor_at("%s_%d" % (name, self.n), list(shape), dt, offset=self.off)
        self.off += per
        return t

    def mark(self):
        return self.off

    def release(self, m):
        self.off = m


def build_nc(dbg=None):
    nc = bass.Bass("TRN2", target_bir_lowering=False)
    P = Prog(nc)
    AR = Arena(nc)
    es = ExitStack()
    fin = []

    def din(name, shape, dt=F32):
        return nc.dram_tensor(name, list(shape), dt, kind="ExternalInput").ap()

    def dscr(name, shape, dt=F32):
        return nc.dram_tensor(name, list(shape), dt, kind="Internal").ap()

    def dout(name, shape, dt=F32):
        return nc.dram_tensor(name, list(shape), dt, kind="ExternalOutput").ap()

    x_d = din("x", [L, D])
    cT_d = din("cT", [128, 16])
    wada_d = din("w_ada", [D, 6 * D])
    bada_d = din("b_ada", [1, 6 * D])
    g1_d = din("g_norm1", [1, D])
    g2_d = din("g_norm2", [1, D])
    gf_d = din("g_final", [1, D])
    win_d = din("w_in_t", [40, 128, 16, 128])
    wglu_d = din("w_glu", [1024, 1024])
    wout_d = din("w_out", [D, D])
    betaT_d = din("betaT", [128, 16])
    dT_d = din("dT", [128, 8])
    wr_d = din("wr", [D, 36])
    br_d = din("br", [1, 36])
    NE = 32 if dbg is None or dbg in ("moe", "final") else 1
    wg_d = din("w_gate", [NE * 1024, 2048])
    wu_d = din("w_up", [NE * 1024, 2048])
    wd_d = din("w_down", [NE * 1024, 2048])
    slpar_d = din("sl_par", [128, 3, 64])
    slV_d = din("sl_V", [128, 2, 64, 16])
    slC_d = din("sl_C", [128, 64, 16])
    plpar_d = din("pl_par", [128, 3, 32])
    plC_d = din("pl_C", [128, 2, 32, 16])
    tlpar_d = din("tl_par", [128, 3, 512])
    tlB_d = din("tl_B", [128, 2, 512])
    cst_d = din("cst32", [128, 5, 128])
    rope_d = din("rope", [128, 2, L])
    decay_d = din("decayT", [128, 8, 128])
    xi_d = din("xiT", [128, 8, 128])
    col_d = din("colc", [128, 24])
    mask4_d = din("mask4", [128, 4, 32])
    rt_d = din("rtc", [128, 16 + 64 + 32])

    out_d = dout("out", [L, D])
    mod_d = dscr("mod_d", [1, 6 * D])
    uT_d = dscr("uT_d", [8, 128, L], BF16)
    acc_d = dscr("acc_d", [L + 1, D])
    h2_d = dscr("h2_d", [L + 1, D], BF16)
    slot_d = dscr("slot_d", [8192, 1])
    ti_d = dscr("ti_d", [L + 1, 4])

    dbg_outs = {}

    def dbg_out(name, shape, dt=F32):
        dbg_outs[name] = dout("dbg_" + name, shape, dt)
        return dbg_outs[name]

    with es:
        ps = [es.enter_context(nc.psum_tensor("ps%d" % i, [128, 512], F32)) for i in range(8)]
        PSK = ["ps%d" % i for i in range(8)]

        def V(fn, r=(), w=()):
            return P.op("dve", fn, r, w)

        def A(fn, r=(), w=()):
            return P.op("act", fn, r, w)

        def G(fn, r=(), w=()):
            return P.op("pool", fn, r, w)

        def T(fn, r=(), w=()):
            return P.op("pe", fn, r, w)

        def DS(fn, r=(), w=()):
            return P.dma("sp", fn, r, w)

        def DG(fn, r=(), w=()):
            return P.dma("pool", fn, r, w)

        def phase_barrier(tag):
            sc = barc
            P.barrier(lambda e: e.memset(sc[:], 0.0))

        def dump(name, t, shape, dt, keys):
            fin.append(DS(lambda e: e.dma_start(out=dbg_out(name, shape, dt), in_=t[:]), r=keys))
            P.emit(fin)
            return nc, dbg_outs

        barc = AR.alloc("barc", [128, 8], F32)
        cst = AR.alloc("cst", [128, 5, 128], F32)
        colc = AR.alloc("colc", [128, 24], F32)
        id16 = AR.alloc("id16", [128, 128], BF16)
        ones16 = AR.alloc("ones16", [128, 128], BF16)
        DS(lambda e: e.dma_start(out=cst[:], in_=cst_d), w=["cst"])
        DS(lambda e: e.dma_start(out=colc[:], in_=col_d), w=["colc"])
        V(lambda e: e.tensor_copy(id16[:], cst[:, 0, :]), r=["cst"], w=["id16"])
        V(lambda e: e.tensor_copy(ones16[:], cst[:, 4, :]), r=["cst"], w=["ones16"])
        ident32 = cst[:, 0, :]
        perm32 = cst[:, 1, :]
        onesdiv32 = cst[:, 2, :]
        tri32 = cst[:, 3, :]
        ones32 = cst[:, 4, :]
        SGN, SGNC, PAR, NPAR, EPS6, EPS5, PIDX, HALFPI = [colc[:, i:i + 1] for i in range(8)]
        base_mark = AR.mark()

        m0 = AR.mark()
        cT = AR.alloc("cT", [128, 16], F32)
        scT = AR.alloc("scT", [128, 16], F32)
        bada = AR.alloc("bada", [1, 6 * D], F32)
        wa = [AR.alloc("wa%d" % i, [128, 16, 512], BF16) for i in range(4)]
        scT16 = AR.alloc("scT16", [128, 16], BF16)
        modrow = [AR.alloc("modrow%d" % i, [1, 512], F32) for i in range(2)]
        DS(lambda e: e.dma_start(out=cT[:], in_=cT_d), w=["cT"])
        DS(lambda e: e.dma_start(out=bada[:], in_=bada_d), w=["bada"])
        A(lambda e: e.activation(scT[:], cT[:], AF.Silu), r=["cT"], w=["scT"])
        V(lambda e: e.tensor_copy(scT16[:], scT[:]), r=["scT"], w=["scT16"])
        wada_v = wada_d.rearrange("(k p) n -> p k n", p=128)
        for nb in range(24):
            b = nb % 2
            wbi = nb % 4
            DG(lambda e, wbi=wbi, nb=nb: e.dma_start(out=wa[wbi][:], in_=wada_v[:, :, nb * 512:(nb + 1) * 512]), w=["wa%d" % wbi])
            for k in range(16):
                T(lambda e, b=b, k=k, wbi=wbi: e.matmul(ps[b][0:1, :], scT16[:, k:k + 1], wa[wbi][:, k, :], start=(k == 0), stop=(k == 15)),
                  r=["scT16", "wa%d" % wbi], w=[PSK[b]])
            V(lambda e, b=b, nb=nb: e.tensor_tensor(modrow[b][:], ps[b][0:1, :], bada[0:1, nb * 512:(nb + 1) * 512], ALU.add),
              r=[PSK[b], "bada"], w=["modrow%d" % b])
            DS(lambda e, b=b, nb=nb: e.dma_start(out=mod_d[0:1, nb * 512:(nb + 1) * 512], in_=modrow[b][:]),
               r=["modrow%d" % b], w=["mod_d"])
        if dbg == "mod":
            t = AR.alloc("dbgm", [1, 6 * D], F32)
            DS(lambda e: e.dma_start(out=t[:], in_=mod_d), r=["mod_d"], w=["dbgm"])
            fin.append(DS(lambda e: e.dma_start(out=dbg_out("mod", [1, 6 * D]), in_=t[:]), r=["dbgm"]))
            P.emit(fin)
            return nc, dbg_outs
        phase_barrier("p0")
        AR.release(m0)

        def mod_bcast(dst, idx, key):
            return DS(lambda e: e.dma_start(out=dst[:], in_=mod_d[0:1, idx * D:(idx + 1) * D].partition_broadcast(128)),
                      r=["mod_d"], w=[key])

        yretT = AR.alloc("yretT", [128, 8, L], BF16)
        mh = AR.mark()
        hT = AR.alloc("hT", [128, 16, L], BF16)
        m1 = AR.mark()
        A1b = AR.alloc("A1b", [128, D], F32)
        B1b = AR.alloc("B1b", [128, D], F32)
        g1b = AR.alloc("g1b", [128, D], F32)
        xb = [AR.alloc("xb%d" % i, [128, D], F32) for i in range(2)]
        junk = AR.alloc("junk", [128, D], F32)
        hf = AR.alloc("hf", [128, D], F32)
        h16 = [AR.alloc("h16_%d" % i, [128, D], BF16) for i in range(2)]
        ss = AR.alloc("ss", [128, 16], F32)
        rs = AR.alloc("rs", [128, 16], F32)
        mod_bcast(A1b, 1, "A1b")
        mod_bcast(B1b, 0, "B1b")
        DS(lambda e: e.dma_start(out=g1b[:], in_=g1_d.partition_broadcast(128)), w=["g1b"])
        V(lambda e: e.scalar_tensor_tensor(out=A1b[:], in0=A1b[:], scalar=1.0, in1=g1b[:], op0=ALU.add, op1=ALU.mult),
          r=["A1b", "g1b"], w=["A1b"])
        V(lambda e: e.memset(ss[:], 0.0), w=["ss"])

        def rms_rstd_g(junk, ss, rs, xt_ap, xkey, col, eps_ap, n):
            A(lambda e: e.activation(junk[:], xt_ap, AF.Square, accum_out=ss[:, col:col + 1]), r=[xkey, "ss"], w=["junk", "ss"])
            A(lambda e: e.activation(rs[:, col:col + 1], ss[:, col:col + 1], AF.Sqrt, bias=eps_ap, scale=1.0 / n),
              r=["ss", "colc"], w=["rs"])
            V(lambda e: e.reciprocal(rs[:, col:col + 1], rs[:, col:col + 1]), r=["rs"], w=["rs"])

        def rms_rstd(xt_ap, xkey, col, eps_ap, n):
            A(lambda e: e.activation(junk[:], xt_ap, AF.Square, accum_out=ss[:, col:col + 1]), r=[xkey, "ss"], w=["junk", "ss"])
            A(lambda e: e.activation(rs[:, col:col + 1], ss[:, col:col + 1], AF.Sqrt, bias=eps_ap, scale=1.0 / n),
              r=["ss", "colc"], w=["rs"])
            V(lambda e: e.reciprocal(rs[:, col:col + 1], rs[:, col:col + 1]), r=["rs"], w=["rs"])

        for tt in range(16):
            b = tt % 2
            DS(lambda e, b=b, tt=tt: e.dma_start(out=xb[b][:], in_=x_d[tt * 128:(tt + 1) * 128, :]), w=["xb%d" % b])
            rms_rstd_g(junk, ss, rs, xb[b][:], "xb%d" % b, tt, EPS6, D)
            V(lambda e, b=b, tt=tt: e.scalar_tensor_tensor(out=hf[:], in0=xb[b][:], scalar=rs[:, tt:tt + 1], in1=A1b[:],
                                                           op0=ALU.mult, op1=ALU.mult), r=["xb%d" % b, "rs", "A1b"], w=["hf"])
            V(lambda e, b=b: e.tensor_tensor(h16[b][:], hf[:], B1b[:], ALU.add), r=["hf", "B1b"], w=["h16_%d" % b])
            for kg in range(4):
                pb = 2 + (tt * 4 + kg) % 4
                for kk in range(4):
                    k = kg * 4 + kk
                    T(lambda e, pb=pb, kk=kk, k=k, b=b: e.matmul(ps[pb][:, kk * 128:(kk + 1) * 128], h16[b][:, k * 128:(k + 1) * 128],
                                                                 id16[:], start=True, stop=True),
                      r=["h16_%d" % b, "id16"], w=[PSK[pb]])
                ev = A if kg % 2 == 0 else V
                if kg % 2 == 0:
                    A(lambda e, pb=pb, kg=kg, tt=tt: e.copy(hT[:, kg * 4:(kg + 1) * 4, tt * 128:(tt + 1) * 128],
                                                           ps[pb][:].rearrange("p (a c) -> p a c", a=4)),
                      r=[PSK[pb]], w=["hT%d" % tt])
                else:
                    V(lambda e, pb=pb, kg=kg, tt=tt: e.tensor_copy(hT[:, kg * 4:(kg + 1) * 4, tt * 128:(tt + 1) * 128],
                                                                  ps[pb][:].rearrange("p (a c) -> p a c", a=4)),
                      r=[PSK[pb]], w=["hT%d" % tt])
        if dbg == "hT":
            t = AR.alloc("dbgh", [128, 16, L], F32)
            V(lambda e: e.tensor_copy(t[:], hT[:]), r=["hT%d" % i for i in range(16)], w=["dbgh"])
            fin.append(DS(lambda e: e.dma_start(out=dbg_out("hT", [128, 16, L]), in_=t[:]), r=["dbgh"]))
            P.emit(fin)
            return nc, dbg_outs
        phase_barrier("p1")
        AR.release(m1)

        m2 = AR.mark()
        win = [AR.alloc("win%d" % i, [128, 16, 128], BF16) for i in range(6)]
        wctr = [0]

        def load_win(tile_idx):
            i = wctr[0] % 6
            wctr[0] += 1
            DG(lambda e, i=i, tile_idx=tile_idx: e.dma_start(out=win[i][:], in_=win_d[tile_idx]), w=["win%d" % i])
            return i

        HTK = ["hT%d" % i for i in range(16)]
        pctr = [0]

        def next_ps():
            pctr[0] += 1
            return pctr[0] % 8

        def inproj_block(wi, tb, pb):
            for k in range(16):
                T(lambda e, wi=wi, tb=tb, pb=pb, k=k: e.matmul(ps[pb][:], win[wi][:, k, :], hT[:, k, tb * 512:(tb + 1) * 512],
                                                               start=(k == 0), stop=(k == 15)),
                  r=["win%d" % wi] + HTK[tb * 4:tb * 4 + 4], w=[PSK[pb]])

        ut = [AR.alloc("ut%d" % i, [128, L], BF16) for i in range(2)]
        for j in range(8):
            wi = load_win(j)
            ub = j % 2
            for tb in range(4):
                pb = next_ps()
                inproj_block(wi, tb, pb)
                ev = A if tb % 2 == 0 else V
                if tb % 2 == 0:
                    A(lambda e, pb=pb, ub=ub, tb=tb: e.copy(ut[ub][:, tb * 512:(tb + 1) * 512], ps[pb][:]), r=[PSK[pb]], w=["ut%d" % ub])
                else:
                    V(lambda e, pb=pb, ub=ub, tb=tb: e.tensor_copy(ut[ub][:, tb * 512:(tb + 1) * 512], ps[pb][:]), r=[PSK[pb]], w=["ut%d" % ub])
            DS(lambda e, ub=ub, j=j: e.dma_start(out=uT_d[j], in_=ut[ub][:]), r=["ut%d" % ub], w=["uT_d"])
        if dbg == "uT":
            t = AR.alloc("dbgu", [128, 8, L], BF16)
            t2 = AR.alloc("dbgu2", [128, 8, L], F32)
            DS(lambda e: e.dma_start(out=t[:], in_=uT_d.rearrange("j p t -> p j t")), r=["uT_d"], w=["dbgu"])
            V(lambda e: e.tensor_copy(t2[:], t[:]), r=["dbgu"], w=["dbgu2"])
            fin.append(DS(lambda e: e.dma_start(out=dbg_out("uT", [128, 8, L]), in_=t2[:]), r=["dbgu2"]))
            P.emit(fin)
            return nc, dbg_outs

        rope = AR.alloc("rope", [128, 2, L], F32)
        decT = AR.alloc("decT", [128, 8, 128], F32)
        xiT = AR.alloc("xiT", [128, 8, 128], F32)
        betaT = AR.alloc("betaT", [128, 16], F32)
        DS(lambda e: e.dma_start(out=rope[:], in_=rope_d), w=["rope"])
        DS(lambda e: e.dma_start(out=decT[:], in_=decay_d), w=["decT"])
        DS(lambda e: e.dma_start(out=xiT[:], in_=xi_d), w=["xiT"])
        DS(lambda e: e.dma_start(out=betaT[:], in_=betaT_d), w=["betaT"])
        qr = AR.alloc("qr", [128, L], BF16)
        kr = AR.alloc("kr", [128, L], BF16)
        qxi = AR.alloc("qxi", [128, L], BF16)
        vtok = AR.alloc("vtok", [128, 16, 128], BF16)
        kz = AR.alloc("kz", [128, 16, 128], BF16)
        gs = AR.alloc("gs", [128, 512], F32)
        qf = [AR.alloc("qf%d" % i, [128, 512], F32) for i in range(2)]
        t1 = [AR.alloc("t1_%d" % i, [128, 512], F32) for i in range(2)]
        t2 = [AR.alloc("t2_%d" % i, [128, 512], F32) for i in range(2)]
        vT16 = AR.alloc("vT16", [128, 512], BF16)
        Sd = [AR.alloc("Sd%d" % i, [128, 128], BF16) for i in range(2)]
        Rf = AR.alloc("Rf", [128, 128], F32)
        Rb = [AR.alloc("Rb%d" % i, [128, 128], BF16) for i in range(2)]
        yh = AR.alloc("yh", [128, 512], F32)
        ysq = AR.alloc("ysq", [128, 512], F32)
        mean = AR.alloc("mean", [128, 512], F32)
        var = AR.alloc("var", [128, 512], F32)
        yc = AR.alloc("yc", [128, 512], F32)
        gC = [float((1.0 - 2.0 ** (-5.0 - h)) ** 128) for h in range(8)]

        for hh in range(8):
            wq, wk, wv, wgt = [load_win(8 + s * 8 + hh) for s in range(4)]
            V(lambda e: e.memset(Rf[:], 0.0), w=["Rf"])
            for tb in range(4):
                sl = slice(tb * 512, (tb + 1) * 512)
                for which, wi, dst, dkey in (("q", wq, qr, "qr"), ("k", wk, kr, "kr")):
                    b2 = 0 if which == "q" else 1
                    pb = next_ps()
                    inproj_block(wi, tb, pb)
                    A(lambda e, pb=pb, b2=b2: e.copy(qf[b2][:], ps[pb][:]), r=[PSK[pb]], w=["qf%d" % b2])
                    pb2 = next_ps()
                    T(lambda e, pb2=pb2, b2=b2: e.matmul(ps[pb2][:], perm32, qf[b2][:], start=True, stop=True),
                      r=["cst", "qf%d" % b2], w=[PSK[pb2]])
                    G(lambda e, b2=b2, sl=sl: e.tensor_tensor(t1[b2][:], qf[b2][:], rope[:, 0, sl], ALU.mult),
                      r=["qf%d" % b2, "rope"], w=["t1_%d" % b2])
                    V(lambda e, pb2=pb2, b2=b2, sl=sl: e.tensor_tensor(t2[b2][:], ps[pb2][:], rope[:, 1, sl], ALU.mult),
                      r=[PSK[pb2], "rope"], w=["t2_%d" % b2])
                    V(lambda e, b2=b2: e.tensor_tensor(t1[b2][:], t1[b2][:], t2[b2][:], ALU.add),
                      r=["t1_%d" % b2, "t2_%d" % b2], w=["t1_%d" % b2])
                    A(lambda e, b2=b2, dst=dst, sl=sl: e.copy(dst[:, sl], t1[b2][:]), r=["t1_%d" % b2], w=[dkey])
                    if which == "q":
                        V(lambda e, hh=hh, sl=sl: e.tensor_tensor(qxi[:, sl].rearrange("p (a c) -> p a c", a=4),
                                                                  t1[0][:].rearrange("p (a c) -> p a c", a=4),
                                                                  xiT[:, hh, :].unsqueeze(1).to_broadcast([128, 4, 128]), ALU.mult),
                          r=["t1_0", "xiT"], w=["qxi"])
                pb = next_ps()
                inproj_block(wv, tb, pb)
                V(lambda e, pb=pb: e.tensor_copy(vT16[:], ps[pb][:]), r=[PSK[pb]], w=["vT16"])
                pb = next_ps()
                for c in range(4):
                    T(lambda e, pb=pb, c=c: e.matmul(ps[pb][:, c * 128:(c + 1) * 128], vT16[:, c * 128:(c + 1) * 128], id16[:],
                                                     start=True, stop=True), r=["vT16", "id16"], w=[PSK[pb]])
                A(lambda e, pb=pb, tb=tb: e.copy(vtok[:, tb * 4:(tb + 1) * 4, :], ps[pb][:].rearrange("p (a c) -> p a c", a=4)),
                  r=[PSK[pb]], w=["vtok"])
                pb = next_ps()
                for c in range(4):
                    T(lambda e, pb=pb, c=c, tb=tb: e.matmul(ps[pb][:, c * 128:(c + 1) * 128], kr[:, tb * 512 + c * 128: tb * 512 + (c + 1) * 128],
                                                            id16[:], start=True, stop=True), r=["kr", "id16"], w=[PSK[pb]])
                V(lambda e, pb=pb, tb=tb, hh=hh: e.tensor_scalar(kz[:, tb * 4:(tb + 1) * 4, :], ps[pb][:].rearrange("p (a c) -> p a c", a=4),
                                                                 colc[:, 8 + hh:9 + hh], None, ALU.mult),
                  r=[PSK[pb], "colc"], w=["kz"])
                pb = next_ps()
                inproj_block(wgt, tb, pb)
                A(lambda e, pb=pb: e.activation(gs[:], ps[pb][:], AF.Silu), r=[PSK[pb]], w=["gs"])
                pbo = next_ps()
                for c in range(4):
                    n = tb * 4 + c
                    cs = slice(n * 128, (n + 1) * 128)
                    pS = next_ps()
                    while pS == pbo:
                        pS = next_ps()
                    T(lambda e, pS=pS, cs=cs: e.matmul(ps[pS][:, 0:128], kr[:, cs], qr[:, cs], start=True, stop=True),
                      r=["kr", "qr"], w=[PSK[pS]])
                    T(lambda e, pS=pS, n=n: e.matmul(ps[pS][:, 128:256], kz[:, n, :], vtok[:, n, :], start=True, stop=True),
                      r=["kz", "vtok"], w=[PSK[pS]])
                    sb_i = n % 2
                    V(lambda e, pS=pS, sb_i=sb_i, hh=hh: e.tensor_tensor(Sd[sb_i][:], ps[pS][:, 0:128], decT[:, hh, :], ALU.mult),
                      r=[PSK[pS], "decT"], w=["Sd%d" % sb_i])
                    T(lambda e, pbo=pbo, c=c, n=n, sb_i=sb_i: e.matmul(ps[pbo][:, c * 128:(c + 1) * 128], vtok[:, n, :], Sd[sb_i][:],
                                                                      start=True, stop=(n == 0)),
                      r=["vtok", "Sd%d" % sb_i], w=[PSK[pbo]])
                    if n > 0:
                        T(lambda e, pbo=pbo, c=c, cs=cs, n=n: e.matmul(ps[pbo][:, c * 128:(c + 1) * 128], Rb[n % 2][:], qxi[:, cs],
                                                                      start=False, stop=True),
                          r=["Rb%d" % (n % 2), "qxi"], w=[PSK[pbo]])
                    V(lambda e, pS=pS, hh=hh: e.scalar_tensor_tensor(out=Rf[:], in0=Rf[:], scalar=gC[hh], in1=ps[pS][:, 128:256],
                                                                     op0=ALU.mult, op1=ALU.add), r=["Rf", PSK[pS]], w=["Rf"])
                    A(lambda e, n=n: e.copy(Rb[(n + 1) % 2][:], Rf[:]), r=["Rf"], w=["Rb%d" % ((n + 1) % 2)])
                A(lambda e, pbo=pbo: e.copy(yh[:], ps[pbo][:]), r=[PSK[pbo]], w=["yh"])
                A(lambda e: e.activation(ysq[:], yh[:], AF.Square), r=["yh"], w=["ysq"])
                pm = next_ps()
                pq = next_ps()
                T(lambda e, pm=pm: e.matmul(ps[pm][:], onesdiv32, yh[:], start=True, stop=True), r=["cst", "yh"], w=[PSK[pm]])
                T(lambda e, pq=pq: e.matmul(ps[pq][:], onesdiv32, ysq[:], start=True, stop=True), r=["cst", "ysq"], w=[PSK[pq]])
                A(lambda e, pm=pm: e.copy(mean[:], ps[pm][:]), r=[PSK[pm]], w=["mean"])
                V(lambda e: e.tensor_tensor(var[:], mean[:], mean[:], ALU.mult), r=["mean"], w=["var"])
                V(lambda e, pq=pq: e.tensor_tensor(var[:], ps[pq][:], var[:], ALU.subtract), r=[PSK[pq], "var"], w=["var"])
                A(lambda e: e.activation(var[:], var[:], AF.Sqrt, bias=EPS5, scale=1.0), r=["var", "colc"], w=["var"])
                V(lambda e: e.reciprocal(var[:], var[:]), r=["var"], w=["var"])
                V(lambda e: e.tensor_tensor(yc[:], yh[:], mean[:], ALU.subtract), r=["yh", "mean"], w=["yc"])
                V(lambda e: e.tensor_tensor(yc[:], yc[:], var[:], ALU.mult), r=["yc", "var"], w=["yc"])
                V(lambda e, hh=hh, sl=sl: e.scalar_tensor_tensor(out=yretT[:, hh, sl], in0=yc[:], scalar=betaT[:, 8 + hh:9 + hh], in1=gs[:],
                                                                 op0=ALU.mult, op1=ALU.mult), r=["yc", "betaT", "gs"], w=["yretT"])
        if dbg == "yret":
            fin.append(DS(lambda e: e.dma_start(out=dbg_out("yret", [128, 8, L], BF16), in_=yretT[:]), r=["yretT"]))
            P.emit(fin)
            return nc, dbg_outs
        phase_barrier("p2")
        AR.release(mh)
        TWO_PI = 2.0 * math.pi
        TOP, BOT, NTOP, NBOT = [colc[:, i:i + 1] for i in range(16, 20)]
        zT = AR.alloc("zT", [128, 8, L], BF16)
        BD = AR.alloc("BD", [128, 8, 8, 128], BF16)
        HC = AR.alloc("HC", [128, 32, 8, 2, 32], BF16)
        ArB = AR.alloc("ArB", [128, 2, 32], F32)
        AiB = AR.alloc("AiB", [128, 2, 32], F32)
        mask4 = AR.alloc("mask4", [128, 4, 32], F32)
        dT = AR.alloc("dT", [128, 8], F32)
        betaS = AR.alloc("betaS", [128, 16], F32)
        DS(lambda e: e.dma_start(out=mask4[:], in_=mask4_d), w=["mask4"])
        DS(lambda e: e.dma_start(out=dT[:], in_=dT_d), w=["dT"])
        DS(lambda e: e.dma_start(out=betaS[:], in_=betaT_d), w=["betaS"])
        ms = AR.mark()
        uid = [0]

        def tmp(shape, dt=F32):
            uid[0] += 1
            return AR.alloc("tmp%d" % uid[0], shape, dt), "tmp%d" % uid[0]

        def disc(par, pk, N, ks):
            K = len(ks)
            dt_, dtk = tmp([128, N]); lr, lrk = tmp([128, N]); li, lik = tmp([128, N])
            A(lambda e: e.activation(dt_[:], par[:, 2, :], AF.Exp), r=[pk], w=[dtk])
            V(lambda e: e.tensor_tensor(lr[:], dt_[:], par[:, 0, :], ALU.mult), r=[dtk, pk], w=[lrk])
            V(lambda e: e.tensor_tensor(li[:], dt_[:], par[:, 1, :], ALU.mult), r=[dtk, pk], w=[lik])
            MAG, mk = tmp([128, K, N]); ANG, ak = tmp([128, K, N]); TF, tfk = tmp([128, K, N]); TI, tik = tmp([128, K, N], I32)
            SN, snk = tmp([128, K, N]); CSN, csk = tmp([128, K, N])
            for i, k in enumerate(ks):
                A(lambda e, i=i, k=k: e.activation(MAG[:, i, :], lr[:], AF.Exp, scale=float(k)), r=[lrk], w=[mk])
                V(lambda e, i=i, k=k: e.tensor_scalar(ANG[:, i, :], li[:], float(k), None, ALU.mult), r=[lik], w=[ak])
            for shift, OUT, ok in ((0.0, SN, snk), (math.pi / 2, CSN, csk)):
                V(lambda e, shift=shift: e.tensor_scalar(TF[:], ANG[:], shift, 1.0 / TWO_PI, ALU.add, ALU.mult), r=[ak], w=[tfk])
                V(lambda e: e.tensor_copy(TI[:], TF[:]), r=[tfk], w=[tik])
                V(lambda e: e.tensor_copy(TF[:], TI[:]), r=[tik], w=[tfk])
                V(lambda e: e.scalar_tensor_tensor(out=TF[:], in0=TF[:], scalar=-TWO_PI, in1=ANG[:], op0=ALU.mult, op1=ALU.add),
                  r=[tfk, ak], w=[tfk])
                V(lambda e, shift=shift: e.tensor_scalar(TF[:], TF[:], shift, 3.1415925, ALU.add, ALU.min), r=[tfk], w=[tfk])
                V(lambda e: e.tensor_scalar(TF[:], TF[:], -3.1415925, None, ALU.max), r=[tfk], w=[tfk])
                A(lambda e, OUT=OUT: e.activation(OUT[:], TF[:], AF.Sin), r=[tfk], w=[ok])
            V(lambda e: e.tensor_tensor(CSN[:], CSN[:], MAG[:], ALU.mult), r=[csk, mk], w=[csk])
            V(lambda e: e.tensor_tensor(SN[:], SN[:], MAG[:], ALU.mult), r=[snk, mk], w=[snk])
            return (CSN, csk), (SN, snk)

        def zoh(par, pk, N, E1re, E1im, ek):
            nre, nk = tmp([128, N]); inv, ik = tmp([128, N]); fre, frk = tmp([128, N]); fim, fik = tmp([128, N]); tq, tqk = tmp([128, N])
            V(lambda e: e.tensor_scalar(nre[:], E1re, -1.0, None, ALU.add), r=ek, w=[nk])
            V(lambda e: e.tensor_tensor(inv[:], par[:, 0, :], par[:, 0, :], ALU.mult), r=[pk], w=[ik])
            V(lambda e: e.tensor_tensor(tq[:], par[:, 1, :], par[:, 1, :], ALU.mult), r=[pk], w=[tqk])
            V(lambda e: e.tensor_tensor(inv[:], inv[:], tq[:], ALU.add), r=[ik, tqk], w=[ik])
            V(lambda e: e.reciprocal(inv[:], inv[:]), r=[ik], w=[ik])
            V(lambda e: e.tensor_tensor(fre[:], nre[:], par[:, 0, :], ALU.mult), r=[nk, pk], w=[frk])
            V(lambda e: e.tensor_tensor(tq[:], E1im, par[:, 1, :], ALU.mult), r=ek + [pk, tqk], w=[tqk])
            V(lambda e: e.tensor_tensor(fre[:], fre[:], tq[:], ALU.add), r=[frk, tqk], w=[frk])
            V(lambda e: e.tensor_tensor(fre[:], fre[:], inv[:], ALU.mult), r=[frk, ik], w=[frk])
            V(lambda e: e.tensor_tensor(fim[:], E1im, par[:, 0, :], ALU.mult), r=ek + [pk], w=[fik])
            V(lambda e: e.tensor_tensor(tq[:], nre[:], par[:, 1, :], ALU.mult), r=[nk, pk, tqk], w=[tqk])
            V(lambda e: e.tensor_tensor(fim[:], fim[:], tq[:], ALU.subtract), r=[fik, tqk], w=[fik])
            V(lambda e: e.tensor_tensor(fim[:], fim[:], inv[:], ALU.mult), r=[fik, ik], w=[fik])
            return (fre, frk), (fim, fik)

        def _sl():
            slpar = AR.alloc("slpar", [128, 3, 64], F32)
            slV = AR.alloc("slV", [128, 2, 64, 16], F32)
            slC = AR.alloc("slC", [128, 64, 16], F32)
            DS(lambda e: e.dma_start(out=slpar[:], in_=slpar_d), w=["slpar"])
            DS(lambda e: e.dma_start(out=slV[:], in_=slV_d), w=["slV"])
            DS(lambda e: e.dma_start(out=slC[:], in_=slC_d), w=["slC"])
            (Ere, erk), (Eim, eik) = disc(slpar, "slpar", 64, list(range(8)))
            (fre, frk), (fim, fik) = zoh(slpar, "slpar", 64, Ere[:, 1, :], Eim[:, 1, :], [erk, eik])
            U1, u1k = tmp([128, 64, 16]); U2, u2k = tmp([128, 64, 16]); tA, tAk = tmp([128, 64, 16]); tB, tBk = tmp([128, 64, 16])
            PSb, psbk = tmp([128, 8, 64, 16], BF16); CSb, csbk = tmp([128, 64, 16], BF16)
            bc = lambda ap: ap.unsqueeze(2).to_broadcast([128, 64, 16])
            V(lambda e: e.tensor_tensor(tA[:], slV[:, 0], bc(fre[:]), ALU.mult), r=["slV", frk], w=[tAk])
            V(lambda e: e.tensor_tensor(tB[:], slV[:, 1], bc(fim[:]), ALU.mult), r=["slV", fik], w=[tBk])
            V(lambda e: e.scalar_tensor_tensor(out=U1[:], in0=tB[:], scalar=SGN, in1=tA[:], op0=ALU.mult, op1=ALU.add), r=[tAk, tBk, "colc"], w=[u1k])
            V(lambda e: e.tensor_tensor(tA[:], slV[:, 1], bc(fre[:]), ALU.mult), r=["slV", frk], w=[tAk])
            V(lambda e: e.tensor_tensor(tB[:], slV[:, 0], bc(fim[:]), ALU.mult), r=["slV", fik], w=[tBk])
            V(lambda e: e.scalar_tensor_tensor(out=U2[:], in0=tB[:], scalar=SGNC, in1=tA[:], op0=ALU.mult, op1=ALU.add), r=[tAk, tBk, "colc"], w=[u2k])
            for j in range(8):
                V(lambda e, j=j: e.tensor_tensor(tA[:], U1[:], bc(Ere[:, j, :]), ALU.mult), r=[u1k, erk], w=[tAk])
                V(lambda e, j=j: e.tensor_tensor(tB[:], U2[:], bc(Eim[:, j, :]), ALU.mult), r=[u2k, eik], w=[tBk])
                V(lambda e, j=j: e.scalar_tensor_tensor(out=PSb[:, j], in0=tB[:], scalar=SGN, in1=tA[:], op0=ALU.mult, op1=ALU.add),
                  r=[tAk, tBk, "colc"], w=[psbk])
            V(lambda e: e.tensor_scalar(CSb[:], slC[:], SGNC, None, ALU.mult), r=["slC", "colc"], w=[csbk])
            for jt in range(8):
                pb = next_ps()
                for pr in range(4):
                    g0 = 2 * (4 * jt + pr)
                    for j in range(8):
                        kw = dict(tile_position=(0, 96)) if pr == 3 else {}
                        T(lambda e, pb=pb, pr=pr, j=j, g0=g0, kw=kw: e.matmul(ps[pb][32 * pr:32 * pr + 32, j * 32:(j + 1) * 32],
                                                                            PSb[:, j, g0:g0 + 2, :], CSb[:, g0:g0 + 2, :],
                                                                            start=True, stop=True, **kw),
                          r=[psbk, csbk], w=[PSK[pb]])
                for pr in range(4):
                    V(lambda e, pb=pb, pr=pr, jt=jt: e.tensor_tensor(BD[:, jt, :, 32 * pr:32 * pr + 32],
                                                                     ps[pb][:, 0:256].rearrange("p (j c) -> p j c", j=8),
                                                                     mask4[:, pr, :].unsqueeze(1).to_broadcast([128, 8, 32]), ALU.mult),
                      r=[PSK[pb], "mask4"], w=["BD"])

        _sl()
        if dbg == "ssm_BD":
            return dump("BD", BD, [128, 8, 8, 128], BF16, ["BD"])
        phase_barrier("sl")
        AR.release(ms)

        def _pl():
            plpar = AR.alloc("plpar", [128, 3, 32], F32)
            plC = AR.alloc("plC", [128, 2, 32, 16], F32)
            DS(lambda e: e.dma_start(out=plpar[:], in_=plpar_d), w=["plpar"])
            DS(lambda e: e.dma_start(out=plC[:], in_=plC_d), w=["plC"])
            (Ere, erk), (Eim, eik) = disc(plpar, "plpar", 32, list(range(1, 9)))
            qa, qak = tmp([128, 32, 16]); qb, qbk = tmp([128, 32, 16]); Qre, qrk = tmp([128, 32, 16]); Qim, qik = tmp([128, 32, 16])
            bc2 = lambda ap: ap.unsqueeze(2).to_broadcast([128, 32, 16])
            for t in range(8):
                V(lambda e, t=t: e.tensor_tensor(qa[:], plC[:, 0], bc2(Ere[:, t, :]), ALU.mult), r=["plC", erk], w=[qak])
                V(lambda e, t=t: e.tensor_tensor(qb[:], plC[:, 1], bc2(Eim[:, t, :]), ALU.mult), r=["plC", eik], w=[qbk])
                V(lambda e: e.tensor_tensor(Qre[:], qa[:], qb[:], ALU.subtract), r=[qak, qbk], w=[qrk])
                V(lambda e, t=t: e.tensor_tensor(qa[:], plC[:, 0], bc2(Eim[:, t, :]), ALU.mult), r=["plC", eik], w=[qak])
                V(lambda e, t=t: e.tensor_tensor(qb[:], plC[:, 1], bc2(Ere[:, t, :]), ALU.mult), r=["plC", erk], w=[qbk])
                V(lambda e: e.tensor_tensor(Qim[:], qa[:], qb[:], ALU.add), r=[qak, qbk], w=[qik])
                V(lambda e, t=t: e.tensor_scalar(HC[:, :, t, 0, 0:16], Qre[:], TOP, None, ALU.mult), r=[qrk, "colc"], w=["HC"])
                V(lambda e, t=t: e.tensor_scalar(HC[:, :, t, 0, 16:32], Qre[:], BOT, None, ALU.mult), r=[qrk, "colc"], w=["HC"])
                V(lambda e, t=t: e.tensor_scalar(HC[:, :, t, 1, 0:16], Qim[:], NTOP, None, ALU.mult), r=[qik, "colc"], w=["HC"])
                V(lambda e, t=t: e.tensor_scalar(HC[:, :, t, 1, 16:32], Qim[:], NBOT, None, ALU.mult), r=[qik, "colc"], w=["HC"])
            for ri in range(2):
                V(lambda e, ri=ri: e.tensor_copy(ArB[:, ri, :], Ere[:, 7, :]), r=[erk], w=["ArB"])
                V(lambda e, ri=ri: e.tensor_copy(AiB[:, ri, :], Eim[:, 7, :]), r=[eik], w=["AiB"])

        _pl()
        if dbg == "ssm_HC":
            return dump("HC", HC, [128, 32, 8, 2, 32], BF16, ["HC"])
        phase_barrier("pl")
        AR.release(ms)

        PTre = AR.alloc("PTre", [128, 8, 512], F32)
        PTim = AR.alloc("PTim", [128, 8, 512], F32)
        mt2 = AR.mark()
        tlpar = AR.alloc("tlpar", [128, 3, 512], F32)
        tlB = AR.alloc("tlB", [128, 2, 512], F32)
        Bbre = AR.alloc("Bbre", [128, 512], F32)
        Bbim = AR.alloc("Bbim", [128, 512], F32)
        ta = AR.alloc("tla", [128, 512], F32)
        tb_ = AR.alloc("tlb", [128, 512], F32)
        DS(lambda e: e.dma_start(out=tlpar[:], in_=tlpar_d), w=["tlpar"])
        DS(lambda e: e.dma_start(out=tlB[:], in_=tlB_d), w=["tlB"])
        mt = AR.mark()

        def _tl0():
            (Ere, erk), (Eim, eik) = disc(tlpar, "tlpar", 512, [1])
            (fre, frk), (fim, fik) = zoh(tlpar, "tlpar", 512, Ere[:, 0, :], Eim[:, 0, :], [erk, eik])
            V(lambda e: e.tensor_tensor(ta[:], fre[:], tlB[:, 0, :], ALU.mult), r=[frk, "tlB"], w=["tla"])
            V(lambda e: e.tensor_tensor(tb_[:], fim[:], tlB[:, 1, :], ALU.mult), r=[fik, "tlB"], w=["tlb"])
            V(lambda e: e.tensor_tensor(Bbre[:], ta[:], tb_[:], ALU.subtract), r=["tla", "tlb"], w=["Bbre"])
            V(lambda e: e.tensor_tensor(ta[:], fre[:], tlB[:, 1, :], ALU.mult), r=[frk, "tlB"], w=["tla"])
            V(lambda e: e.tensor_tensor(tb_[:], fim[:], tlB[:, 0, :], ALU.mult), r=[fik, "tlB"], w=["tlb"])
            V(lambda e: e.tensor_tensor(Bbim[:], ta[:], tb_[:], ALU.add), r=["tla", "tlb"], w=["Bbim"])

        def _tltau(tau):
            (Ere, erk), (Eim, eik) = disc(tlpar, "tlpar", 512, [7 - tau])
            V(lambda e: e.tensor_tensor(ta[:], Ere[:, 0, :], Bbre[:], ALU.mult), r=[erk, "Bbre"], w=["tla"])
            V(lambda e: e.tensor_tensor(tb_[:], Eim[:, 0, :], Bbim[:], ALU.mult), r=[eik, "Bbim"], w=["tlb"])
            V(lambda e: e.tensor_tensor(PTre[:, tau, :], ta[:], tb_[:], ALU.subtract), r=["tla", "tlb"], w=["PTre"])
            V(lambda e: e.tensor_tensor(ta[:], Ere[:, 0, :], Bbim[:], ALU.mult), r=[erk, "Bbim"], w=["tla"])
            V(lambda e: e.tensor_tensor(tb_[:], Eim[:, 0, :], Bbre[:], ALU.mult), r=[eik, "Bbre"], w=["tlb"])
            V(lambda e: e.tensor_tensor(PTim[:, tau, :], ta[:], tb_[:], ALU.add), r=["tla", "tlb"], w=["PTim"])

        _tl0()
        for tau in range(8):
            phase_barrier("tl%d" % tau)
            AR.release(mt)
            _tltau(tau)
        if dbg == "ssm_PT":
            fin.append(DS(lambda e: e.dma_start(out=dbg_out("PTim", [128, 8, 512], F32), in_=PTim[:]), r=["PTim"]))
            return dump("PTre", PTre, [128, 8, 512], F32, ["PTre"])
        phase_barrier("tl")
        AR.release(mt2)
        XS = AR.alloc("XS", [128, 2, 256, 32], BF16)

        GT = [AR.alloc("GT%d" % i, [128, 8, 2, 128], BF16) for i in range(2)]
        ut3 = [AR.alloc("ut3_%d" % i, [128, L], BF16) for i in range(2)]
        for j in range(8):
            b = j % 2
            DS(lambda e, b=b, j=j: e.dma_start(out=ut3[b][:], in_=uT_d[j]), r=["uT_d"], w=["ut%d" % b])
            for ri, PT, ptk in ((0, PTre, "PTre"), (1, PTim, "PTim")):
                for gg, colsel in ((0, NPAR), (1, PAR)):
                    V(lambda e, b=b, j=j, ri=ri, gg=gg, PT=PT, colsel=colsel: e.tensor_scalar(
                        GT[b][:, :, ri, gg * 64:(gg + 1) * 64], PT[:, :, j * 64:(j + 1) * 64], colsel, None, ALU.mult),
                      r=[ptk, "colc"], w=["GT%d" % b])
            for pr in range(4):
                pb = next_ps()
                for ri in range(2):
                    for tau in range(8):
                        kw = dict(tile_position=(96, 0)) if pr == 3 else {}
                        T(lambda e, pb=pb, b=b, pr=pr, ri=ri, tau=tau, kw=kw: e.matmul(
                            ps[pb][:, ri * 256:(ri + 1) * 256], GT[b][32 * pr:32 * pr + 32, tau, ri, :], ut3[b][32 * pr:32 * pr + 32, tau::8],
                            start=(tau == 0), stop=(tau == 7), **kw),
                          r=["GT%d" % b, "ut%d" % b], w=[PSK[pb]])
                ev = A if pr % 2 == 0 else V
                if pr % 2 == 0:
                    A(lambda e, pb=pb, j=j, pr=pr: e.copy(XS[:, :, :, 4 * j + pr], ps[pb][:].rearrange("p (r n) -> p r n", r=2)),
                      r=[PSK[pb]], w=["XS"])
                else:
                    V(lambda e, pb=pb, j=j, pr=pr: e.tensor_copy(XS[:, :, :, 4 * j + pr], ps[pb][:].rearrange("p (r n) -> p r n", r=2)),
                      r=[PSK[pb]], w=["XS"])

        if dbg == "ssm_X":
            return dump("XS", XS, [128, 2, 256, 32], BF16, ["XS"])
        st = AR.alloc("st", [128, 2, 32], F32)
        T13 = AR.alloc("T13", [128, 2, 32], F32)
        T24 = AR.alloc("T24", [128, 2, 32], F32)
        V(lambda e: e.tensor_copy(st[:], XS[:, :, 0, :]), r=["XS"], w=["st"])
        for n in range(1, 255):
            V(lambda e: e.tensor_tensor(T13[:], st[:], ArB[:], ALU.mult), r=["st", "ArB"], w=["T13"])
            V(lambda e: e.tensor_tensor(T24[:], st[:], AiB[:], ALU.mult), r=["st", "AiB"], w=["T24"])
            V(lambda e: e.tensor_tensor(st[:, 0, :], T13[:, 0, :], T24[:, 1, :], ALU.subtract), r=["T13", "T24"], w=["st"])
            V(lambda e: e.tensor_tensor(st[:, 1, :], T13[:, 1, :], T24[:, 0, :], ALU.add), r=["T13", "T24"], w=["st"])
            V(lambda e, n=n: e.tensor_tensor(st[:], st[:], XS[:, :, n, :], ALU.add), r=["st", "XS"], w=["st"])
            V(lambda e, n=n: e.tensor_copy(XS[:, :, n, :], st[:]), r=["st"], w=["XS"])

        if dbg == "ssm_S":
            return dump("XS", XS, [128, 2, 256, 32], BF16, ["XS"])
        ytmp = [AR.alloc("ytmp%d" % i, [128, 256], F32) for i in range(2)]
        yin = [AR.alloc("yin%d" % i, [128, 256], F32) for i in range(2)]
        ysg = [AR.alloc("ysg%d" % i, [128, 256], F32) for i in range(2)]
        for j in range(8):
            b = j % 2
            DS(lambda e, b=b, j=j: e.dma_start(out=ut3[b][:], in_=uT_d[j]), r=["uT_d"], w=["ut%d" % b])
            for t in range(8):
                pb = next_ps()
                q = t % 2
                for tau in range(t + 1):
                    T(lambda e, pb=pb, b=b, j=j, t=t, tau=tau: e.matmul(ps[pb][:, 0:256], BD[:, j, t - tau, :], ut3[b][:, tau::8],
                                                                       start=(tau == 0), stop=False),
                      r=["BD", "ut%d" % b], w=[PSK[pb]])
                for pr in range(4):
                    for ri in range(2):
                        kw = dict(tile_position=(0, 96)) if pr == 3 else {}
                        last = (pr == 3 and ri == 1)
                        T(lambda e, pb=pb, j=j, t=t, pr=pr, ri=ri, kw=kw, last=last: e.matmul(
                            ps[pb][32 * pr:32 * pr + 32, 1:256], HC[:, 4 * j + pr, t, ri, :], XS[:, ri, 0:255, 4 * j + pr],
                            start=False, stop=last, **kw),
                          r=["HC", "XS"], w=[PSK[pb]])
                V(lambda e, pb=pb, b=b, j=j, t=t, q=q: e.scalar_tensor_tensor(out=ytmp[q][:], in0=ut3[b][:, t::8], scalar=dT[:, j:j + 1],
                                                                              in1=ps[pb][:, 0:256], op0=ALU.mult, op1=ALU.add),
                  r=["ut%d" % b, "dT", PSK[pb]], w=["ytmp%d" % q])
                G(lambda e, q=q: e.tensor_tensor(yin[q][:], ytmp[q][:], ytmp[q][:], ALU.mult), r=["ytmp%d" % q], w=["yin%d" % q])
                G(lambda e, q=q: e.tensor_scalar(yin[q][:], yin[q][:], 0.044715, 1.0, ALU.mult, ALU.add), r=["yin%d" % q], w=["yin%d" % q])
                G(lambda e, q=q: e.tensor_tensor(yin[q][:], yin[q][:], ytmp[q][:], ALU.mult), r=["yin%d" % q, "ytmp%d" % q], w=["yin%d" % q])
                A(lambda e, q=q: e.activation(ysg[q][:], yin[q][:], AF.Sigmoid, scale=1.5957691216057308), r=["yin%d" % q], w=["ysg%d" % q])
                V(lambda e, q=q, j=j, t=t: e.tensor_tensor(zT[:, j, t::8], ytmp[q][:], ysg[q][:], ALU.mult),
                  r=["ytmp%d" % q, "ysg%d" % q], w=["zT"])
        if dbg == "zT":
            fin.append(DS(lambda e: e.dma_start(out=dbg_out("zT", [128, 8, L], BF16), in_=zT[:]), r=["zT"]))
            P.emit(fin)
            return nc, dbg_outs
        phase_barrier("p3a")
        AR.release(ms)

        wglu = AR.alloc("wglu", [128, 8, 1024], BF16)
        DG(lambda e: e.dma_start(out=wglu[:], in_=wglu_d.rearrange("(k p) n -> p k n", p=128)), w=["wglu"])
        oT = AR.alloc("oT", [128, 8, 512], BF16)
        sig = [AR.alloc("sig%d" % i, [128, 512], F32) for i in range(2)]
        sq16 = [AR.alloc("sq16_%d" % i, [128, 512], BF16) for i in range(2)]
        rstd = AR.alloc("rstd", [128, 512], F32)
        for tb in range(4):
            sl = slice(tb * 512, (tb + 1) * 512)
            pq = next_ps()
            for ft in range(8):
                pb = next_ps()
                while pb == pq:
                    pb = next_ps()
                q = ft % 2
                for k in range(8):
                    T(lambda e, pb=pb, k=k, ft=ft, sl=sl: e.matmul(ps[pb][:], wglu[:, k, ft * 128:(ft + 1) * 128], zT[:, k, sl],
                                                                  start=(k == 0), stop=(k == 7)), r=["wglu", "zT"], w=[PSK[pb]])
                A(lambda e, pb=pb, q=q: e.activation(sig[q][:], ps[pb][:], AF.Sigmoid), r=[PSK[pb]], w=["sig%d" % q])
                V(lambda e, q=q, ft=ft, sl=sl: e.tensor_tensor(sig[q][:], sig[q][:], zT[:, ft, sl], ALU.mult), r=["sig%d" % q, "zT"], w=["sig%d" % q])
                A(lambda e, q=q, ft=ft: e.copy(oT[:, ft, :], sig[q][:]), r=["sig%d" % q], w=["oT"])
                G(lambda e, q=q: e.tensor_tensor(sq16[q][:], sig[q][:], sig[q][:], ALU.mult), r=["sig%d" % q], w=["sq16_%d" % q])
                T(lambda e, pq=pq, q=q, ft=ft: e.matmul(ps[pq][:], ones16[:], sq16[q][:], start=(ft == 0), stop=(ft == 7)),
                  r=["ones16", "sq16_%d" % q], w=[PSK[pq]])
            A(lambda e, pq=pq: e.activation(rstd[:], ps[pq][:], AF.Sqrt, bias=EPS6, scale=1.0 / 1024), r=[PSK[pq], "colc"], w=["rstd"])
            V(lambda e: e.reciprocal(rstd[:], rstd[:]), r=["rstd"], w=["rstd"])
            for ft in range(8):
                V(lambda e, ft=ft, sl=sl: e.scalar_tensor_tensor(out=zT[:, ft, sl], in0=oT[:, ft, :], scalar=betaS[:, ft:ft + 1], in1=rstd[:],
                                                                 op0=ALU.mult, op1=ALU.mult), r=["oT", "betaS", "rstd"], w=["zT"])
        if dbg == "yssm":
            fin.append(DS(lambda e: e.dma_start(out=dbg_out("yssm", [128, 8, L], BF16), in_=zT[:]), r=["zT"]))
            P.emit(fin)
            return nc, dbg_outs
        phase_barrier("p3")
        AR.release(ms)
        m4 = AR.mark()
        gt1b = AR.alloc("gt1b", [128, D], F32)
        wo = [AR.alloc("wo%d" % i, [128, 16, 512], BF16) for i in range(2)]
        xt = [AR.alloc("xt%d" % i, [128, 512], F32) for i in range(3)]
        x1t = [AR.alloc("x1t%d" % i, [128, 512], F32) for i in range(3)]
        zrow = AR.alloc("zrow", [1, D], F32)
        zrow16 = AR.alloc("zrow16", [1, D], BF16)
        fillt = AR.alloc("fillt", [128, 64], F32)
        mod_bcast(gt1b, 2, "gt1b")
        V(lambda e: e.memset(zrow[:], 0.0), w=["zrow"])
        V(lambda e: e.memset(zrow16[:], 0.0), w=["zrow16"])
        V(lambda e: e.memset(fillt[:], 2048.0), w=["fillt"])
        DS(lambda e: e.dma_start(out=acc_d[L:L + 1, :], in_=zrow[:]), r=["zrow"], w=["acc_pad"])
        DS(lambda e: e.dma_start(out=h2_d[L:L + 1, :], in_=zrow16[:]), r=["zrow16"], w=["h2_pad"])
        DS(lambda e: e.dma_start(out=ti_d[L:L + 1, :], in_=zrow[0:1, 0:4]), r=["zrow"], w=["ti_pad"])
        DS(lambda e: e.dma_start(out=slot_d.rearrange("(p j) o -> p (j o)", p=128), in_=fillt[:]), r=["fillt"], w=["slot_d"])
        wout_v = wout_d.rearrange("(k p) n -> p k n", p=128)
        cnt4 = 0
        for cb in range(4):
            wb = cb % 2
            DG(lambda e, wb=wb, cb=cb: e.dma_start(out=wo[wb][:], in_=wout_v[:, :, cb * 512:(cb + 1) * 512]), w=["wo%d" % wb])
            for tt in range(16):
                pb = next_ps()
                q = cnt4 % 3
                cnt4 += 1
                for k in range(16):
                    src = zT if k < 8 else yretT
                    skey = "zT" if k < 8 else "yretT"
                    T(lambda e, pb=pb, k=k, tt=tt, wb=wb, src=src: e.matmul(ps[pb][:], src[:, k % 8, tt * 128:(tt + 1) * 128], wo[wb][:, k, :],
                                                                           start=(k == 0), stop=(k == 15)),
                      r=[skey, "wo%d" % wb], w=[PSK[pb]])
                DS(lambda e, q=q, tt=tt, cb=cb: e.dma_start(out=xt[q][:], in_=x_d[tt * 128:(tt + 1) * 128, cb * 512:(cb + 1) * 512]), w=["xt%d" % q])
                V(lambda e, pb=pb, q=q, cb=cb: e.tensor_tensor(x1t[q][:], ps[pb][:], gt1b[:, cb * 512:(cb + 1) * 512], ALU.mult),
                  r=[PSK[pb], "gt1b"], w=["x1t%d" % q])
                G(lambda e, q=q: e.tensor_tensor(x1t[q][:], x1t[q][:], xt[q][:], ALU.add), r=["x1t%d" % q, "xt%d" % q], w=["x1t%d" % q])
                DS(lambda e, q=q, tt=tt, cb=cb: e.dma_start(out=acc_d[tt * 128:(tt + 1) * 128, cb * 512:(cb + 1) * 512], in_=x1t[q][:]),
                   r=["x1t%d" % q], w=["acc%d" % tt])
        phase_barrier("p4")
        AR.release(base_mark)

        blke = AR.alloc("blke", [128, 64], F32)
        rtc = AR.alloc("rtc", [128, 112], F32)
        m5 = AR.mark()
        A2b = AR.alloc("A2b", [128, D], F32)
        B2b = AR.alloc("B2b", [128, D], F32)
        g2b = AR.alloc("g2b", [128, D], F32)
        wr = AR.alloc("wr", [128, 16, 36], F32)
        brb = AR.alloc("brb", [128, 36], F32)
        LG = AR.alloc("LG", [128, 16, 36], F32)
        ss5 = AR.alloc("ss", [128, 16], F32)
        rs5 = AR.alloc("rs", [128, 16], F32)
        junk5 = AR.alloc("junk", [128, D], F32)
        xb5 = [AR.alloc("xb%d" % i, [128, D], F32) for i in range(2)]
        h2f = AR.alloc("h2f", [128, D], F32)
        h2b = [AR.alloc("h2b%d" % i, [128, D], BF16) for i in range(2)]
        h2T = AR.alloc("h2T", [128, 16, 128], F32)
        mod_bcast(A2b, 4, "A2b")
        mod_bcast(B2b, 3, "B2b")
        DS(lambda e: e.dma_start(out=g2b[:], in_=g2_d.partition_broadcast(128)), w=["g2b"])
        DS(lambda e: e.dma_start(out=wr[:], in_=wr_d.rearrange("(k p) n -> p k n", p=128)), w=["wr"])
        DS(lambda e: e.dma_start(out=brb[:], in_=br_d.partition_broadcast(128)), w=["brb"])
        V(lambda e: e.scalar_tensor_tensor(out=A2b[:], in0=A2b[:], scalar=1.0, in1=g2b[:], op0=ALU.add, op1=ALU.mult),
          r=["A2b", "g2b"], w=["A2b"])
        V(lambda e: e.memset(ss5[:], 0.0), w=["ss"])
        for tt in range(16):
            b = tt % 2
            DS(lambda e, b=b, tt=tt: e.dma_start(out=xb5[b][:], in_=acc_d[tt * 128:(tt + 1) * 128, :]), w=["xb%d" % b])
            rms_rstd_g(junk5, ss5, rs5, xb5[b][:], "xb%d" % b, tt, EPS6, D)
            V(lambda e, b=b, tt=tt: e.scalar_tensor_tensor(out=h2f[:], in0=xb5[b][:], scalar=rs5[:, tt:tt + 1], in1=A2b[:],
                                                           op0=ALU.mult, op1=ALU.mult), r=["xb%d" % b, "rs", "A2b"], w=["h2f"])
            V(lambda e: e.tensor_tensor(h2f[:], h2f[:], B2b[:], ALU.add), r=["h2f", "B2b"], w=["h2f"])
            A(lambda e, b=b: e.copy(h2b[b][:], h2f[:]), r=["h2f"], w=["h2b%d" % b])
            DS(lambda e, b=b, tt=tt: e.dma_start(out=h2_d[tt * 128:(tt + 1) * 128, :], in_=h2b[b][:]), r=["h2b%d" % b], w=["h2_d"])
            for kg in range(4):
                pb = next_ps()
                for kk in range(4):
                    k = kg * 4 + kk
                    T(lambda e, pb=pb, kk=kk, k=k: e.matmul(ps[pb][:, kk * 128:(kk + 1) * 128], h2f[:, k * 128:(k + 1) * 128], ident32,
                                                          start=True, stop=True), r=["h2f", "cst"], w=[PSK[pb]])
                if kg % 2 == 0:
                    A(lambda e, pb=pb, kg=kg: e.copy(h2T[:, kg * 4:(kg + 1) * 4, :], ps[pb][:].rearrange("p (a c) -> p a c", a=4)),
                      r=[PSK[pb]], w=["h2T"])
                else:
                    V(lambda e, pb=pb, kg=kg: e.tensor_copy(h2T[:, kg * 4:(kg + 1) * 4, :], ps[pb][:].rearrange("p (a c) -> p a c", a=4)),
                      r=[PSK[pb]], w=["h2T"])
            pb = next_ps()
            for k in range(16):
                T(lambda e, pb=pb, k=k: e.matmul(ps[pb][:, 0:36], h2T[:, k, :], wr[:, k, :], start=(k == 0), stop=(k == 15)),
                  r=["h2T", "wr"], w=[PSK[pb]])
            V(lambda e, pb=pb, tt=tt: e.tensor_tensor(LG[:, tt, :], ps[pb][:, 0:36], brb[:], ALU.add), r=[PSK[pb], "brb"], w=["LG"])
        if dbg == "LG":
            return dump("LG", LG, [128, 16, 36], F32, ["LG"])

        DS(lambda e: e.dma_start(out=rtc[:], in_=rt_d), w=["rtc"])
        tokid = rtc[:, 0:16]
        blkthr = rtc[:, 16:80]
        iota32 = rtc[:, 80:112]
        rk = [0]

        def rt(shape, dt=F32):
            rk[0] += 1
            return AR.alloc("rt%d" % rk[0], shape, dt), "rt%d" % rk[0]

        gl = LG[:, :, 0:4]
        gmax, gmaxk = rt([128, 16]); ohg, ohgk = rt([128, 16, 4]); ge, gek = rt([128, 16, 4]); gw, gwk = rt([128, 16])
        b3 = lambda ap, n: ap.unsqueeze(2).to_broadcast([128, 16, n])
        V(lambda e: e.tensor_reduce(out=gmax[:], in_=gl, axis=AX.X, op=ALU.max), r=["LG"], w=[gmaxk])
        V(lambda e: e.tensor_tensor(ohg[:], gl, b3(gmax[:], 4), ALU.is_equal), r=["LG", gmaxk], w=[ohgk])
        V(lambda e: e.tensor_tensor(ge[:], gl, b3(gmax[:], 4), ALU.subtract), r=["LG", gmaxk], w=[gek])
        A(lambda e: e.activation(ge[:], ge[:], AF.Exp), r=[gek], w=[gek])
        V(lambda e: e.tensor_reduce(out=gw[:], in_=ge[:], axis=AX.X, op=ALU.add), r=[gek], w=[gwk])
        V(lambda e: e.reciprocal(gw[:], gw[:]), r=[gwk], w=[gwk])
        els, elsk = rt([128, 16, 8]); etmp, etk = rt([128, 16, 8])
        V(lambda e: e.tensor_tensor(els[:], LG[:, :, 4:12], b3(ohg[:, :, 0], 8), ALU.mult), r=["LG", ohgk], w=[elsk])
        for g in range(1, 4):
            V(lambda e, g=g: e.tensor_tensor(etmp[:], LG[:, :, 4 + 8 * g:12 + 8 * g], b3(ohg[:, :, g], 8), ALU.mult), r=["LG", ohgk], w=[etk])
            V(lambda e: e.tensor_tensor(els[:], els[:], etmp[:], ALU.add), r=[elsk, etk], w=[elsk])
        mx1, mx1k = rt([128, 16]); oh1, oh1k = rt([128, 16, 8]); el2, el2k = rt([128, 16, 8]); mx2, mx2k = rt([128, 16]); oh2, oh2k = rt([128, 16, 8])
        V(lambda e: e.tensor_reduce(out=mx1[:], in_=els[:], axis=AX.X, op=ALU.max), r=[elsk], w=[mx1k])
        V(lambda e: e.tensor_tensor(oh1[:], els[:], b3(mx1[:], 8), ALU.is_equal), r=[elsk, mx1k], w=[oh1k])
        V(lambda e: e.scalar_tensor_tensor(out=el2[:], in0=oh1[:], scalar=-1.0e30, in1=els[:], op0=ALU.mult, op1=ALU.add), r=[oh1k, elsk], w=[el2k])
        V(lambda e: e.tensor_reduce(out=mx2[:], in_=el2[:], axis=AX.X, op=ALU.max), r=[el2k], w=[mx2k])
        V(lambda e: e.tensor_tensor(oh2[:], el2[:], b3(mx2[:], 8), ALU.is_equal), r=[el2k, mx2k], w=[oh2k])
        ee, eek = rt([128, 16]); w1, w1k = rt([128, 16]); w2, w2k = rt([128, 16])
        V(lambda e: e.tensor_tensor(ee[:], mx2[:], mx1[:], ALU.subtract), r=[mx1k, mx2k], w=[eek])
        A(lambda e: e.activation(ee[:], ee[:], AF.Exp), r=[eek], w=[eek])
        V(lambda e: e.tensor_scalar(w1[:], ee[:], 1.0, None, ALU.add), r=[eek], w=[w1k])
        V(lambda e: e.reciprocal(w1[:], w1[:]), r=[w1k], w=[w1k])
        V(lambda e: e.tensor_tensor(w2[:], ee[:], w1[:], ALU.mult), r=[eek, w1k], w=[w2k])
        V(lambda e: e.tensor_tensor(w1[:], w1[:], gw[:], ALU.mult), r=[w1k, gwk], w=[w1k])
        V(lambda e: e.tensor_tensor(w2[:], w2[:], gw[:], ALU.mult), r=[w2k, gwk], w=[w2k])
        gsel, gselk = rt([128, 16]); j1, j1k = rt([128, 16]); j2, j2k = rt([128, 16]); itmp, itk = rt([128, 16, 8])
        ib = lambda n: iota32[:, 0:n].unsqueeze(1).to_broadcast([128, 16, n])
        V(lambda e: e.tensor_tensor(itmp[:, :, 0:4], ohg[:], ib(4), ALU.mult), r=[ohgk, "rtc"], w=[itk])
        V(lambda e: e.tensor_reduce(out=gsel[:], in_=itmp[:, :, 0:4], axis=AX.X, op=ALU.add), r=[itk], w=[gselk])
        V(lambda e: e.tensor_tensor(itmp[:], oh1[:], ib(8), ALU.mult), r=[oh1k, "rtc"], w=[itk])
        V(lambda e: e.tensor_reduce(out=j1[:], in_=itmp[:], axis=AX.X, op=ALU.add), r=[itk], w=[j1k])
        V(lambda e: e.tensor_tensor(itmp[:], oh2[:], ib(8), ALU.mult), r=[oh2k, "rtc"], w=[itk])
        V(lambda e: e.tensor_reduce(out=j2[:], in_=itmp[:], axis=AX.X, op=ALU.add), r=[itk], w=[j2k])
        TIt, tik = rt([128, 16, 4])
        V(lambda e: e.tensor_copy(TIt[:, :, 0], w1[:]), r=[w1k], w=[tik])
        V(lambda e: e.tensor_copy(TIt[:, :, 2], w2[:]), r=[w2k], w=[tik])
        V(lambda e: e.scalar_tensor_tensor(out=TIt[:, :, 1], in0=gsel[:], scalar=8.0, in1=j1[:], op0=ALU.mult, op1=ALU.add), r=[gselk, j1k], w=[tik])
        V(lambda e: e.scalar_tensor_tensor(out=TIt[:, :, 3], in0=gsel[:], scalar=8.0, in1=j2[:], op0=ALU.mult, op1=ALU.add), r=[gselk, j2k], w=[tik])
        DS(lambda e: e.dma_start(out=ti_d[0:L, :].rearrange("(t p) c -> p t c", p=128), in_=TIt[:]), r=[tik], w=["ti_d"])
        OH1, OH1k = rt([128, 16, 32]); OH2, OH2k = rt([128, 16, 32]); Mt, Mk = rt([128, 16, 32])
        i32b = iota32.unsqueeze(1).to_broadcast([128, 16, 32])
        V(lambda e: e.tensor_tensor(OH1[:], i32b, b3(TIt[:, :, 1], 32), ALU.is_equal), r=["rtc", tik], w=[OH1k])
        V(lambda e: e.tensor_tensor(OH2[:], i32b, b3(TIt[:, :, 3], 32), ALU.is_equal), r=["rtc", tik], w=[OH2k])
        V(lambda e: e.tensor_tensor(Mt[:], OH1[:], OH2[:], ALU.add), r=[OH1k, OH2k], w=[Mk])
        pc = next_ps()
        pr_ = next_ps()
        T(lambda e, pc=pc: e.matmul(ps[pc][:], ones32, Mt[:].rearrange("p a b -> p (a b)"), start=True, stop=True), r=["cst", Mk], w=[PSK[pc]])
        T(lambda e, pr_=pr_: e.matmul(ps[pr_][:], tri32, Mt[:].rearrange("p a b -> p (a b)"), start=True, stop=True), r=["cst", Mk], w=[PSK[pr_]])
        tot, totk = rt([128, 16, 32]); dest, destk = rt([128, 16, 32]); texc, texck = rt([128, 16, 32])
        A(lambda e, pc=pc: e.copy(tot[:].rearrange("p a b -> p (a b)"), ps[pc][:]), r=[PSK[pc]], w=[totk])
        V(lambda e, pr_=pr_: e.tensor_copy(dest[:].rearrange("p a b -> p (a b)"), ps[pr_][:]), r=[PSK[pr_]], w=[destk])
        V(lambda e: e.memset(texc[:, 0, :], 0.0), w=[texck])
        for tt in range(1, 16):
            V(lambda e, tt=tt: e.tensor_tensor(texc[:, tt, :], texc[:, tt - 1, :], tot[:, tt - 1, :], ALU.add), r=[texck, totk], w=[texck])
        cntt, cntk = rt([128, 32]); nbi, nbik = rt([128, 32], I32); padd, padk = rt([128, 32]); pend, pendk = rt([128, 32]); poff, poffk = rt([128, 32])
        onesr, onesk = rt([128, 32])
        V(lambda e: e.tensor_tensor(cntt[:], texc[:, 15, :], tot[:, 15, :], ALU.add), r=[texck, totk], w=[cntk])
        V(lambda e: e.tensor_scalar(cntt[:], cntt[:], 1.0 / 128, 0.49609375, ALU.mult, ALU.add), r=[cntk], w=[cntk])
        V(lambda e: e.tensor_copy(nbi[:], cntt[:]), r=[cntk], w=[nbik])
        V(lambda e: e.tensor_copy(padd[:], nbi[:]), r=[nbik], w=[padk])
        V(lambda e: e.tensor_scalar(padd[:], padd[:], 128.0, None, ALU.mult), r=[padk], w=[padk])
        V(lambda e: e.memset(onesr[:], 1.0), w=[onesk])
        V(lambda e: e.tensor_tensor_scan(pend[:], onesr[:], padd[:], 0.0, ALU.mult, ALU.add), r=[onesk, padk], w=[pendk])
        V(lambda e: e.tensor_tensor(poff[:], pend[:], padd[:], ALU.subtract), r=[pendk, padk], w=[poffk])
        V(lambda e: e.tensor_tensor(dest[:], dest[:], texc[:], ALU.add), r=[destk, texck], w=[destk])
        V(lambda e: e.tensor_tensor(dest[:], dest[:], poff[:, :].unsqueeze(1).to_broadcast([128, 16, 32]), ALU.add), r=[destk, poffk], w=[destk])
        d12, d12k = rt([128, 2, 16]); d12i, d12ik = rt([128, 2, 16], I32)
        V(lambda e: e.tensor_tensor(OH1[:], OH1[:], dest[:], ALU.mult), r=[OH1k, destk], w=[OH1k])
        V(lambda e: e.tensor_reduce(out=d12[:, 0, :], in_=OH1[:], axis=AX.X, op=ALU.add), r=[OH1k], w=[d12k])
        V(lambda e: e.tensor_tensor(OH2[:], OH2[:], dest[:], ALU.mult), r=[OH2k, destk], w=[OH2k])
        V(lambda e: e.tensor_reduce(out=d12[:, 1, :], in_=OH2[:], axis=AX.X, op=ALU.add), r=[OH2k], w=[d12k])
        V(lambda e: e.tensor_copy(d12i[:], d12[:]), r=[d12k], w=[d12ik])
        cmpt, cmpk = rt([128, 64, 32])
        V(lambda e: e.tensor_tensor(cmpt[:], pend[:, :].unsqueeze(1).to_broadcast([128, 64, 32]), blkthr.unsqueeze(2).to_broadcast([128, 64, 32]), ALU.is_le),
          r=[pendk, "rtc"], w=[cmpk])
        V(lambda e: e.tensor_reduce(out=blke[:], in_=cmpt[:], axis=AX.X, op=ALU.add), r=[cmpk], w=["blke"])
        V(lambda e: e.tensor_scalar(blke[:], blke[:], 31.0, None, ALU.min), r=["blke"], w=["blke"])
        for tt in range(16):
            for a_ in range(2):
                DG(lambda e, tt=tt, a_=a_: e.indirect_dma_start(out=slot_d, out_offset=bass.IndirectOffsetOnAxis(ap=d12i[:, a_, tt:tt + 1], axis=0),
                                                               in_=tokid[:, tt:tt + 1], in_offset=None),
                   r=[d12ik, "rtc", "slot_d"], w=["slot_d"])
        if dbg == "route":
            t_ = AR.alloc("dbgslot", [128, 64], F32)
            DS(lambda e: e.dma_start(out=t_[:], in_=slot_d.rearrange("(p j) o -> p (j o)", p=128)), r=["slot_d"], w=["dbgslot"])
            fin.append(DS(lambda e: e.dma_start(out=dbg_out("slot", [128, 64]), in_=t_[:]), r=["dbgslot"]))
            fin.append(DS(lambda e: e.dma_start(out=dbg_out("blke", [128, 64]), in_=blke[:]), r=["blke"]))
            fin.append(DS(lambda e: e.dma_start(out=dbg_out("TI", [128, 16, 4]), in_=TIt[:]), r=[tik]))
            return dump("pend", pend, [128, 32], F32, [pendk])

        phase_barrier("p6")
        AR.release(m5)
        gt2b = AR.alloc("gt2b", [128, D], F32)
        mod_bcast(gt2b, 5, "gt2b")
        wgS = AR.alloc("wgS", [128, 16, 1024], BF16)
        wuS = AR.alloc("wuS", [128, 16, 1024], BF16)
        wdS = AR.alloc("wdS", [128, 8, 2048], BF16)
        xs = [AR.alloc("xs%d" % i, [128, D], BF16) for i in range(2)]
        xsT = AR.alloc("xsT", [128, 16, 128], BF16)
        sg = AR.alloc("sg", [128, 1024], F32)
        a16 = AR.alloc("a16", [128, 1024], BF16)
        actT = AR.alloc("actT", [128, 8, 128], BF16)
        yb = AR.alloc("yb", [128, D], F32)
        stok = [AR.alloc("stok%d" % i, [128, 1], F32) for i in range(2)]
        idx = [AR.alloc("idx%d" % i, [128, 1], I32) for i in range(2)]
        tinf = [AR.alloc("tinf%d" % i, [128, 4], F32) for i in range(2)]
        wsl = [AR.alloc("wsl%d" % i, [128, 2], F32) for i in range(2)]
        offf = AR.alloc("offf", [128, 2], F32)
        offs16f = AR.alloc("offs16f", [128, 16], F32)
        offs8f = AR.alloc("offs8f", [128, 8], F32)
        offs16i = [AR.alloc("offs16i%d" % i, [128, 16], I32) for i in range(2)]
        offs8i = [AR.alloc("offs8i%d" % i, [128, 8], I32) for i in range(2)]
        NB = 64
        skipb = AR.alloc("skipb", [128, 64], F32)
        V(lambda e: e.memset(skipb[:, 0:1], 0.0), w=["skipb"])
        V(lambda e: e.tensor_tensor(skipb[:, 1:64], blke[:, 1:64], blke[:, 0:63], ALU.is_equal), r=["blke", "skipb"], w=["skipb"])
        V(lambda e: e.tensor_scalar(skipb[:], skipb[:], 524288.0, None, ALU.mult), r=["skipb"], w=["skipb"])
        GU_BOUND = NE * 2048 - 1
        DN_BOUND = NE * 1024 - 1
        regcache = {}

        def breg(e, val):
            if val not in regcache:
                regcache[val] = e.to_reg(val)
            return regcache[val]

        def moe_loads(b):
            q = b % 2
            DS(lambda e, q=q, b=b: e.dma_start(out=stok[q][:], in_=slot_d[b * 128:(b + 1) * 128, :]), r=["slot_d"], w=["stok%d" % q])
            V(lambda e, q=q: e.tensor_copy(idx[q][:], stok[q][:]), r=["stok%d" % q], w=["idx%d" % q])
            DG(lambda e, q=q: e.indirect_dma_start(out=tinf[q][:], out_offset=None, in_=ti_d,
                                                   in_offset=bass.IndirectOffsetOnAxis(ap=idx[q][:, :], axis=0)),
               r=["idx%d" % q, "ti_d", "ti_pad"], w=["tinf%d" % q])
            DG(lambda e, q=q: e.indirect_dma_start(out=xs[q][:], out_offset=None, in_=h2_d,
                                                   in_offset=bass.IndirectOffsetOnAxis(ap=idx[q][:, :], axis=0)),
               r=["idx%d" % q, "h2_d", "h2_pad"], w=["xs%d" % q])
            V(lambda e, b=b: e.scalar_tensor_tensor(out=offf[:, 0:1], in0=blke[:, b:b + 1], scalar=128.0, in1=PIDX, op0=ALU.mult, op1=ALU.add),
              r=["blke", "colc"], w=["offf"])
            V(lambda e, b=b: e.tensor_tensor(offf[:, 0:1], offf[:, 0:1], skipb[:, b:b + 1], ALU.add), r=["offf", "skipb"], w=["offf"])
            V(lambda e: e.tensor_scalar(offf[:, 1:2], offf[:, 0:1], 8.0, None, ALU.mult), r=["offf"], w=["offf1"])
            V(lambda e: e.tensor_scalar(offf[:, 0:1], offf[:, 0:1], 16.0, None, ALU.mult), r=["offf", "offf1"], w=["offf"])
            V(lambda e: e.tensor_scalar(offs16f[:], rtc[:, 80:96], offf[:, 0:1], None, ALU.add), r=["rtc", "offf"], w=["offs16f"])
            V(lambda e: e.tensor_scalar(offs8f[:], rtc[:, 80:88], offf[:, 1:2], None, ALU.add), r=["rtc", "offf1"], w=["offs8f"])
            V(lambda e, q=q: e.tensor_copy(offs16i[q][:], offs16f[:]), r=["offs16f"], w=["offs16i%d" % q])
            V(lambda e, q=q: e.tensor_copy(offs8i[q][:], offs8f[:]), r=["offs8f"], w=["offs8i%d" % q])
            for k2 in range(8):
                for wS, w_d, key in ((wgS, wg_d, "wgS"), (wuS, wu_d, "wuS")):
                    DG(lambda e, q=q, wS=wS, w_d=w_d, k2=k2: e.indirect_dma_start(
                        out=wS[:, 2 * k2:2 * k2 + 2, :].rearrange("p a f -> p (a f)"), out_offset=None, in_=w_d,
                        in_offset=bass.IndirectOffsetOnAxis(ap=offs8i[q][:, k2:k2 + 1], axis=0),
                        bounds_check=breg(e, DN_BOUND), oob_is_err=False),
                       r=["offs8i%d" % q], w=["%s%d" % (key, k2)])
            for ff in range(8):
                DG(lambda e, q=q, ff=ff: e.indirect_dma_start(
                    out=wdS[:, ff, :], out_offset=None, in_=wd_d, in_offset=bass.IndirectOffsetOnAxis(ap=offs8i[q][:, ff:ff + 1], axis=0),
                    bounds_check=breg(e, DN_BOUND), oob_is_err=False),
                   r=["offs8i%d" % q], w=["wdS%d" % ff])

        moe_loads(0)
        for b in range(NB):
            q = b % 2
            V(lambda e, q=q, b=b: e.tensor_scalar(wsl[q][:, 0:1], tinf[q][:, 1:2], blke[:, b:b + 1], tinf[q][:, 0:1], ALU.is_equal, ALU.mult),
              r=["tinf%d" % q, "blke"], w=["wsl%d" % q])
            V(lambda e, q=q, b=b: e.tensor_scalar(wsl[q][:, 1:2], tinf[q][:, 3:4], blke[:, b:b + 1], tinf[q][:, 2:3], ALU.is_equal, ALU.mult),
              r=["tinf%d" % q, "blke"], w=["wsl%d" % q])
            V(lambda e, q=q: e.tensor_tensor(wsl[q][:, 0:1], wsl[q][:, 0:1], wsl[q][:, 1:2], ALU.add), r=["wsl%d" % q], w=["wsl%d" % q])
            for kg in range(4):
                pb = 4 + kg % 2
                for kk in range(4):
                    k = kg * 4 + kk
                    T(lambda e, pb=pb, kk=kk, k=k, q=q: e.matmul(ps[pb][:, kk * 128:(kk + 1) * 128], xs[q][:, k::16], id16[:], start=True, stop=True),
                      r=["xs%d" % q, "id16"], w=[PSK[pb]])
                if kg % 2 == 0:
                    A(lambda e, pb=pb, kg=kg: e.copy(xsT[:, kg * 4:(kg + 1) * 4, :], ps[pb][:].rearrange("p (a c) -> p a c", a=4)), r=[PSK[pb]], w=["xsT"])
                else:
                    V(lambda e, pb=pb, kg=kg: e.tensor_copy(xsT[:, kg * 4:(kg + 1) * 4, :], ps[pb][:].rearrange("p (a c) -> p a c", a=4)), r=[PSK[pb]], w=["xsT"])
            for kk in range(16):
                for wi_, (wS, key) in enumerate(((wgS, "wgS"), (wuS, "wuS"))):
                    for hf_ in range(2):
                        pb = wi_ * 2 + hf_
                        T(lambda e, pb=pb, kk=kk, wS=wS, hf_=hf_: e.matmul(ps[pb][:], xsT[:, kk, :], wS[:, kk, hf_ * 512:(hf_ + 1) * 512],
                                                                          start=(kk == 0), stop=(kk == 15)),
                          r=["xsT", "%s%d" % (key, kk // 2)], w=[PSK[pb]])
            for hf_ in range(2):
                A(lambda e, hf_=hf_: e.activation(sg[:, hf_ * 512:(hf_ + 1) * 512], ps[hf_][:], AF.Silu), r=[PSK[hf_]], w=["sg"])
                V(lambda e, hf_=hf_: e.tensor_tensor(a16[:, hf_ * 512:(hf_ + 1) * 512], sg[:, hf_ * 512:(hf_ + 1) * 512], ps[2 + hf_][:], ALU.mult),
                  r=["sg", PSK[2 + hf_]], w=["a16"])
            for fg in range(2):
                pb = 4 + fg
                for fi in range(4):
                    ff = fg * 4 + fi
                    T(lambda e, pb=pb, fi=fi, ff=ff: e.matmul(ps[pb][:, fi * 128:(fi + 1) * 128], a16[:, ff::8], id16[:], start=True, stop=True),
                      r=["a16", "id16"], w=[PSK[pb]])
                if fg == 0:
                    A(lambda e, pb=pb, fg=fg: e.copy(actT[:, fg * 4:(fg + 1) * 4, :], ps[pb][:].rearrange("p (a c) -> p a c", a=4)), r=[PSK[pb]], w=["actT"])
                else:
                    V(lambda e, pb=pb, fg=fg: e.tensor_copy(actT[:, fg * 4:(fg + 1) * 4, :], ps[pb][:].rearrange("p (a c) -> p a c", a=4)), r=[PSK[pb]], w=["actT"])
            for db in range(4):
                pb = 6 + db % 2
                for ff in range(8):
                    T(lambda e, pb=pb, ff=ff, db=db: e.matmul(ps[pb][:], actT[:, ff, :], wdS[:, ff, db * 512:(db + 1) * 512],
                                                            start=(ff == 0), stop=(ff == 7)), r=["actT", "wdS%d" % ff], w=[PSK[pb]])
                V(lambda e, pb=pb, db=db, q=q: e.scalar_tensor_tensor(out=yb[:, db * 512:(db + 1) * 512], in0=ps[pb][:], scalar=wsl[q][:, 0:1],
                                                                     in1=gt2b[:, db * 512:(db + 1) * 512], op0=ALU.mult, op1=ALU.mult),
                  r=[PSK[pb], "wsl%d" % q, "gt2b"], w=["yb"])
            if b + 1 < NB:
                moe_loads(b + 1)
            DG(lambda e, q=q: e.indirect_dma_start(out=acc_d, out_offset=bass.IndirectOffsetOnAxis(ap=idx[q][:, :], axis=0), in_=yb[:],
                                                   in_offset=None, compute_op=ALU.add),
               r=["yb", "idx%d" % q, "acc_pad"] + ["acc%d" % i for i in range(16)], w=["accs"])
        phase_barrier("p7")
        AR.release(base_mark)

        gfb = AR.alloc("gfb", [128, D], F32)
        ss8v = AR.alloc("ss8", [128, 16], F32)
        rs8v = AR.alloc("rs8", [128, 16], F32)
        junk8v = AR.alloc("junk8", [128, D], F32)
        xb8v = [AR.alloc("xf%d" % i, [128, D], F32) for i in range(2)]
        ob = [AR.alloc("ob%d" % i, [128, D], F32) for i in range(2)]
        DS(lambda e: e.dma_start(out=gfb[:], in_=gf_d.partition_broadcast(128)), w=["gfb"])
        V(lambda e: e.memset(ss8v[:], 0.0), w=["ss"])
        for tt in range(16):
            b = tt % 2
            DS(lambda e, b=b, tt=tt: e.dma_start(out=xb8v[b][:], in_=acc_d[tt * 128:(tt + 1) * 128, :]), w=["xf%d" % b])
            rms_rstd_g(junk8v, ss8v, rs8v, xb8v[b][:], "xf%d" % b, tt, EPS6, D)
            V(lambda e, b=b, tt=tt: e.scalar_tensor_tensor(out=ob[b][:], in0=xb8v[b][:], scalar=rs8v[:, tt:tt + 1], in1=gfb[:],
                                                           op0=ALU.mult, op1=ALU.mult), r=["xf%d" % b, "rs", "gfb"], w=["ob%d" % b])
            fin.append(DS(lambda e, b=b, tt=tt: e.dma_start(out=out_d[tt * 128:(tt + 1) * 128, :], in_=ob[b][:]), r=["ob%d" % b]))
        P.emit(fin)
        return nc, dbg_outs


def host_consts():
    c = {}
    I = np.eye(128, dtype=np.float32)
    perm = np.zeros((128, 128), np.float32)
    for i in range(128):
        perm[(i + 64) % 128, i] = 1.0
    onesdiv = np.full((128, 128), 1.0 / 128, np.float32)
    tri = np.triu(np.ones((128, 128), np.float32), k=1)
    ones = np.ones((128, 128), np.float32)
    c["cst32"] = np.ascontiguousarray(np.stack([I, perm, onesdiv, tri, ones], axis=1))
    half = 64
    inv_freq = 1.0 / (10000.0 ** (np.arange(half, dtype=np.float32) * 2.0 / 128))
    ang = np.arange(L, dtype=np.float32)[:, None] * inv_freq[None, :]
    cos = np.cos(ang).astype(np.float32).T
    sin = np.sin(ang).astype(np.float32).T
    cosT = np.concatenate([cos, cos], axis=0)
    sinT = np.concatenate([-sin, sin], axis=0)
    c["rope"] = np.ascontiguousarray(np.stack([cosT, sinT], axis=1).astype(np.float32))
    H = 8
    gamma = 1.0 - np.exp2(-5.0 - np.arange(H, dtype=np.float32))
    log_g = np.log(gamma).astype(np.float32)
    idx = np.arange(128, dtype=np.float32)
    rel = idx[:, None] - idx[None, :]
    decay = np.where(rel >= 0, np.exp(log_g[:, None, None] * np.maximum(rel, 0.0)), 0.0)
    sc = 128.0 ** -0.5
    decT = (decay.transpose(2, 0, 1) * sc).astype(np.float32)
    c["decayT"] = np.ascontiguousarray(decT)
    zeta = np.exp(log_g[:, None] * (127.0 - idx)[None, :]).astype(np.float32)
    xi = np.exp(log_g[None, :] * (idx + 1.0)[:, None]).astype(np.float32)
    xiT = np.broadcast_to(xi.T[None, :, :], (128, 8, 128)).astype(np.float32)
    c["xiT"] = np.ascontiguousarray(xiT)
    col = np.zeros((128, 24), np.float32)
    p = np.arange(128)
    col[:, 0] = np.where(p < 64, -1.0, 1.0)
    col[:, 1] = np.where(p < 64, 1.0, -1.0)
    col[:, 2] = (p // 16) % 2
    col[:, 3] = 1.0 - col[:, 2]
    col[:, 4] = 1e-6
    col[:, 5] = 1e-5
    col[:, 6] = p
    col[:, 7] = math.pi / 2
    col[:, 8:16] = (zeta.T * sc)
    col[:, 16] = (p < 64)
    col[:, 17] = (p >= 64)
    col[:, 18] = -col[:, 16]
    col[:, 19] = -col[:, 17]
    c["colc"] = col
    q = np.arange(128)[:, None]
    cc = np.arange(32)[None, :]
    m32 = (((q % 32) // 16) == (cc // 16)).astype(np.float32)
    m4 = np.zeros((128, 4, 32), np.float32)
    for pr in range(4):
        m4[32 * pr:32 * pr + 32, pr, :] = m32[32 * pr:32 * pr + 32]
    c["mask4"] = m4
    rt = np.zeros((128, 16 + 64 + 32), np.float32)
    rt[:, 0:16] = np.arange(16)[None, :] * 128 + p[:, None]
    rt[:, 16:80] = (np.arange(64) * 128)[None, :]
    rt[:, 80:112] = np.arange(32)[None, :]
    c["rtc"] = rt
    return c


def host_layout(inp):
    f = lambda a: np.ascontiguousarray(np.asarray(a, dtype=np.float32))
    o = {}
    o["w_ada"] = f(inp["w_ada"][0])
    o["b_ada"] = f(inp["b_ada"][0][None, :])
    o["g_norm1"] = f(inp["g_norm1"][0][None, :])
    o["g_norm2"] = f(inp["g_norm2"][0][None, :])
    o["g_final"] = f(inp["g_final"][None, :])
    w_in = np.asarray(inp["w_in"][0])
    order = list(range(8))
    for s in range(4):
        for h in range(8):
            order.append(8 + s * 8 + h)
    wt = w_in.reshape(16, 128, 40, 128).transpose(2, 1, 0, 3)
    o["w_in_t"] = f(wt)
    o["w_glu"] = f(inp["w_glu"][0])
    o["w_out"] = f(inp["w_out"][0])
    beta = np.concatenate([np.asarray(inp["beta_ssm"][0]), np.asarray(inp["beta_ret"][0])])
    o["betaT"] = f(beta.reshape(16, 128).T)
    o["dT"] = f(np.asarray(inp["ssm_d"][0]).reshape(8, 128).T)
    o["wr"] = f(np.concatenate([np.asarray(inp["w_router_group"][0]), np.asarray(inp["w_router_expert"][0])], axis=1))
    o["br"] = f(np.concatenate([np.asarray(inp["b_router_group"][0]), np.asarray(inp["b_router_expert"][0])])[None, :])
    o["w_gate"] = f(inp["w_gate"][0]).reshape(32 * 1024, 2048)
    o["w_up"] = f(inp["w_up"][0]).reshape(32 * 1024, 2048)
    o["w_down"] = f(inp["w_down"][0]).reshape(32 * 1024, 2048)
    a_re = np.asarray(inp["ssm_a_re"][0]); a_im = np.asarray(inp["ssm_a_im"][0])
    ldt = np.asarray(inp["ssm_log_dt"][0])
    B_re = np.asarray(inp["ssm_b_re"][0]); B_im = np.asarray(inp["ssm_b_im"][0])
    C_re = np.asarray(inp["ssm_c_re"][0]); C_im = np.asarray(inp["ssm_c_im"][0])
    par = np.stack([a_re.T, a_im.T, np.broadcast_to(ldt[None, :], (64, 64))], axis=1)
    o["sl_par"] = f(np.concatenate([par, par], axis=0))
    Bre_p = B_re.transpose(1, 0, 2); Bim_p = B_im.transpose(1, 0, 2)
    V1 = np.concatenate([Bre_p, Bim_p], axis=0); V2 = np.concatenate([Bim_p, Bre_p], axis=0)
    o["sl_V"] = f(np.stack([V1, V2], axis=1))
    o["sl_C"] = f(np.concatenate([C_re.transpose(2, 0, 1), C_im.transpose(2, 0, 1)], axis=0))
    def pl(arr_gp):
        return arr_gp.reshape(32, 2, 64).transpose(1, 2, 0).reshape(128, 32)
    ldt_gp = np.broadcast_to(ldt[:, None], (64, 64))
    o["pl_par"] = f(np.stack([pl(a_re), pl(a_im), pl(ldt_gp)], axis=1))
    def plC(c_ghp):
        return c_ghp.reshape(32, 2, 16, 64).transpose(1, 3, 0, 2).reshape(128, 32, 16)
    o["pl_C"] = f(np.stack([plC(C_re), plC(C_im)], axis=1))
    def tl_gp(arr_gp):
        a = arr_gp.reshape(8, 8, 64)
        a = np.broadcast_to(a[:, :, None, :], (8, 8, 16, 64)).transpose(1, 2, 0, 3).reshape(128, 8 * 64)
        return a
    o["tl_par"] = f(np.stack([tl_gp(a_re), tl_gp(a_im), tl_gp(ldt_gp)], axis=1))
    def tl_B(b_gph):
        a = b_gph.reshape(8, 8, 64, 16).transpose(1, 3, 0, 2).reshape(128, 8 * 64)
        return a
    o["tl_B"] = f(np.stack([tl_B(B_re), tl_B(B_im)], axis=1))
    return o


_CACHE = {}


def kernel(**inputs):
    x = np.asarray(inputs["x"], dtype=np.float32)
    c = np.asarray(inputs["c"], dtype=np.float32)
    shared = host_layout(inputs)
    shared.update(host_consts())
    if "nc" not in _CACHE:
        _CACHE["nc"] = build_nc()[0]
    nc = _CACHE["nc"]
    in_maps = []
    for b in range(NCORES):
        m = dict(shared)
        m["x"] = np.ascontiguousarray(x[b])
        m["cT"] = np.ascontiguousarray(c[b].reshape(16, 128).T)
        in_maps.append(m)
    res = run_bass_kernel_spmd(nc, in_maps, core_ids=list(range(NCORES)))
    return np.stack([np.asarray(r["out"], dtype=np.float32) for r in res.results], axis=0)
```

```python
import math
import os
from contextlib import ExitStack

import numpy as np
import concourse.bass as bass
import concourse.mybir as mybir
from concourse.bass_utils import run_bass_kernel_spmd

F32 = mybir.dt.float32
BF16 = mybir.dt.bfloat16
I32 = mybir.dt.int32
ALU = mybir.AluOpType
AF = mybir.ActivationFunctionType
AX = mybir.AxisListType

D = 2048
L = 2048
NCORES = 8
COMPUTE = ("pe", "act", "dve", "pool")
NDMA_SEMS = 28
SB_BASE = 16640
SB_LIMIT = 229376


class Prog:
    def __init__(self, nc):
        self.nc = nc
        self.ops = []

    def op(self, eng, fn, reads=(), writes=()):
        self.ops.append(dict(kind="c", eng=eng, fn=fn, reads=tuple(reads), writes=tuple(writes), bar=False))
        return len(self.ops) - 1

    def dma(self, q, fn, reads=(), writes=()):
        self.ops.append(dict(kind="d", eng=q, fn=fn, reads=tuple(reads), writes=tuple(writes), bar=False))
        return len(self.ops) - 1

    def barrier(self, fn):
        self.ops.append(dict(kind="c", eng="dve", fn=fn, reads=(), writes=(), bar=True))
        return len(self.ops) - 1

    def _analyze(self, final):
        ops = self.ops
        last_w, readers = {}, {}
        last_on = {}
        dmas_since = []
        pending_bar = {}
        for i, o in enumerate(ops):
            deps = set()
            raw = set()
            if o["bar"]:
                for e, j in last_on.items():
                    deps.add(j)
                deps.update(dmas_since)
                dmas_since = []
                for e in ("pe", "act", "dve", "pool", "sp"):
                    pending_bar[e] = i
                pending_bar.pop("dve", None)
                last_w, readers = {}, {}
            else:
                for b in o["reads"]:
                    if b in last_w:
                        deps.add(last_w[b])
                        raw.add(last_w[b])
                for b in o["writes"]:
                    if b in last_w:
                        deps.add(last_w[b])
                    deps.update(readers.get(b, ()))
                if o["eng"] in pending_bar:
                    deps.add(pending_bar.pop(o["eng"]))
            deps.discard(i)
            raw.discard(i)
            o["deps"] = deps
            o["raw"] = raw
            for b in o["reads"]:
                readers.setdefault(b, []).append(i)
            for b in o["writes"]:
                last_w[b] = i
                readers[b] = []
            if o["kind"] == "c":
                last_on[o["eng"]] = i
            else:
                dmas_since.append(i)
        for o in ops:
            o["signal"] = False
        for d in final:
            ops[d]["signal"] = True
        for i, o in enumerate(ops):
            for d in o["deps"]:
                p = ops[d]
                if p["kind"] == "c" and (p["eng"] != o["eng"] or o["kind"] == "d"
                                         or (d in o["raw"] and p["eng"] != "pe")):
                    p["signal"] = True
        cnt = {e: 0 for e in COMPUTE}
        dcnt = [0] * NDMA_SEMS
        nd = 0
        for o in ops:
            if o["kind"] == "c":
                if o["signal"]:
                    cnt[o["eng"]] += 1
                    o["sigval"] = cnt[o["eng"]]
            else:
                s = nd % NDMA_SEMS
                nd += 1
                o["dsem"] = s
                o["dprev"] = dcnt[s] * 16
                dcnt[s] += 1
                o["sigval"] = dcnt[s] * 16

    def emit(self, final):
        nc = self.nc
        self._analyze(final)
        ops = self.ops
        with ExitStack() as es:
            esem = {e: es.enter_context(nc.semaphore("s_" + e)) for e in COMPUTE}
            dsem = [es.enter_context(nc.semaphore("d_%d" % i)) for i in range(NDMA_SEMS)]
            block = es.enter_context(nc.Block())

            def run(engname, engobj):
                seen = {}

                def wait(key, sem, val):
                    if seen.get(key, 0) >= val:
                        return
                    seen[key] = val
                    engobj.wait_ge(sem, val)

                def wait_on(p):
                    if p["kind"] == "c":
                        wait(("c", p["eng"]), esem[p["eng"]], p["sigval"])
                    else:
                        wait(("d", p["dsem"]), dsem[p["dsem"]], p["sigval"])

                for i, o in enumerate(ops):
                    if o["eng"] != engname:
                        continue
                    for d in sorted(o["deps"]):
                        p = ops[d]
                        if p["kind"] == "c" and p["eng"] == engname and o["kind"] == "c":
                            if engname == "pe" or d not in o["raw"]:
                                continue
                        wait_on(p)
                    if o["kind"] == "d":
                        if o["dprev"] > 0:
                            wait(("d", o["dsem"]), dsem[o["dsem"]], o["dprev"])
                        o["fn"](engobj).then_inc(dsem[o["dsem"]], 16)
                    else:
                        ins = o["fn"](engobj)
                        if o["signal"]:
                            ins.then_inc(esem[engname], 1)
                if engname == "sp":
                    for d in final:
                        wait_on(ops[d])

            block.sync(lambda e: run("sp", e))
            block.scalar(lambda e: run("act", e))
            block.vector(lambda e: run("dve", e))
            block.gpsimd(lambda e: run("pool", e))
            block.tensor(lambda e: run("pe", e))


class Arena:
    def __init__(self, nc):
        self.nc = nc
        self.off = SB_BASE
        self.n = 0

    def alloc(self, name, shape, dt):
        nb = {F32: 4, BF16: 2, I32: 4}[dt]
        per = nb
        for s in shape[1:]:
            per *= s
        per = (per + 31) // 32 * 32
        assert self.off + per <= SB_LIMIT, (name, self.off, per)
        self.n += 1
        t = self.nc.alloc_sbuf_tensor_at("%s_%d" % (name, self.n), list(shape), dt, offset=self.off)
        self.off += per
        return t

    def mark(self):
        return self.off

    def release(self, m):
        self.off = m


def build_nc(dbg=None):
    nc = bass.Bass("TRN2", target_bir_lowering=False)
    P = Prog(nc)
    AR = Arena(nc)
    es = ExitStack()
    fin = []

    def din(name, shape, dt=F32):
        return nc.dram_tensor(name, list(shape), dt, kind="ExternalInput").ap()

    def dscr(name, shape, dt=F32):
        return nc.dram_tensor(name, list(shape), dt, kind="Internal").ap()

    def dout(name, shape, dt=F32):
        return nc.dram_tensor(name, list(shape), dt, kind="ExternalOutput").ap()

    x_d = din("x", [L, D])
    cT_d = din("cT", [128, 16])
    wada_d = din("w_ada", [D, 6 * D])
    bada_d = din("b_ada", [1, 6 * D])
    g1_d = din("g_norm1", [1, D])
    g2_d = din("g_norm2", [1, D])
    gf_d = din("g_final", [1, D])
    win_d = din("w_in_t", [40, 128, 16, 128])
    wglu_d = din("w_glu", [1024, 1024])
    wout_d = din("w_out", [D, D])
    betaT_d = din("betaT", [128, 16])
    dT_d = din("dT", [128, 8])
    wr_d = din("wr", [D, 36])
    br_d = din("br", [1, 36])
    NE = 32 if dbg is None or dbg in ("moe", "final") else 1
    wg_d = din("w_gate", [NE * 1024, 2048])
    wu_d = din("w_up", [NE * 1024, 2048])
    wd_d = din("w_down", [NE * 1024, 2048])
    slpar_d = din("sl_par", [128, 3, 64])
    slV_d = din("sl_V", [128, 2, 64, 16])
    slC_d = din("sl_C", [128, 64, 16])
    plpar_d = din("pl_par", [128, 3, 32])
    plC_d = din("pl_C", [128, 2, 32, 16])
    tlpar_d = din("tl_par", [128, 3, 512])
    tlB_d = din("tl_B", [128, 2, 512])
    cst_d = din("cst32", [128, 5, 128])
    rope_d = din("rope", [128, 2, L])
    decay_d = din("decayT", [128, 8, 128])
    xi_d = din("xiT", [128, 8, 128])
    col_d = din("colc", [128, 24])
    mask4_d = din("mask4", [128, 4, 32])
    rt_d = din("rtc", [128, 16 + 64 + 32])

    out_d = dout("out", [L, D])
    mod_d = dscr("mod_d", [1, 6 * D])
    uT_d = dscr("uT_d", [8, 128, L], BF16)
    acc_d = dscr("acc_d", [L + 1, D])
    h2_d = dscr("h2_d", [L + 1, D], BF16)
    slot_d = dscr("slot_d", [8192, 1])
    ti_d = dscr("ti_d", [L + 1, 4])

    dbg_outs = {}

    def dbg_out(name, shape, dt=F32):
        dbg_outs[name] = dout("dbg_" + name, shape, dt)
        return dbg_outs[name]

    with es:
        ps = [es.enter_context(nc.psum_tensor("ps%d" % i, [128, 512], F32)) for i in range(8)]
        PSK = ["ps%d" % i for i in range(8)]

        def V(fn, r=(), w=()):
            return P.op("dve", fn, r, w)

        def A(fn, r=(), w=()):
            return P.op("act", fn, r, w)

        def G(fn, r=(), w=()):
            return P.op("pool", fn, r, w)

        def T(fn, r=(), w=()):
            return P.op("pe", fn, r, w)

        def DS(fn, r=(), w=()):
            return P.dma("sp", fn, r, w)

        def DG(fn, r=(), w=()):
            return P.dma("pool", fn, r, w)

        def phase_barrier(tag):
            sc = barc
            P.barrier(lambda e: e.memset(sc[:], 0.0))

        def dump(name, t, shape, dt, keys):
            fin.append(DS(lambda e: e.dma_start(out=dbg_out(name, shape, dt), in_=t[:]), r=keys))
            P.emit(fin)
            return nc, dbg_outs

        barc = AR.alloc("barc", [128, 8], F32)
        cst = AR.alloc("cst", [128, 5, 128], F32)
        colc = AR.alloc("colc", [128, 24], F32)
        id16 = AR.alloc("id16", [128, 128], BF16)
        ones16 = AR.alloc("ones16", [128, 128], BF16)
        DS(lambda e: e.dma_start(out=cst[:], in_=cst_d), w=["cst"])
        DS(lambda e: e.dma_start(out=colc[:], in_=col_d), w=["colc"])
        V(lambda e: e.tensor_copy(id16[:], cst[:, 0, :]), r=["cst"], w=["id16"])
        V(lambda e: e.tensor_copy(ones16[:], cst[:, 4, :]), r=["cst"], w=["ones16"])
        ident32 = cst[:, 0, :]
        perm32 = cst[:, 1, :]
        onesdiv32 = cst[:, 2, :]
        tri32 = cst[:, 3, :]
        ones32 = cst[:, 4, :]
        SGN, SGNC, PAR, NPAR, EPS6, EPS5, PIDX, HALFPI = [colc[:, i:i + 1] for i in range(8)]
        base_mark = AR.mark()

        m0 = AR.mark()
        cT = AR.alloc("cT", [128, 16], F32)
        scT = AR.alloc("scT", [128, 16], F32)
        bada = AR.alloc("bada", [1, 6 * D], F32)
        wa = [AR.alloc("wa%d" % i, [128, 16, 512], BF16) for i in range(4)]
        scT16 = AR.alloc("scT16", [128, 16], BF16)
        modrow = [AR.alloc("modrow%d" % i, [1, 512], F32) for i in range(2)]
        DS(lambda e: e.dma_start(out=cT[:], in_=cT_d), w=["cT"])
        DS(lambda e: e.dma_start(out=bada[:], in_=bada_d), w=["bada"])
        A(lambda e: e.activation(scT[:], cT[:], AF.Silu), r=["cT"], w=["scT"])
        V(lambda e: e.tensor_copy(scT16[:], scT[:]), r=["scT"], w=["scT16"])
        wada_v = wada_d.rearrange("(k p) n -> p k n", p=128)
        for nb in range(24):
            b = nb % 2
            wbi = nb % 4
            DG(lambda e, wbi=wbi, nb=nb: e.dma_start(out=wa[wbi][:], in_=wada_v[:, :, nb * 512:(nb + 1) * 512]), w=["wa%d" % wbi])
            for k in range(16):
                T(lambda e, b=b, k=k, wbi=wbi: e.matmul(ps[b][0:1, :], scT16[:, k:k + 1], wa[wbi][:, k, :], start=(k == 0), stop=(k == 15)),
                  r=["scT16", "wa%d" % wbi], w=[PSK[b]])
            V(lambda e, b=b, nb=nb: e.tensor_tensor(modrow[b][:], ps[b][0:1, :], bada[0:1, nb * 512:(nb + 1) * 512], ALU.add),
              r=[PSK[b], "bada"], w=["modrow%d" % b])
            DS(lambda e, b=b, nb=nb: e.dma_start(out=mod_d[0:1, nb * 512:(nb + 1) * 512], in_=modrow[b][:]),
               r=["modrow%d" % b], w=["mod_d"])
        if dbg == "mod":
            t = AR.alloc("dbgm", [1, 6 * D], F32)
            DS(lambda e: e.dma_start(out=t[:], in_=mod_d), r=["mod_d"], w=["dbgm"])
            fin.append(DS(lambda e: e.dma_start(out=dbg_out("mod", [1, 6 * D]), in_=t[:]), r=["dbgm"]))
            P.emit(fin)
            return nc, dbg_outs
        phase_barrier("p0")
        AR.release(m0)

        def mod_bcast(dst, idx, key):
            return DS(lambda e: e.dma_start(out=dst[:], in_=mod_d[0:1, idx * D:(idx + 1) * D].partition_broadcast(128)),
                      r=["mod_d"], w=[key])

        yretT = AR.alloc("yretT", [128, 8, L], BF16)
        mh = AR.mark()
        hT = AR.alloc("hT", [128, 16, L], BF16)
        m1 = AR.mark()
        A1b = AR.alloc("A1b", [128, D], F32)
        B1b = AR.alloc("B1b", [128, D], F32)
        g1b = AR.alloc("g1b", [128, D], F32)
        xb = [AR.alloc("xb%d" % i, [128, D], F32) for i in range(2)]
        junk = AR.alloc("junk", [128, D], F32)
        hf = AR.alloc("hf", [128, D], F32)
        h16 = [AR.alloc("h16_%d" % i, [128, D], BF16) for i in range(2)]
        ss = AR.alloc("ss", [128, 16], F32)
        rs = AR.alloc("rs", [128, 16], F32)
        mod_bcast(A1b, 1, "A1b")
        mod_bcast(B1b, 0, "B1b")
        DS(lambda e: e.dma_start(out=g1b[:], in_=g1_d.partition_broadcast(128)), w=["g1b"])
        V(lambda e: e.scalar_tensor_tensor(out=A1b[:], in0=A1b[:], scalar=1.0, in1=g1b[:], op0=ALU.add, op1=ALU.mult),
          r=["A1b", "g1b"], w=["A1b"])
        V(lambda e: e.memset(ss[:], 0.0), w=["ss"])

        def rms_rstd_g(junk, ss, rs, xt_ap, xkey, col, eps_ap, n):
            A(lambda e: e.activation(junk[:], xt_ap, AF.Square, accum_out=ss[:, col:col + 1]), r=[xkey, "ss"], w=["junk", "ss"])
            A(lambda e: e.activation(rs[:, col:col + 1], ss[:, col:col + 1], AF.Sqrt, bias=eps_ap, scale=1.0 / n),
              r=["ss", "colc"], w=["rs"])
            V(lambda e: e.reciprocal(rs[:, col:col + 1], rs[:, col:col + 1]), r=["rs"], w=["rs"])

        def rms_rstd(xt_ap, xkey, col, eps_ap, n):
            A(lambda e: e.activation(junk[:], xt_ap, AF.Square, accum_out=ss[:, col:col + 1]), r=[xkey, "ss"], w=["junk", "ss"])
            A(lambda e: e.activation(rs[:, col:col + 1], ss[:, col:col + 1], AF.Sqrt, bias=eps_ap, scale=1.0 / n),
              r=["ss", "colc"], w=["rs"])
            V(lambda e: e.reciprocal(rs[:, col:col + 1], rs[:, col:col + 1]), r=["rs"], w=["rs"])

        for tt in range(16):
            b = tt % 2
            DS(lambda e, b=b, tt=tt: e.dma_start(out=xb[b][:], in_=x_d[tt * 128:(tt + 1) * 128, :]), w=["xb%d" % b])
            rms_rstd_g(junk, ss, rs, xb[b][:], "xb%d" % b, tt, EPS6, D)
            V(lambda e, b=b, tt=tt: e.scalar_tensor_tensor(out=hf[:], in0=xb[b][:], scalar=rs[:, tt:tt + 1], in1=A1b[:],
                                                           op0=ALU.mult, op1=ALU.mult), r=["xb%d" % b, "rs", "A1b"], w=["hf"])
            V(lambda e, b=b: e.tensor_tensor(h16[b][:], hf[:], B1b[:], ALU.add), r=["hf", "B1b"], w=["h16_%d" % b])
            for kg in range(4):
                pb = 2 + (tt * 4 + kg) % 4
                for kk in range(4):
                    k = kg * 4 + kk
                    T(lambda e, pb=pb, kk=kk, k=k, b=b: e.matmul(ps[pb][:, kk * 128:(kk + 1) * 128], h16[b][:, k * 128:(k + 1) * 128],
                                                                 id16[:], start=True, stop=True),
                      r=["h16_%d" % b, "id16"], w=[PSK[pb]])
                ev = A if kg % 2 == 0 else V
                if kg % 2 == 0:
                    A(lambda e, pb=pb, kg=kg, tt=tt: e.copy(hT[:, kg * 4:(kg + 1) * 4, tt * 128:(tt + 1) * 128],
                                                           ps[pb][:].rearrange("p (a c) -> p a c", a=4)),
                      r=[PSK[pb]], w=["hT%d" % tt])
                else:
                    V(lambda e, pb=pb, kg=kg, tt=tt: e.tensor_copy(hT[:, kg * 4:(kg + 1) * 4, tt * 128:(tt + 1) * 128],
                                                                  ps[pb][:].rearrange("p (a c) -> p a c", a=4)),
                      r=[PSK[pb]], w=["hT%d" % tt])
        if dbg == "hT":
            t = AR.alloc("dbgh", [128, 16, L], F32)
            V(lambda e: e.tensor_copy(t[:], hT[:]), r=["hT%d" % i for i in range(16)], w=["dbgh"])
            fin.append(DS(lambda e: e.dma_start(out=dbg_out("hT", [128, 16, L]), in_=t[:]), r=["dbgh"]))
            P.emit(fin)
            return nc, dbg_outs
        phase_barrier("p1")
        AR.release(m1)

        m2 = AR.mark()
        win = [AR.alloc("win%d" % i, [128, 16, 128], BF16) for i in range(6)]
        wctr = [0]

        def load_win(tile_idx):
            i = wctr[0] % 6
            wctr[0] += 1
            DG(lambda e, i=i, tile_idx=tile_idx: e.dma_start(out=win[i][:], in_=win_d[tile_idx]), w=["win%d" % i])
            return i

        HTK = ["hT%d" % i for i in range(16)]
        pctr = [0]

        def next_ps():
            pctr[0] += 1
            return pctr[0] % 8

        def inproj_block(wi, tb, pb):
            for k in range(16):
                T(lambda e, wi=wi, tb=tb, pb=pb, k=k: e.matmul(ps[pb][:], win[wi][:, k, :], hT[:, k, tb * 512:(tb + 1) * 512],
                                                               start=(k == 0), stop=(k == 15)),
                  r=["win%d" % wi] + HTK[tb * 4:tb * 4 + 4], w=[PSK[pb]])

        ut = [AR.alloc("ut%d" % i, [128, L], BF16) for i in range(2)]
        for j in range(8):
            wi = load_win(j)
            ub = j % 2
            for tb in range(4):
                pb = next_ps()
                inproj_block(wi, tb, pb)
                ev = A if tb % 2 == 0 else V
                if tb % 2 == 0:
                    A(lambda e, pb=pb, ub=ub, tb=tb: e.copy(ut[ub][:, tb * 512:(tb + 1) * 512], ps[pb][:]), r=[PSK[pb]], w=["ut%d" % ub])
                else:
                    V(lambda e, pb=pb, ub=ub, tb=tb: e.tensor_copy(ut[ub][:, tb * 512:(tb + 1) * 512], ps[pb][:]), r=[PSK[pb]], w=["ut%d" % ub])
            DS(lambda e, ub=ub, j=j: e.dma_start(out=uT_d[j], in_=ut[ub][:]), r=["ut%d" % ub], w=["uT_d"])
        if dbg == "uT":
            t = AR.alloc("dbgu", [128, 8, L], BF16)
            t2 = AR.alloc("dbgu2", [128, 8, L], F32)
            DS(lambda e: e.dma_start(out=t[:], in_=uT_d.rearrange("j p t -> p j t")), r=["uT_d"], w=["dbgu"])
            V(lambda e: e.tensor_copy(t2[:], t[:]), r=["dbgu"], w=["dbgu2"])
            fin.append(DS(lambda e: e.dma_start(out=dbg_out("uT", [128, 8, L]), in_=t2[:]), r=["dbgu2"]))
            P.emit(fin)
            return nc, dbg_outs

        rope = AR.alloc("rope", [128, 2, L], F32)
        decT = AR.alloc("decT", [128, 8, 128], F32)
        xiT = AR.alloc("xiT", [128, 8, 128], F32)
        betaT = AR.alloc("betaT", [128, 16], F32)
        DS(lambda e: e.dma_start(out=rope[:], in_=rope_d), w=["rope"])
        DS(lambda e: e.dma_start(out=decT[:], in_=decay_d), w=["decT"])
        DS(lambda e: e.dma_start(out=xiT[:], in_=xi_d), w=["xiT"])
        DS(lambda e: e.dma_start(out=betaT[:], in_=betaT_d), w=["betaT"])
        qr = AR.alloc("qr", [128, L], BF16)
        kr = AR.alloc("kr", [128, L], BF16)
        qxi = AR.alloc("qxi", [128, L], BF16)
        vtok = AR.alloc("vtok", [128, 16, 128], BF16)
        kz = AR.alloc("kz", [128, 16, 128], BF16)
        gs = AR.alloc("gs", [128, 512], F32)
        qf = [AR.alloc("qf%d" % i, [128, 512], F32) for i in range(2)]
        t1 = [AR.alloc("t1_%d" % i, [128, 512], F32) for i in range(2)]
        t2 = [AR.alloc("t2_%d" % i, [128, 512], F32) for i in range(2)]
        vT16 = AR.alloc("vT16", [128, 512], BF16)
        Sd = [AR.alloc("Sd%d" % i, [128, 128], BF16) for i in range(2)]
        Rf = AR.alloc("Rf", [128, 128], F32)
        Rb = [AR.alloc("Rb%d" % i, [128, 128], BF16) for i in range(2)]
        yh = AR.alloc("yh", [128, 512], F32)
        ysq = AR.alloc("ysq", [128, 512], F32)
        mean = AR.alloc("mean", [128, 512], F32)
        var = AR.alloc("var", [128, 512], F32)
        yc = AR.alloc("yc", [128, 512], F32)
        gC = [float((1.0 - 2.0 ** (-5.0 - h)) ** 128) for h in range(8)]

        for hh in range(8):
            wq, wk, wv, wgt = [load_win(8 + s * 8 + hh) for s in range(4)]
            V(lambda e: e.memset(Rf[:], 0.0), w=["Rf"])
            for tb in range(4):
                sl = slice(tb * 512, (tb + 1) * 512)
                for which, wi, dst, dkey in (("q", wq, qr, "qr"), ("k", wk, kr, "kr")):
                    b2 = 0 if which == "q" else 1
                    pb = next_ps()
                    inproj_block(wi, tb, pb)
                    A(lambda e, pb=pb, b2=b2: e.copy(qf[b2][:], ps[pb][:]), r=[PSK[pb]], w=["qf%d" % b2])
                    pb2 = next_ps()
                    T(lambda e, pb2=pb2, b2=b2: e.matmul(ps[pb2][:], perm32, qf[b2][:], start=True, stop=True),
                      r=["cst", "qf%d" % b2], w=[PSK[pb2]])
                    G(lambda e, b2=b2, sl=sl: e.tensor_tensor(t1[b2][:], qf[b2][:], rope[:, 0, sl], ALU.mult),
                      r=["qf%d" % b2, "rope"], w=["t1_%d" % b2])
                    V(lambda e, pb2=pb2, b2=b2, sl=sl: e.tensor_tensor(t2[b2][:], ps[pb2][:], rope[:, 1, sl], ALU.mult),
                      r=[PSK[pb2], "rope"], w=["t2_%d" % b2])
                    V(lambda e, b2=b2: e.tensor_tensor(t1[b2][:], t1[b2][:], t2[b2][:], ALU.add),
                      r=["t1_%d" % b2, "t2_%d" % b2], w=["t1_%d" % b2])
                    A(lambda e, b2=b2, dst=dst, sl=sl: e.copy(dst[:, sl], t1[b2][:]), r=["t1_%d" % b2], w=[dkey])
                    if which == "q":
                        V(lambda e, hh=hh, sl=sl: e.tensor_tensor(qxi[:, sl].rearrange("p (a c) -> p a c", a=4),
                                                                  t1[0][:].rearrange("p (a c) -> p a c", a=4),
                                                                  xiT[:, hh, :].unsqueeze(1).to_broadcast([128, 4, 128]), ALU.mult),
                          r=["t1_0", "xiT"], w=["qxi"])
                pb = next_ps()
                inproj_block(wv, tb, pb)
                V(lambda e, pb=pb: e.tensor_copy(vT16[:], ps[pb][:]), r=[PSK[pb]], w=["vT16"])
                pb = next_ps()
                for c in range(4):
                    T(lambda e, pb=pb, c=c: e.matmul(ps[pb][:, c * 128:(c + 1) * 128], vT16[:, c * 128:(c + 1) * 128], id16[:],
                                                     start=True, stop=True), r=["vT16", "id16"], w=[PSK[pb]])
                A(lambda e, pb=pb, tb=tb: e.copy(vtok[:, tb * 4:(tb + 1) * 4, :], ps[pb][:].rearrange("p (a c) -> p a c", a=4)),
                  r=[PSK[pb]], w=["vtok"])
                pb = next_ps()
                for c in range(4):
                    T(lambda e, pb=pb, c=c, tb=tb: e.matmul(ps[pb][:, c * 128:(c + 1) * 128], kr[:, tb * 512 + c * 128: tb * 512 + (c + 1) * 128],
                                                            id16[:], start=True, stop=True), r=["kr", "id16"], w=[PSK[pb]])
                V(lambda e, pb=pb, tb=tb, hh=hh: e.tensor_scalar(kz[:, tb * 4:(tb + 1) * 4, :], ps[pb][:].rearrange("p (a c) -> p a c", a=4),
                                                                 colc[:, 8 + hh:9 + hh], None, ALU.mult),
                  r=[PSK[pb], "colc"], w=["kz"])
                pb = next_ps()
                inproj_block(wgt, tb, pb)
                A(lambda e, pb=pb: e.activation(gs[:], ps[pb][:], AF.Silu), r=[PSK[pb]], w=["gs"])
                pbo = next_ps()
                for c in range(4):
                    n = tb * 4 + c
                    cs = slice(n * 128, (n + 1) * 128)
                    pS = next_ps()
                    while pS == pbo:
                        pS = next_ps()
                    T(lambda e, pS=pS, cs=cs: e.matmul(ps[pS][:, 0:128], kr[:, cs], qr[:, cs], start=True, stop=True),
                      r=["kr", "qr"], w=[PSK[pS]])
                    T(lambda e, pS=pS, n=n: e.matmul(ps[pS][:, 128:256], kz[:, n, :], vtok[:, n, :], start=True, stop=True),
                      r=["kz", "vtok"], w=[PSK[pS]])
                    sb_i = n % 2
                    V(lambda e, pS=pS, sb_i=sb_i, hh=hh: e.tensor_tensor(Sd[sb_i][:], ps[pS][:, 0:128], decT[:, hh, :], ALU.mult),
                      r=[PSK[pS], "decT"], w=["Sd%d" % sb_i])
                    T(lambda e, pbo=pbo, c=c, n=n, sb_i=sb_i: e.matmul(ps[pbo][:, c * 128:(c + 1) * 128], vtok[:, n, :], Sd[sb_i][:],
                                                                      start=True, stop=(n == 0)),
                      r=["vtok", "Sd%d" % sb_i], w=[PSK[pbo]])
                    if n > 0:
                        T(lambda e, pbo=pbo, c=c, cs=cs, n=n: e.matmul(ps[pbo][:, c * 128:(c + 1) * 128], Rb[n % 2][:], qxi[:, cs],
                                                                      start=False, stop=True),
                          r=["Rb%d" % (n % 2), "qxi"], w=[PSK[pbo]])
                    V(lambda e, pS=pS, hh=hh: e.scalar_tensor_tensor(out=Rf[:], in0=Rf[:], scalar=gC[hh], in1=ps[pS][:, 128:256],
                                                                     op0=ALU.mult, op1=ALU.add), r=["Rf", PSK[pS]], w=["Rf"])
                    A(lambda e, n=n: e.copy(Rb[(n + 1) % 2][:], Rf[:]), r=["Rf"], w=["Rb%d" % ((n + 1) % 2)])
                A(lambda e, pbo=pbo: e.copy(yh[:], ps[pbo][:]), r=[PSK[pbo]], w=["yh"])
                A(lambda e: e.activation(ysq[:], yh[:], AF.Square), r=["yh"], w=["ysq"])
                pm = next_ps()
                pq = next_ps()
                T(lambda e, pm=pm: e.matmul(ps[pm][:], onesdiv32, yh[:], start=True, stop=True), r=["cst", "yh"], w=[PSK[pm]])
                T(lambda e, pq=pq: e.matmul(ps[pq][:], onesdiv32, ysq[:], start=True, stop=True), r=["cst", "ysq"], w=[PSK[pq]])
                A(lambda e, pm=pm: e.copy(mean[:], ps[pm][:]), r=[PSK[pm]], w=["mean"])
                V(lambda e: e.tensor_tensor(var[:], mean[:], mean[:], ALU.mult), r=["mean"], w=["var"])
                V(lambda e, pq=pq: e.tensor_tensor(var[:], ps[pq][:], var[:], ALU.subtract), r=[PSK[pq], "var"], w=["var"])
                A(lambda e: e.activation(var[:], var[:], AF.Sqrt, bias=EPS5, scale=1.0), r=["var", "colc"], w=["var"])
                V(lambda e: e.reciprocal(var[:], var[:]), r=["var"], w=["var"])
                V(lambda e: e.tensor_tensor(yc[:], yh[:], mean[:], ALU.subtract), r=["yh", "mean"], w=["yc"])
                V(lambda e: e.tensor_tensor(yc[:], yc[:], var[:], ALU.mult), r=["yc", "var"], w=["yc"])
                V(lambda e, hh=hh, sl=sl: e.scalar_tensor_tensor(out=yretT[:, hh, sl], in0=yc[:], scalar=betaT[:, 8 + hh:9 + hh], in1=gs[:],
                                                                 op0=ALU.mult, op1=ALU.mult), r=["yc", "betaT", "gs"], w=["yretT"])
        if dbg == "yret":
            fin.append(DS(lambda e: e.dma_start(out=dbg_out("yret", [128, 8, L], BF16), in_=yretT[:]), r=["yretT"]))
            P.emit(fin)
            return nc, dbg_outs
        phase_barrier("p2")
        AR.release(mh)
        TWO_PI = 2.0 * math.pi
        TOP, BOT, NTOP, NBOT = [colc[:, i:i + 1] for i in range(16, 20)]
        zT = AR.alloc("zT", [128, 8, L], BF16)
        BD = AR.alloc("BD", [128, 8, 8, 128], BF16)
        HC = AR.alloc("HC", [128, 32, 8, 2, 32], BF16)
        ArB = AR.alloc("ArB", [128, 2, 32], F32)
        AiB = AR.alloc("AiB", [128, 2, 32], F32)
        mask4 = AR.alloc("mask4", [128, 4, 32], F32)
        dT = AR.alloc("dT", [128, 8], F32)
        betaS = AR.alloc("betaS", [128, 16], F32)
        DS(lambda e: e.dma_start(out=mask4[:], in_=mask4_d), w=["mask4"])
        DS(lambda e: e.dma_start(out=dT[:], in_=dT_d), w=["dT"])
        DS(lambda e: e.dma_start(out=betaS[:], in_=betaT_d), w=["betaS"])
        ms = AR.mark()
        uid = [0]

        def tmp(shape, dt=F32):
            uid[0] += 1
            return AR.alloc("tmp%d" % uid[0], shape, dt), "tmp%d" % uid[0]

        def disc(par, pk, N, ks):
            K = len(ks)
            dt_, dtk = tmp([128, N]); lr, lrk = tmp([128, N]); li, lik = tmp([128, N])
            A(lambda e: e.activation(dt_[:], par[:, 2, :], AF.Exp), r=[pk], w=[dtk])
            V(lambda e: e.tensor_tensor(lr[:], dt_[:], par[:, 0, :], ALU.mult), r=[dtk, pk], w=[lrk])
            V(lambda e: e.tensor_tensor(li[:], dt_[:], par[:, 1, :], ALU.mult), r=[dtk, pk], w=[lik])
            MAG, mk = tmp([128, K, N]); ANG, ak = tmp([128, K, N]); TF, tfk = tmp([128, K, N]); TI, tik = tmp([128, K, N], I32)
            SN, snk = tmp([128, K, N]); CSN, csk = tmp([128, K, N])
            for i, k in enumerate(ks):
                A(lambda e, i=i, k=k: e.activation(MAG[:, i, :], lr[:], AF.Exp, scale=float(k)), r=[lrk], w=[mk])
                V(lambda e, i=i, k=k: e.tensor_scalar(ANG[:, i, :], li[:], float(k), None, ALU.mult), r=[lik], w=[ak])
            for shift, OUT, ok in ((0.0, SN, snk), (math.pi / 2, CSN, csk)):
                V(lambda e, shift=shift: e.tensor_scalar(TF[:], ANG[:], shift, 1.0 / TWO_PI, ALU.add, ALU.mult), r=[ak], w=[tfk])
                V(lambda e: e.tensor_copy(TI[:], TF[:]), r=[tfk], w=[tik])
                V(lambda e: e.tensor_copy(TF[:], TI[:]), r=[tik], w=[tfk])
                V(lambda e: e.scalar_tensor_tensor(out=TF[:], in0=TF[:], scalar=-TWO_PI, in1=ANG[:], op0=ALU.mult, op1=ALU.add),
                  r=[tfk, ak], w=[tfk])
                V(lambda e, shift=shift: e.tensor_scalar(TF[:], TF[:], shift, 3.1415925, ALU.add, ALU.min), r=[tfk], w=[tfk])
                V(lambda e: e.tensor_scalar(TF[:], TF[:], -3.1415925, None, ALU.max), r=[tfk], w=[tfk])
                A(lambda e, OUT=OUT: e.activation(OUT[:], TF[:], AF.Sin), r=[tfk], w=[ok])
            V(lambda e: e.tensor_tensor(CSN[:], CSN[:], MAG[:], ALU.mult), r=[csk, mk], w=[csk])
            V(lambda e: e.tensor_tensor(SN[:], SN[:], MAG[:], ALU.mult), r=[snk, mk], w=[snk])
            return (CSN, csk), (SN, snk)

        def zoh(par, pk, N, E1re, E1im, ek):
            nre, nk = tmp([128, N]); inv, ik = tmp([128, N]); fre, frk = tmp([128, N]); fim, fik = tmp([128, N]); tq, tqk = tmp([128, N])
            V(lambda e: e.tensor_scalar(nre[:], E1re, -1.0, None, ALU.add), r=ek, w=[nk])
            V(lambda e: e.tensor_tensor(inv[:], par[:, 0, :], par[:, 0, :], ALU.mult), r=[pk], w=[ik])
            V(lambda e: e.tensor_tensor(tq[:], par[:, 1, :], par[:, 1, :], ALU.mult), r=[pk], w=[tqk])
            V(lambda e: e.tensor_tensor(inv[:], inv[:], tq[:], ALU.add), r=[ik, tqk], w=[ik])
            V(lambda e: e.reciprocal(inv[:], inv[:]), r=[ik], w=[ik])
            V(lambda e: e.tensor_tensor(fre[:], nre[:], par[:, 0, :], ALU.mult), r=[nk, pk], w=[frk])
            V(lambda e: e.tensor_tensor(tq[:], E1im, par[:, 1, :], ALU.mult), r=ek + [pk, tqk], w=[tqk])
            V(lambda e: e.tensor_tensor(fre[:], fre[:], tq[:], ALU.add), r=[frk, tqk], w=[frk])
            V(lambda e: e.tensor_tensor(fre[:], fre[:], inv[:], ALU.mult), r=[frk, ik], w=[frk])
            V(lambda e: e.tensor_tensor(fim[:], E1im, par[:, 0, :], ALU.mult), r=ek + [pk], w=[fik])
            V(lambda e: e.tensor_tensor(tq[:], nre[:], par[:, 1, :], ALU.mult), r=[nk, pk, tqk], w=[tqk])
            V(lambda e: e.tensor_tensor(fim[:], fim[:], tq[:], ALU.subtract), r=[fik, tqk], w=[fik])
            V(lambda e: e.tensor_tensor(fim[:], fim[:], inv[:], ALU.mult), r=[fik, ik], w=[fik])
            return (fre, frk), (fim, fik)

        def _sl():
            slpar = AR.alloc("slpar", [128, 3, 64], F32)
            slV = AR.alloc("slV", [128, 2, 64, 16], F32)
            slC = AR.alloc("slC", [128, 64, 16], F32)
            DS(lambda e: e.dma_start(out=slpar[:], in_=slpar_d), w=["slpar"])
            DS(lambda e: e.dma_start(out=slV[:], in_=slV_d), w=["slV"])
            DS(lambda e: e.dma_start(out=slC[:], in_=slC_d), w=["slC"])
            (Ere, erk), (Eim, eik) = disc(slpar, "slpar", 64, list(range(8)))
            (fre, frk), (fim, fik) = zoh(slpar, "slpar", 64, Ere[:, 1, :], Eim[:, 1, :], [erk, eik])
            U1, u1k = tmp([128, 64, 16]); U2, u2k = tmp([128, 64, 16]); tA, tAk = tmp([128, 64, 16]); tB, tBk = tmp([128, 64, 16])
            PSb, psbk = tmp([128, 8, 64, 16], BF16); CSb, csbk = tmp([128, 64, 16], BF16)
            bc = lambda ap: ap.unsqueeze(2).to_broadcast([128, 64, 16])
            V(lambda e: e.tensor_tensor(tA[:], slV[:, 0], bc(fre[:]), ALU.mult), r=["slV", frk], w=[tAk])
            V(lambda e: e.tensor_tensor(tB[:], slV[:, 1], bc(fim[:]), ALU.mult), r=["slV", fik], w=[tBk])
            V(lambda e: e.scalar_tensor_tensor(out=U1[:], in0=tB[:], scalar=SGN, in1=tA[:], op0=ALU.mult, op1=ALU.add), r=[tAk, tBk, "colc"], w=[u1k])
            V(lambda e: e.tensor_tensor(tA[:], slV[:, 1], bc(fre[:]), ALU.mult), r=["slV", frk], w=[tAk])
            V(lambda e: e.tensor_tensor(tB[:], slV[:, 0], bc(fim[:]), ALU.mult), r=["slV", fik], w=[tBk])
            V(lambda e: e.scalar_tensor_tensor(out=U2[:], in0=tB[:], scalar=SGNC, in1=tA[:], op0=ALU.mult, op1=ALU.add), r=[tAk, tBk, "colc"], w=[u2k])
            for j in range(8):
                V(lambda e, j=j: e.tensor_tensor(tA[:], U1[:], bc(Ere[:, j, :]), ALU.mult), r=[u1k, erk], w=[tAk])
                V(lambda e, j=j: e.tensor_tensor(tB[:], U2[:], bc(Eim[:, j, :]), ALU.mult), r=[u2k, eik], w=[tBk])
                V(lambda e, j=j: e.scalar_tensor_tensor(out=PSb[:, j], in0=tB[:], scalar=SGN, in1=tA[:], op0=ALU.mult, op1=ALU.add),
                  r=[tAk, tBk, "colc"], w=[psbk])
            V(lambda e: e.tensor_scalar(CSb[:], slC[:], SGNC, None, ALU.mult), r=["slC", "colc"], w=[csbk])
            for jt in range(8):
                pb = next_ps()
                for pr in range(4):
                    g0 = 2 * (4 * jt + pr)
                    for j in range(8):
                        kw = dict(tile_position=(0, 96)) if pr == 3 else {}
                        T(lambda e, pb=pb, pr=pr, j=j, g0=g0, kw=kw: e.matmul(ps[pb][32 * pr:32 * pr + 32, j * 32:(j + 1) * 32],
                                                                            PSb[:, j, g0:g0 + 2, :], CSb[:, g0:g0 + 2, :],
                                                                            start=True, stop=True, **kw),
                          r=[psbk, csbk], w=[PSK[pb]])
                for pr in range(4):
                    V(lambda e, pb=pb, pr=pr, jt=jt: e.tensor_tensor(BD[:, jt, :, 32 * pr:32 * pr + 32],
                                                                     ps[pb][:, 0:256].rearrange("p (j c) -> p j c", j=8),
                                                                     mask4[:, pr, :].unsqueeze(1).to_broadcast([128, 8, 32]), ALU.mult),
                      r=[PSK[pb], "mask4"], w=["BD"])

        _sl()
        if dbg == "ssm_BD":
            return dump("BD", BD, [128, 8, 8, 128], BF16, ["BD"])
        phase_barrier("sl")
        AR.release(ms)

        def _pl():
            plpar = AR.alloc("plpar", [128, 3, 32], F32)
            plC = AR.alloc("plC", [128, 2, 32, 16], F32)
            DS(lambda e: e.dma_start(out=plpar[:], in_=plpar_d), w=["plpar"])
            DS(lambda e: e.dma_start(out=plC[:], in_=plC_d), w=["plC"])
            (Ere, erk), (Eim, eik) = disc(plpar, "plpar", 32, list(range(1, 9)))
            qa, qak = tmp([128, 32, 16]); qb, qbk = tmp([128, 32, 16]); Qre, qrk = tmp([128, 32, 16]); Qim, qik = tmp([128, 32, 16])
            bc2 = lambda ap: ap.unsqueeze(2).to_broadcast([128, 32, 16])
            for t in range(8):
                V(lambda e, t=t: e.tensor_tensor(qa[:], plC[:, 0], bc2(Ere[:, t, :]), ALU.mult), r=["plC", erk], w=[qak])
                V(lambda e, t=t: e.tensor_tensor(qb[:], plC[:, 1], bc2(Eim[:, t, :]), ALU.mult), r=["plC", eik], w=[qbk])
                V(lambda e: e.tensor_tensor(Qre[:], qa[:], qb[:], ALU.subtract), r=[qak, qbk], w=[qrk])
                V(lambda e, t=t: e.tensor_tensor(qa[:], plC[:, 0], bc2(Eim[:, t, :]), ALU.mult), r=["plC", eik], w=[qak])
                V(lambda e, t=t: e.tensor_tensor(qb[:], plC[:, 1], bc2(Ere[:, t, :]), ALU.mult), r=["plC", erk], w=[qbk])
                V(lambda e: e.tensor_tensor(Qim[:], qa[:], qb[:], ALU.add), r=[qak, qbk], w=[qik])
                V(lambda e, t=t: e.tensor_scalar(HC[:, :, t, 0, 0:16], Qre[:], TOP, None, ALU.mult), r=[qrk, "colc"], w=["HC"])
                V(lambda e, t=t: e.tensor_scalar(HC[:, :, t, 0, 16:32], Qre[:], BOT, None, ALU.mult), r=[qrk, "colc"], w=["HC"])
                V(lambda e, t=t: e.tensor_scalar(HC[:, :, t, 1, 0:16], Qim[:], NTOP, None, ALU.mult), r=[qik, "colc"], w=["HC"])
                V(lambda e, t=t: e.tensor_scalar(HC[:, :, t, 1, 16:32], Qim[:], NBOT, None, ALU.mult), r=[qik, "colc"], w=["HC"])
            for ri in range(2):
                V(lambda e, ri=ri: e.tensor_copy(ArB[:, ri, :], Ere[:, 7, :]), r=[erk], w=["ArB"])
                V(lambda e, ri=ri: e.tensor_copy(AiB[:, ri, :], Eim[:, 7, :]), r=[eik], w=["AiB"])

        _pl()
        if dbg == "ssm_HC":
            return dump("HC", HC, [128, 32, 8, 2, 32], BF16, ["HC"])
        phase_barrier("pl")
        AR.release(ms)

        PTre = AR.alloc("PTre", [128, 8, 512], F32)
        PTim = AR.alloc("PTim", [128, 8, 512], F32)
        mt2 = AR.mark()
        tlpar = AR.alloc("tlpar", [128, 3, 512], F32)
        tlB = AR.alloc("tlB", [128, 2, 512], F32)
        Bbre = AR.alloc("Bbre", [128, 512], F32)
        Bbim = AR.alloc("Bbim", [128, 512], F32)
        ta = AR.alloc("tla", [128, 512], F32)
        tb_ = AR.alloc("tlb", [128, 512], F32)
        DS(lambda e: e.dma_start(out=tlpar[:], in_=tlpar_d), w=["tlpar"])
        DS(lambda e: e.dma_start(out=tlB[:], in_=tlB_d), w=["tlB"])
        mt = AR.mark()

        def _tl0():
            (Ere, erk), (Eim, eik) = disc(tlpar, "tlpar", 512, [1])
            (fre, frk), (fim, fik) = zoh(tlpar, "tlpar", 512, Ere[:, 0, :], Eim[:, 0, :], [erk, eik])
            V(lambda e: e.tensor_tensor(ta[:], fre[:], tlB[:, 0, :], ALU.mult), r=[frk, "tlB"], w=["tla"])
            V(lambda e: e.tensor_tensor(tb_[:], fim[:], tlB[:, 1, :], ALU.mult), r=[fik, "tlB"], w=["tlb"])
            V(lambda e: e.tensor_tensor(Bbre[:], ta[:], tb_[:], ALU.subtract), r=["tla", "tlb"], w=["Bbre"])
            V(lambda e: e.tensor_tensor(ta[:], fre[:], tlB[:, 1, :], ALU.mult), r=[frk, "tlB"], w=["tla"])
            V(lambda e: e.tensor_tensor(tb_[:], fim[:], tlB[:, 0, :], ALU.mult), r=[fik, "tlB"], w=["tlb"])
            V(lambda e: e.tensor_tensor(Bbim[:], ta[:], tb_[:], ALU.add), r=["tla", "tlb"], w=["Bbim"])

        def _tltau(tau):
            (Ere, erk), (Eim, eik) = disc(tlpar, "tlpar", 512, [7 - tau])
            V(lambda e: e.tensor_tensor(ta[:], Ere[:, 0, :], Bbre[:], ALU.mult), r=[erk, "Bbre"], w=["tla"])
            V(lambda e: e.tensor_tensor(tb_[:], Eim[:, 0, :], Bbim[:], ALU.mult), r=[eik, "Bbim"], w=["tlb"])
            V(lambda e: e.tensor_tensor(PTre[:, tau, :], ta[:], tb_[:], ALU.subtract), r=["tla", "tlb"], w=["PTre"])
            V(lambda e: e.tensor_tensor(ta[:], Ere[:, 0, :], Bbim[:], ALU.mult), r=[erk, "Bbim"], w=["tla"])
            V(lambda e: e.tensor_tensor(tb_[:], Eim[:, 0, :], Bbre[:], ALU.mult), r=[eik, "Bbre"], w=["tlb"])
            V(lambda e: e.tensor_tensor(PTim[:, tau, :], ta[:], tb_[:], ALU.add), r=["tla", "tlb"], w=["PTim"])

        _tl0()
        for tau in range(8):
            phase_barrier("tl%d" % tau)
            AR.release(mt)
            _tltau(tau)
        if dbg == "ssm_PT":
            fin.append(DS(lambda e: e.dma_start(out=dbg_out("PTim", [128, 8, 512], F32), in_=PTim[:]), r=["PTim"]))
            return dump("PTre", PTre, [128, 8, 512], F32, ["PTre"])
        phase_barrier("tl")
        AR.release(mt2)
        XS = AR.alloc("XS", [128, 2, 256, 32], BF16)

        GT = [AR.alloc("GT%d" % i, [128, 8, 2, 128], BF16) for i in range(2)]
        ut3 = [AR.alloc("ut3_%d" % i, [128, L], BF16) for i in range(2)]
        for j in range(8):
            b = j % 2
            DS(lambda e, b=b, j=j: e.dma_start(out=ut3[b][:], in_=uT_d[j]), r=["uT_d"], w=["ut%d" % b])
            for ri, PT, ptk in ((0, PTre, "PTre"), (1, PTim, "PTim")):
                for gg, colsel in ((0, NPAR), (1, PAR)):
                    V(lambda e, b=b, j=j, ri=ri, gg=gg, PT=PT, colsel=colsel: e.tensor_scalar(
                        GT[b][:, :, ri, gg * 64:(gg + 1) * 64], PT[:, :, j * 64:(j + 1) * 64], colsel, None, ALU.mult),
                      r=[ptk, "colc"], w=["GT%d" % b])
            for pr in range(4):
                pb = next_ps()
                for ri in range(2):
                    for tau in range(8):
                        kw = dict(tile_position=(96, 0)) if pr == 3 else {}
                        T(lambda e, pb=pb, b=b, pr=pr, ri=ri, tau=tau, kw=kw: e.matmul(
                            ps[pb][:, ri * 256:(ri + 1) * 256], GT[b][32 * pr:32 * pr + 32, tau, ri, :], ut3[b][32 * pr:32 * pr + 32, tau::8],
                            start=(tau == 0), stop=(tau == 7), **kw),
                          r=["GT%d" % b, "ut%d" % b], w=[PSK[pb]])
                ev = A if pr % 2 == 0 else V
                if pr % 2 == 0:
                    A(lambda e, pb=pb, j=j, pr=pr: e.copy(XS[:, :, :, 4 * j + pr], ps[pb][:].rearrange("p (r n) -> p r n", r=2)),
                      r=[PSK[pb]], w=["XS"])
                else:
                    V(lambda e, pb=pb, j=j, pr=pr: e.tensor_copy(XS[:, :, :, 4 * j + pr], ps[pb][:].rearrange("p (r n) -> p r n", r=2)),
                      r=[PSK[pb]], w=["XS"])

        if dbg == "ssm_X":
            return dump("XS", XS, [128, 2, 256, 32], BF16, ["XS"])
        st = AR.alloc("st", [128, 2, 32], F32)
        T13 = AR.alloc("T13", [128, 2, 32], F32)
        T24 = AR.alloc("T24", [128, 2, 32], F32)
        V(lambda e: e.tensor_copy(st[:], XS[:, :, 0, :]), r=["XS"], w=["st"])
        for n in range(1, 255):
            V(lambda e: e.tensor_tensor(T13[:], st[:], ArB[:], ALU.mult), r=["st", "ArB"], w=["T13"])
            V(lambda e: e.tensor_tensor(T24[:], st[:], AiB[:], ALU.mult), r=["st", "AiB"], w=["T24"])
            V(lambda e: e.tensor_tensor(st[:, 0, :], T13[:, 0, :], T24[:, 1, :], ALU.subtract), r=["T13", "T24"], w=["st"])
            V(lambda e: e.tensor_tensor(st[:, 1, :], T13[:, 1, :], T24[:, 0, :], ALU.add), r=["T13", "T24"], w=["st"])
            V(lambda e, n=n: e.tensor_tensor(st[:], st[:], XS[:, :, n, :], ALU.add), r=["st", "XS"], w=["st"])
            V(lambda e, n=n: e.tensor_copy(XS[:, :, n, :], st[:]), r=["st"], w=["XS"])

        if dbg == "ssm_S":
            return dump("XS", XS, [128, 2, 256, 32], BF16, ["XS"])
        ytmp = [AR.alloc("ytmp%d" % i, [128, 256], F32) for i in range(2)]
        yin = [AR.alloc("yin%d" % i, [128, 256], F32) for i in range(2)]
        ysg = [AR.alloc("ysg%d" % i, [128, 256], F32) for i in range(2)]
        for j in range(8):
            b = j % 2
            DS(lambda e, b=b, j=j: e.dma_start(out=ut3[b][:], in_=uT_d[j]), r=["uT_d"], w=["ut%d" % b])
            for t in range(8):
                pb = next_ps()
                q = t % 2
                for tau in range(t + 1):
                    T(lambda e, pb=pb, b=b, j=j, t=t, tau=tau: e.matmul(ps[pb][:, 0:256], BD[:, j, t - tau, :], ut3[b][:, tau::8],
                                                                       start=(tau == 0), stop=False),
                      r=["BD", "ut%d" % b], w=[PSK[pb]])
                for pr in range(4):
                    for ri in range(2):
                        kw = dict(tile_position=(0, 96)) if pr == 3 else {}
                        last = (pr == 3 and ri == 1)
                        T(lambda e, pb=pb, j=j, t=t, pr=pr, ri=ri, kw=kw, last=last: e.matmul(
                            ps[pb][32 * pr:32 * pr + 32, 1:256], HC[:, 4 * j + pr, t, ri, :], XS[:, ri, 0:255, 4 * j + pr],
                            start=False, stop=last, **kw),
                          r=["HC", "XS"], w=[PSK[pb]])
                V(lambda e, pb=pb, b=b, j=j, t=t, q=q: e.scalar_tensor_tensor(out=ytmp[q][:], in0=ut3[b][:, t::8], scalar=dT[:, j:j + 1],
                                                                              in1=ps[pb][:, 0:256], op0=ALU.mult, op1=ALU.add),
                  r=["ut%d" % b, "dT", PSK[pb]], w=["ytmp%d" % q])
                G(lambda e, q=q: e.tensor_tensor(yin[q][:], ytmp[q][:], ytmp[q][:], ALU.mult), r=["ytmp%d" % q], w=["yin%d" % q])
                G(lambda e, q=q: e.tensor_scalar(yin[q][:], yin[q][:], 0.044715, 1.0, ALU.mult, ALU.add), r=["yin%d" % q], w=["yin%d" % q])
                G(lambda e, q=q: e.tensor_tensor(yin[q][:], yin[q][:], ytmp[q][:], ALU.mult), r=["yin%d" % q, "ytmp%d" % q], w=["yin%d" % q])
                A(lambda e, q=q: e.activation(ysg[q][:], yin[q][:], AF.Sigmoid, scale=1.5957691216057308), r=["yin%d" % q], w=["ysg%d" % q])
                V(lambda e, q=q, j=j, t=t: e.tensor_tensor(zT[:, j, t::8], ytmp[q][:], ysg[q][:], ALU.mult),
                  r=["ytmp%d" % q, "ysg%d" % q], w=["zT"])
        if dbg == "zT":
            fin.append(DS(lambda e: e.dma_start(out=dbg_out("zT", [128, 8, L], BF16), in_=zT[:]), r=["zT"]))
            P.emit(fin)
            return nc, dbg_outs
        phase_barrier("p3a")
        AR.release(ms)

        wglu = AR.alloc("wglu", [128, 8, 1024], BF16)
        DG(lambda e: e.dma_start(out=wglu[:], in_=wglu_d.rearrange("(k p) n -> p k n", p=128)), w=["wglu"])
        oT = AR.alloc("oT", [128, 8, 512], BF16)
        sig = [AR.alloc("sig%d" % i, [128, 512], F32) for i in range(2)]
        sq16 = [AR.alloc("sq16_%d" % i, [128, 512], BF16) for i in range(2)]
        rstd = AR.alloc("rstd", [128, 512], F32)
        for tb in range(4):
            sl = slice(tb * 512, (tb + 1) * 512)
            pq = next_ps()
            for ft in range(8):
                pb = next_ps()
                while pb == pq:
                    pb = next_ps()
                q = ft % 2
                for k in range(8):
                    T(lambda e, pb=pb, k=k, ft=ft, sl=sl: e.matmul(ps[pb][:], wglu[:, k, ft * 128:(ft + 1) * 128], zT[:, k, sl],
                                                                  start=(k == 0), stop=(k == 7)), r=["wglu", "zT"], w=[PSK[pb]])
                A(lambda e, pb=pb, q=q: e.activation(sig[q][:], ps[pb][:], AF.Sigmoid), r=[PSK[pb]], w=["sig%d" % q])
                V(lambda e, q=q, ft=ft, sl=sl: e.tensor_tensor(sig[q][:], sig[q][:], zT[:, ft, sl], ALU.mult), r=["sig%d" % q, "zT"], w=["sig%d" % q])
                A(lambda e, q=q, ft=ft: e.copy(oT[:, ft, :], sig[q][:]), r=["sig%d" % q], w=["oT"])
                G(lambda e, q=q: e.tensor_tensor(sq16[q][:], sig[q][:], sig[q][:], ALU.mult), r=["sig%d" % q], w=["sq16_%d" % q])
                T(lambda e, pq=pq, q=q, ft=ft: e.matmul(ps[pq][:], ones16[:], sq16[q][:], start=(ft == 0), stop=(ft == 7)),
                  r=["ones16", "sq16_%d" % q], w=[PSK[pq]])
            A(lambda e, pq=pq: e.activation(rstd[:], ps[pq][:], AF.Sqrt, bias=EPS6, scale=1.0 / 1024), r=[PSK[pq], "colc"], w=["rstd"])
            V(lambda e: e.reciprocal(rstd[:], rstd[:]), r=["rstd"], w=["rstd"])
            for ft in range(8):
                V(lambda e, ft=ft, sl=sl: e.scalar_tensor_tensor(out=zT[:, ft, sl], in0=oT[:, ft, :], scalar=betaS[:, ft:ft + 1], in1=rstd[:],
                                                                 op0=ALU.mult, op1=ALU.mult), r=["oT", "betaS", "rstd"], w=["zT"])
        if dbg == "yssm":
            fin.append(DS(lambda e: e.dma_start(out=dbg_out("yssm", [128, 8, L], BF16), in_=zT[:]), r=["zT"]))
            P.emit(fin)
            return nc, dbg_outs
        phase_barrier("p3")
        AR.release(ms)
        m4 = AR.mark()
        gt1b = AR.alloc("gt1b", [128, D], F32)
        wo = [AR.alloc("wo%d" % i, [128, 16, 512], BF16) for i in range(2)]
        xt = [AR.alloc("xt%d" % i, [128, 512], F32) for i in range(3)]
        x1t = [AR.alloc("x1t%d" % i, [128, 512], F32) for i in range(3)]
        zrow = AR.alloc("zrow", [1, D], F32)
        zrow16 = AR.alloc("zrow16", [1, D], BF16)
        fillt = AR.alloc("fillt", [128, 64], F32)
        mod_bcast(gt1b, 2, "gt1b")
        V(lambda e: e.memset(zrow[:], 0.0), w=["zrow"])
        V(lambda e: e.memset(zrow16[:], 0.0), w=["zrow16"])
        V(lambda e: e.memset(fillt[:], 2048.0), w=["fillt"])
        DS(lambda e: e.dma_start(out=acc_d[L:L + 1, :], in_=zrow[:]), r=["zrow"], w=["acc_pad"])
        DS(lambda e: e.dma_start(out=h2_d[L:L + 1, :], in_=zrow16[:]), r=["zrow16"], w=["h2_pad"])
        DS(lambda e: e.dma_start(out=ti_d[L:L + 1, :], in_=zrow[0:1, 0:4]), r=["zrow"], w=["ti_pad"])
        DS(lambda e: e.dma_start(out=slot_d.rearrange("(p j) o -> p (j o)", p=128), in_=fillt[:]), r=["fillt"], w=["slot_d"])
        wout_v = wout_d.rearrange("(k p) n -> p k n", p=128)
        cnt4 = 0
        for cb in range(4):
            wb = cb % 2
            DG(lambda e, wb=wb, cb=cb: e.dma_start(out=wo[wb][:], in_=wout_v[:, :, cb * 512:(cb + 1) * 512]), w=["wo%d" % wb])
            for tt in range(16):
                pb = next_ps()
                q = cnt4 % 3
                cnt4 += 1
                for k in range(16):
                    src = zT if k < 8 else yretT
                    skey = "zT" if k < 8 else "yretT"
                    T(lambda e, pb=pb, k=k, tt=tt, wb=wb, src=src: e.matmul(ps[pb][:], src[:, k % 8, tt * 128:(tt + 1) * 128], wo[wb][:, k, :],
                                                                           start=(k == 0), stop=(k == 15)),
                      r=[skey, "wo%d" % wb], w=[PSK[pb]])
                DS(lambda e, q=q, tt=tt, cb=cb: e.dma_start(out=xt[q][:], in_=x_d[tt * 128:(tt + 1) * 128, cb * 512:(cb + 1) * 512]), w=["xt%d" % q])
                V(lambda e, pb=pb, q=q, cb=cb: e.tensor_tensor(x1t[q][:], ps[pb][:], gt1b[:, cb * 512:(cb + 1) * 512], ALU.mult),
                  r=[PSK[pb], "gt1b"], w=["x1t%d" % q])
                G(lambda e, q=q: e.tensor_tensor(x1t[q][:], x1t[q][:], xt[q][:], ALU.add), r=["x1t%d" % q, "xt%d" % q], w=["x1t%d" % q])
                DS(lambda e, q=q, tt=tt, cb=cb: e.dma_start(out=acc_d[tt * 128:(tt + 1) * 128, cb * 512:(cb + 1) * 512], in_=x1t[q][:]),
                   r=["x1t%d" % q], w=["acc%d" % tt])
        phase_barrier("p4")
        AR.release(base_mark)

        blke = AR.alloc("blke", [128, 64], F32)
        rtc = AR.alloc("rtc", [128, 112], F32)
        m5 = AR.mark()
        A2b = AR.alloc("A2b", [128, D], F32)
        B2b = AR.alloc("B2b", [128, D], F32)
        g2b = AR.alloc("g2b", [128, D], F32)
        wr = AR.alloc("wr", [128, 16, 36], F32)
        brb = AR.alloc("brb", [128, 36], F32)
        LG = AR.alloc("LG", [128, 16, 36], F32)
        ss5 = AR.alloc("ss", [128, 16], F32)
        rs5 = AR.alloc("rs", [128, 16], F32)
        junk5 = AR.alloc("junk", [128, D], F32)
        xb5 = [AR.alloc("xb%d" % i, [128, D], F32) for i in range(2)]
        h2f = AR.alloc("h2f", [128, D], F32)
        h2b = [AR.alloc("h2b%d" % i, [128, D], BF16) for i in range(2)]
        h2T = AR.alloc("h2T", [128, 16, 128], F32)
        mod_bcast(A2b, 4, "A2b")
        mod_bcast(B2b, 3, "B2b")
        DS(lambda e: e.dma_start(out=g2b[:], in_=g2_d.partition_broadcast(128)), w=["g2b"])
        DS(lambda e: e.dma_start(out=wr[:], in_=wr_d.rearrange("(k p) n -> p k n", p=128)), w=["wr"])
        DS(lambda e: e.dma_start(out=brb[:], in_=br_d.partition_broadcast(128)), w=["brb"])
        V(lambda e: e.scalar_tensor_tensor(out=A2b[:], in0=A2b[:], scalar=1.0, in1=g2b[:], op0=ALU.add, op1=ALU.mult),
          r=["A2b", "g2b"], w=["A2b"])
        V(lambda e: e.memset(ss5[:], 0.0), w=["ss"])
        for tt in range(16):
            b = tt % 2
            DS(lambda e, b=b, tt=tt: e.dma_start(out=xb5[b][:], in_=acc_d[tt * 128:(tt + 1) * 128, :]), w=["xb%d" % b])
            rms_rstd_g(junk5, ss5, rs5, xb5[b][:], "xb%d" % b, tt, EPS6, D)
            V(lambda e, b=b, tt=tt: e.scalar_tensor_tensor(out=h2f[:], in0=xb5[b][:], scalar=rs5[:, tt:tt + 1], in1=A2b[:],
                                                           op0=ALU.mult, op1=ALU.mult), r=["xb%d" % b, "rs", "A2b"], w=["h2f"])
            V(lambda e: e.tensor_tensor(h2f[:], h2f[:], B2b[:], ALU.add), r=["h2f", "B2b"], w=["h2f"])
            A(lambda e, b=b: e.copy(h2b[b][:], h2f[:]), r=["h2f"], w=["h2b%d" % b])
            DS(lambda e, b=b, tt=tt: e.dma_start(out=h2_d[tt * 128:(tt + 1) * 128, :], in_=h2b[b][:]), r=["h2b%d" % b], w=["h2_d"])
            for kg in range(4):
                pb = next_ps()
                for kk in range(4):
                    k = kg * 4 + kk
                    T(lambda e, pb=pb, kk=kk, k=k: e.matmul(ps[pb][:, kk * 128:(kk + 1) * 128], h2f[:, k * 128:(k + 1) * 128], ident32,
                                                          start=True, stop=True), r=["h2f", "cst"], w=[PSK[pb]])
                if kg % 2 == 0:
                    A(lambda e, pb=pb, kg=kg: e.copy(h2T[:, kg * 4:(kg + 1) * 4, :], ps[pb][:].rearrange("p (a c) -> p a c", a=4)),
                      r=[PSK[pb]], w=["h2T"])
                else:
                    V(lambda e, pb=pb, kg=kg: e.tensor_copy(h2T[:, kg * 4:(kg + 1) * 4, :], ps[pb][:].rearrange("p (a c) -> p a c", a=4)),
                      r=[PSK[pb]], w=["h2T"])
            pb = next_ps()
            for k in range(16):
                T(lambda e, pb=pb, k=k: e.matmul(ps[pb][:, 0:36], h2T[:, k, :], wr[:, k, :], start=(k == 0), stop=(k == 15)),
                  r=["h2T", "wr"], w=[PSK[pb]])
            V(lambda e, pb=pb, tt=tt: e.tensor_tensor(LG[:, tt, :], ps[pb][:, 0:36], brb[:], ALU.add), r=[PSK[pb], "brb"], w=["LG"])
        if dbg == "LG":
            return dump("LG", LG, [128, 16, 36], F32, ["LG"])

        DS(lambda e: e.dma_start(out=rtc[:], in_=rt_d), w=["rtc"])
        tokid = rtc[:, 0:16]
        blkthr = rtc[:, 16:80]
        iota32 = rtc[:, 80:112]
        rk = [0]

        def rt(shape, dt=F32):
            rk[0] += 1
            return AR.alloc("rt%d" % rk[0], shape, dt), "rt%d" % rk[0]

        gl = LG[:, :, 0:4]
        gmax, gmaxk = rt([128, 16]); ohg, ohgk = rt([128, 16, 4]); ge, gek = rt([128, 16, 4]); gw, gwk = rt([128, 16])
        b3 = lambda ap, n: ap.unsqueeze(2).to_broadcast([128, 16, n])
        V(lambda e: e.tensor_reduce(out=gmax[:], in_=gl, axis=AX.X, op=ALU.max), r=["LG"], w=[gmaxk])
        V(lambda e: e.tensor_tensor(ohg[:], gl, b3(gmax[:], 4), ALU.is_equal), r=["LG", gmaxk], w=[ohgk])
        V(lambda e: e.tensor_tensor(ge[:], gl, b3(gmax[:], 4), ALU.subtract), r=["LG", gmaxk], w=[gek])
        A(lambda e: e.activation(ge[:], ge[:], AF.Exp), r=[gek], w=[gek])
        V(lambda e: e.tensor_reduce(out=gw[:], in_=ge[:], axis=AX.X, op=ALU.add), r=[gek], w=[gwk])
        V(lambda e: e.reciprocal(gw[:], gw[:]), r=[gwk], w=[gwk])
        els, elsk = rt([128, 16, 8]); etmp, etk = rt([128, 16, 8])
        V(lambda e: e.tensor_tensor(els[:], LG[:, :, 4:12], b3(ohg[:, :, 0], 8), ALU.mult), r=["LG", ohgk], w=[elsk])
        for g in range(1, 4):
            V(lambda e, g=g: e.tensor_tensor(etmp[:], LG[:, :, 4 + 8 * g:12 + 8 * g], b3(ohg[:, :, g], 8), ALU.mult), r=["LG", ohgk], w=[etk])
            V(lambda e: e.tensor_tensor(els[:], els[:], etmp[:], ALU.add), r=[elsk, etk], w=[elsk])
        mx1, mx1k = rt([128, 16]); oh1, oh1k = rt([128, 16, 8]); el2, el2k = rt([128, 16, 8]); mx2, mx2k = rt([128, 16]); oh2, oh2k = rt([128, 16, 8])
        V(lambda e: e.tensor_reduce(out=mx1[:], in_=els[:], axis=AX.X, op=ALU.max), r=[elsk], w=[mx1k])
        V(lambda e: e.tensor_tensor(oh1[:], els[:], b3(mx1[:], 8), ALU.is_equal), r=[elsk, mx1k], w=[oh1k])
        V(lambda e: e.scalar_tensor_tensor(out=el2[:], in0=oh1[:], scalar=-1.0e30, in1=els[:], op0=ALU.mult, op1=ALU.add), r=[oh1k, elsk], w=[el2k])
        V(lambda e: e.tensor_reduce(out=mx2[:], in_=el2[:], axis=AX.X, op=ALU.max), r=[el2k], w=[mx2k])
        V(lambda e: e.tensor_tensor(oh2[:], el2[:], b3(mx2[:], 8), ALU.is_equal), r=[el2k, mx2k], w=[oh2k])
        ee, eek = rt([128, 16]); w1, w1k = rt([128, 16]); w2, w2k = rt([128, 16])
        V(lambda e: e.tensor_tensor(ee[:], mx2[:], mx1[:], ALU.subtract), r=[mx1k, mx2k], w=[eek])
        A(lambda e: e.activation(ee[:], ee[:], AF.Exp), r=[eek], w=[eek])
        V(lambda e: e.tensor_scalar(w1[:], ee[:], 1.0, None, ALU.add), r=[eek], w=[w1k])
        V(lambda e: e.reciprocal(w1[:], w1[:]), r=[w1k], w=[w1k])
        V(lambda e: e.tensor_tensor(w2[:], ee[:], w1[:], ALU.mult), r=[eek, w1k], w=[w2k])
        V(lambda e: e.tensor_tensor(w1[:], w1[:], gw[:], ALU.mult), r=[w1k, gwk], w=[w1k])
        V(lambda e: e.tensor_tensor(w2[:], w2[:], gw[:], ALU.mult), r=[w2k, gwk], w=[w2k])
        gsel, gselk = rt([128, 16]); j1, j1k = rt([128, 16]); j2, j2k = rt([128, 16]); itmp, itk = rt([128, 16, 8])
        ib = lambda n: iota32[:, 0:n].unsqueeze(1).to_broadcast([128, 16, n])
        V(lambda e: e.tensor_tensor(itmp[:, :, 0:4], ohg[:], ib(4), ALU.mult), r=[ohgk, "rtc"], w=[itk])
        V(lambda e: e.tensor_reduce(out=gsel[:], in_=itmp[:, :, 0:4], axis=AX.X, op=ALU.add), r=[itk], w=[gselk])
        V(lambda e: e.tensor_tensor(itmp[:], oh1[:], ib(8), ALU.mult), r=[oh1k, "rtc"], w=[itk])
        V(lambda e: e.tensor_reduce(out=j1[:], in_=itmp[:], axis=AX.X, op=ALU.add), r=[itk], w=[j1k])
        V(lambda e: e.tensor_tensor(itmp[:], oh2[:], ib(8), ALU.mult), r=[oh2k, "rtc"], w=[itk])
        V(lambda e: e.tensor_reduce(out=j2[:], in_=itmp[:], axis=AX.X, op=ALU.add), r=[itk], w=[j2k])
        TIt, tik = rt([128, 16, 4])
        V(lambda e: e.tensor_copy(TIt[:, :, 0], w1[:]), r=[w1k], w=[tik])
        V(lambda e: e.tensor_copy(TIt[:, :, 2], w2[:]), r=[w2k], w=[tik])
        V(lambda e: e.scalar_tensor_tensor(out=TIt[:, :, 1], in0=gsel[:], scalar=8.0, in1=j1[:], op0=ALU.mult, op1=ALU.add), r=[gselk, j1k], w=[tik])
        V(lambda e: e.scalar_tensor_tensor(out=TIt[:, :, 3], in0=gsel[:], scalar=8.0, in1=j2[:], op0=ALU.mult, op1=ALU.add), r=[gselk, j2k], w=[tik])
        DS(lambda e: e.dma_start(out=ti_d[0:L, :].rearrange("(t p) c -> p t c", p=128), in_=TIt[:]), r=[tik], w=["ti_d"])
        OH1, OH1k = rt([128, 16, 32]); OH2, OH2k = rt([128, 16, 32]); Mt, Mk = rt([128, 16, 32])
        i32b = iota32.unsqueeze(1).to_broadcast([128, 16, 32])
        V(lambda e: e.tensor_tensor(OH1[:], i32b, b3(TIt[:, :, 1], 32), ALU.is_equal), r=["rtc", tik], w=[OH1k])
        V(lambda e: e.tensor_tensor(OH2[:], i32b, b3(TIt[:, :, 3], 32), ALU.is_equal), r=["rtc", tik], w=[OH2k])
        V(lambda e: e.tensor_tensor(Mt[:], OH1[:], OH2[:], ALU.add), r=[OH1k, OH2k], w=[Mk])
        pc = next_ps()
        pr_ = next_ps()
        T(lambda e, pc=pc: e.matmul(ps[pc][:], ones32, Mt[:].rearrange("p a b -> p (a b)"), start=True, stop=True), r=["cst", Mk], w=[PSK[pc]])
        T(lambda e, pr_=pr_: e.matmul(ps[pr_][:], tri32, Mt[:].rearrange("p a b -> p (a b)"), start=True, stop=True), r=["cst", Mk], w=[PSK[pr_]])
        tot, totk = rt([128, 16, 32]); dest, destk = rt([128, 16, 32]); texc, texck = rt([128, 16, 32])
        A(lambda e, pc=pc: e.copy(tot[:].rearrange("p a b -> p (a b)"), ps[pc][:]), r=[PSK[pc]], w=[totk])
        V(lambda e, pr_=pr_: e.tensor_copy(dest[:].rearrange("p a b -> p (a b)"), ps[pr_][:]), r=[PSK[pr_]], w=[destk])
        V(lambda e: e.memset(texc[:, 0, :], 0.0), w=[texck])
        for tt in range(1, 16):
            V(lambda e, tt=tt: e.tensor_tensor(texc[:, tt, :], texc[:, tt - 1, :], tot[:, tt - 1, :], ALU.add), r=[texck, totk], w=[texck])
        cntt, cntk = rt([128, 32]); nbi, nbik = rt([128, 32], I32); padd, padk = rt([128, 32]); pend, pendk = rt([128, 32]); poff, poffk = rt([128, 32])
        onesr, onesk = rt([128, 32])
        V(lambda e: e.tensor_tensor(cntt[:], texc[:, 15, :], tot[:, 15, :], ALU.add), r=[texck, totk], w=[cntk])
        V(lambda e: e.tensor_scalar(cntt[:], cntt[:], 1.0 / 128, 0.49609375, ALU.mult, ALU.add), r=[cntk], w=[cntk])
        V(lambda e: e.tensor_copy(nbi[:], cntt[:]), r=[cntk], w=[nbik])
        V(lambda e: e.tensor_copy(padd[:], nbi[:]), r=[nbik], w=[padk])
        V(lambda e: e.tensor_scalar(padd[:], padd[:], 128.0, None, ALU.mult), r=[padk], w=[padk])
        V(lambda e: e.memset(onesr[:], 1.0), w=[onesk])
        V(lambda e: e.tensor_tensor_scan(pend[:], onesr[:], padd[:], 0.0, ALU.mult, ALU.add), r=[onesk, padk], w=[pendk])
        V(lambda e: e.tensor_tensor(poff[:], pend[:], padd[:], ALU.subtract), r=[pendk, padk], w=[poffk])
        V(lambda e: e.tensor_tensor(dest[:], dest[:], texc[:], ALU.add), r=[destk, texck], w=[destk])
        V(lambda e: e.tensor_tensor(dest[:], dest[:], poff[:, :].unsqueeze(1).to_broadcast([128, 16, 32]), ALU.add), r=[destk, poffk], w=[destk])
        d12, d12k = rt([128, 2, 16]); d12i, d12ik = rt([128, 2, 16], I32)
        V(lambda e: e.tensor_tensor(OH1[:], OH1[:], dest[:], ALU.mult), r=[OH1k, destk], w=[OH1k])
        V(lambda e: e.tensor_reduce(out=d12[:, 0, :], in_=OH1[:], axis=AX.X, op=ALU.add), r=[OH1k], w=[d12k])
        V(lambda e: e.tensor_tensor(OH2[:], OH2[:], dest[:], ALU.mult), r=[OH2k, destk], w=[OH2k])
        V(lambda e: e.tensor_reduce(out=d12[:, 1, :], in_=OH2[:], axis=AX.X, op=ALU.add), r=[OH2k], w=[d12k])
        V(lambda e: e.tensor_copy(d12i[:], d12[:]), r=[d12k], w=[d12ik])
        cmpt, cmpk = rt([128, 64, 32])
        V(lambda e: e.tensor_tensor(cmpt[:], pend[:, :].unsqueeze(1).to_broadcast([128, 64, 32]), blkthr.unsqueeze(2).to_broadcast([128, 64, 32]), ALU.is_le),
          r=[pendk, "rtc"], w=[cmpk])
        V(lambda e: e.tensor_reduce(out=blke[:], in_=cmpt[:], axis=AX.X, op=ALU.add), r=[cmpk], w=["blke"])
        V(lambda e: e.tensor_scalar(blke[:], blke[:], 31.0, None, ALU.min), r=["blke"], w=["blke"])
        for tt in range(16):
            for a_ in range(2):
                DG(lambda e, tt=tt, a_=a_: e.indirect_dma_start(out=slot_d, out_offset=bass.IndirectOffsetOnAxis(ap=d12i[:, a_, tt:tt + 1], axis=0),
                                                               in_=tokid[:, tt:tt + 1], in_offset=None),
                   r=[d12ik, "rtc", "slot_d"], w=["slot_d"])
        if dbg == "route":
            t_ = AR.alloc("dbgslot", [128, 64], F32)
            DS(lambda e: e.dma_start(out=t_[:], in_=slot_d.rearrange("(p j) o -> p (j o)", p=128)), r=["slot_d"], w=["dbgslot"])
            fin.append(DS(lambda e: e.dma_start(out=dbg_out("slot", [128, 64]), in_=t_[:]), r=["dbgslot"]))
            fin.append(DS(lambda e: e.dma_start(out=dbg_out("blke", [128, 64]), in_=blke[:]), r=["blke"]))
            fin.append(DS(lambda e: e.dma_start(out=dbg_out("TI", [128, 16, 4]), in_=TIt[:]), r=[tik]))
            return dump("pend", pend, [128, 32], F32, [pendk])

        phase_barrier("p6")
        AR.release(m5)
        gt2b = AR.alloc("gt2b", [128, D], F32)
        mod_bcast(gt2b, 5, "gt2b")
        wgS = AR.alloc("wgS", [128, 16, 1024], BF16)
        wuS = AR.alloc("wuS", [128, 16, 1024], BF16)
        wdS = AR.alloc("wdS", [128, 8, 2048], BF16)
        xs = [AR.alloc("xs%d" % i, [128, D], BF16) for i in range(3)]
        xsT = [AR.alloc("xsT%d" % i, [128, 16, 128], BF16) for i in range(2)]
        sg = AR.alloc("sg", [128, 1024], F32)
        a16 = AR.alloc("a16", [128, 1024], BF16)
        actT = AR.alloc("actT", [128, 8, 128], BF16)
        yb = AR.alloc("yb", [128, D], F32)
        stok = [AR.alloc("stok%d" % i, [128, 1], F32) for i in range(3)]
        idx = [AR.alloc("idx%d" % i, [128, 1], I32) for i in range(3)]
        tinf = [AR.alloc("tinf%d" % i, [128, 4], F32) for i in range(3)]
        wsl = [AR.alloc("wsl%d" % i, [128, 2], F32) for i in range(2)]
        offf = AR.alloc("offf", [128, 2], F32)
        offs8f = AR.alloc("offs8f", [128, 8], F32)
        offs8i = [AR.alloc("offs8i%d" % i, [128, 8], I32) for i in range(2)]
        NB = 64
        skipb = AR.alloc("skipb", [128, 64], F32)
        V(lambda e: e.memset(skipb[:, 0:1], 0.0), w=["skipb"])
        V(lambda e: e.tensor_tensor(skipb[:, 1:64], blke[:, 1:64], blke[:, 0:63], ALU.is_equal), r=["blke", "skipb"], w=["skipb"])
        V(lambda e: e.tensor_scalar(skipb[:], skipb[:], 524288.0, None, ALU.mult), r=["skipb"], w=["skipb"])
        DN_BOUND = NE * 1024 - 1
        regcache = {}

        def breg(e, val):
            if val not in regcache:
                regcache[val] = e.to_reg(val)
            return regcache[val]

        def moe_gathers(b):
            q = b % 3
            DS(lambda e, q=q, b=b: e.dma_start(out=stok[q][:], in_=slot_d[b * 128:(b + 1) * 128, :]), r=["slot_d"], w=["stok%d" % q])
            V(lambda e, q=q: e.tensor_copy(idx[q][:], stok[q][:]), r=["stok%d" % q], w=["idx%d" % q])
            DG(lambda e, q=q: e.indirect_dma_start(out=tinf[q][:], out_offset=None, in_=ti_d,
                                                   in_offset=bass.IndirectOffsetOnAxis(ap=idx[q][:, :], axis=0)),
               r=["idx%d" % q, "ti_d", "ti_pad"], w=["tinf%d" % q])
            DG(lambda e, q=q: e.indirect_dma_start(out=xs[q][:], out_offset=None, in_=h2_d,
                                                   in_offset=bass.IndirectOffsetOnAxis(ap=idx[q][:, :], axis=0)),
               r=["idx%d" % q, "h2_d", "h2_pad"], w=["xs%d" % q])

        def moe_loadsA(b):
            q = b % 2
            V(lambda e, b=b: e.scalar_tensor_tensor(out=offf[:, 0:1], in0=blke[:, b:b + 1], scalar=128.0, in1=PIDX, op0=ALU.mult, op1=ALU.add),
              r=["blke", "colc"], w=["offf"])
            V(lambda e, b=b: e.tensor_tensor(offf[:, 0:1], offf[:, 0:1], skipb[:, b:b + 1], ALU.add), r=["offf", "skipb"], w=["offf"])
            V(lambda e: e.tensor_scalar(offf[:, 1:2], offf[:, 0:1], 8.0, None, ALU.mult), r=["offf"], w=["offf1"])
            V(lambda e: e.tensor_scalar(offs8f[:], rtc[:, 80:88], offf[:, 1:2], None, ALU.add), r=["rtc", "offf1"], w=["offs8f"])
            V(lambda e, q=q: e.tensor_copy(offs8i[q][:], offs8f[:]), r=["offs8f"], w=["offs8i%d" % q])
            for k2 in range(8):
                for wS, w_d, key in ((wgS, wg_d, "wgS"), (wuS, wu_d, "wuS")):
                    DG(lambda e, q=q, wS=wS, w_d=w_d, k2=k2: e.indirect_dma_start(
                        out=wS[:, 2 * k2:2 * k2 + 2, :].rearrange("p a f -> p (a f)"), out_offset=None, in_=w_d,
                        in_offset=bass.IndirectOffsetOnAxis(ap=offs8i[q][:, k2:k2 + 1], axis=0),
                        bounds_check=breg(e, DN_BOUND), oob_is_err=False),
                       r=["offs8i%d" % q], w=["%s%d" % (key, k2)])

        def moe_loadsB(b):
            q = b % 2
            for ff in range(8):
                DG(lambda e, q=q, ff=ff: e.indirect_dma_start(
                    out=wdS[:, ff, :], out_offset=None, in_=wd_d, in_offset=bass.IndirectOffsetOnAxis(ap=offs8i[q][:, ff:ff + 1], axis=0),
                    bounds_check=breg(e, DN_BOUND), oob_is_err=False),
                   r=["offs8i%d" % q], w=["wdS%d" % ff])

        def moe_xsT(b):
            q3 = b % 3
            q = b % 2
            for kg in range(4):
                pb = 4 + kg % 2
                for kk in range(4):
                    k = kg * 4 + kk
                    T(lambda e, pb=pb, kk=kk, k=k, q3=q3: e.matmul(ps[pb][:, kk * 128:(kk + 1) * 128], xs[q3][:, k::16], id16[:], start=True, stop=True),
                      r=["xs%d" % q3, "id16"], w=[PSK[pb]])
                if kg % 2 == 0:
                    A(lambda e, pb=pb, kg=kg, q=q: e.copy(xsT[q][:, kg * 4:(kg + 1) * 4, :], ps[pb][:].rearrange("p (a c) -> p a c", a=4)),
                      r=[PSK[pb]], w=["xsT%d" % q])
                else:
                    V(lambda e, pb=pb, kg=kg, q=q: e.tensor_copy(xsT[q][:, kg * 4:(kg + 1) * 4, :], ps[pb][:].rearrange("p (a c) -> p a c", a=4)),
                      r=[PSK[pb]], w=["xsT%d" % q])

        moe_gathers(0)
        moe_gathers(1)
        moe_loadsA(0)
        moe_loadsB(0)
        moe_xsT(0)
        for b in range(NB):
            q = b % 2
            q3 = b % 3
            V(lambda e, q=q, q3=q3, b=b: e.tensor_scalar(wsl[q][:, 0:1], tinf[q3][:, 1:2], blke[:, b:b + 1], tinf[q3][:, 0:1], ALU.is_equal, ALU.mult),
              r=["tinf%d" % q3, "blke"], w=["wsl%d" % q])
            V(lambda e, q=q, q3=q3, b=b: e.tensor_scalar(wsl[q][:, 1:2], tinf[q3][:, 3:4], blke[:, b:b + 1], tinf[q3][:, 2:3], ALU.is_equal, ALU.mult),
              r=["tinf%d" % q3, "blke"], w=["wsl%d" % q])
            V(lambda e, q=q: e.tensor_tensor(wsl[q][:, 0:1], wsl[q][:, 0:1], wsl[q][:, 1:2], ALU.add), r=["wsl%d" % q], w=["wsl%d" % q])
            for kk in range(16):
                for wi_, (wS, key) in enumerate(((wgS, "wgS"), (wuS, "wuS"))):
                    for hf_ in range(2):
                        pb = wi_ * 2 + hf_
                        T(lambda e, pb=pb, kk=kk, wS=wS, hf_=hf_, q=q: e.matmul(ps[pb][:], xsT[q][:, kk, :], wS[:, kk, hf_ * 512:(hf_ + 1) * 512],
                                                                               start=(kk == 0), stop=(kk == 15)),
                          r=["xsT%d" % q, "%s%d" % (key, kk // 2)], w=[PSK[pb]])
            if b + 2 < NB:
                moe_gathers(b + 2)
            if b + 1 < NB:
                moe_loadsA(b + 1)
                moe_xsT(b + 1)
            for hf_ in range(2):
                A(lambda e, hf_=hf_: e.activation(sg[:, hf_ * 512:(hf_ + 1) * 512], ps[hf_][:], AF.Silu), r=[PSK[hf_]], w=["sg"])
                V(lambda e, hf_=hf_: e.tensor_tensor(a16[:, hf_ * 512:(hf_ + 1) * 512], sg[:, hf_ * 512:(hf_ + 1) * 512], ps[2 + hf_][:], ALU.mult),
                  r=["sg", PSK[2 + hf_]], w=["a16"])
            for fg in range(2):
                pb = 4 + fg
                for fi in range(4):
                    ff = fg * 4 + fi
                    T(lambda e, pb=pb, fi=fi, ff=ff: e.matmul(ps[pb][:, fi * 128:(fi + 1) * 128], a16[:, ff::8], id16[:], start=True, stop=True),
                      r=["a16", "id16"], w=[PSK[pb]])
                if fg == 0:
                    A(lambda e, pb=pb, fg=fg: e.copy(actT[:, fg * 4:(fg + 1) * 4, :], ps[pb][:].rearrange("p (a c) -> p a c", a=4)), r=[PSK[pb]], w=["actT"])
                else:
                    V(lambda e, pb=pb, fg=fg: e.tensor_copy(actT[:, fg * 4:(fg + 1) * 4, :], ps[pb][:].rearrange("p (a c) -> p a c", a=4)), r=[PSK[pb]], w=["actT"])
            for db in range(4):
                pb = 6 + db % 2
                for ff in range(8):
                    T(lambda e, pb=pb, ff=ff, db=db: e.matmul(ps[pb][:], actT[:, ff, :], wdS[:, ff, db * 512:(db + 1) * 512],
                                                            start=(ff == 0), stop=(ff == 7)), r=["actT", "wdS%d" % ff], w=[PSK[pb]])
                V(lambda e, pb=pb, db=db, q=q: e.scalar_tensor_tensor(out=yb[:, db * 512:(db + 1) * 512], in0=ps[pb][:], scalar=wsl[q][:, 0:1],
                                                                     in1=gt2b[:, db * 512:(db + 1) * 512], op0=ALU.mult, op1=ALU.mult),
                  r=[PSK[pb], "wsl%d" % q, "gt2b"], w=["yb"])
            if b + 1 < NB:
                moe_loadsB(b + 1)
            DG(lambda e, q3=q3: e.indirect_dma_start(out=acc_d, out_offset=bass.IndirectOffsetOnAxis(ap=idx[q3][:, :], axis=0), in_=yb[:],
                                                    in_offset=None, compute_op=ALU.add),
               r=["yb", "idx%d" % q3, "acc_pad"] + ["acc%d" % i for i in range(16)], w=["accs"])
        phase_barrier("p7")
        AR.release(base_mark)

        gfb = AR.alloc("gfb", [128, D], F32)
        ss8v = AR.alloc("ss8", [128, 16], F32)
        rs8v = AR.alloc("rs8", [128, 16], F32)
        junk8v = AR.alloc("junk8", [128, D], F32)
        xb8v = [AR.alloc("xf%d" % i, [128, D], F32) for i in range(2)]
        ob = [AR.alloc("ob%d" % i, [128, D], F32) for i in range(2)]
        DS(lambda e: e.dma_start(out=gfb[:], in_=gf_d.partition_broadcast(128)), w=["gfb"])
        V(lambda e: e.memset(ss8v[:], 0.0), w=["ss"])
        for tt in range(16):
            b = tt % 2
            DS(lambda e, b=b, tt=tt: e.dma_start(out=xb8v[b][:], in_=acc_d[tt * 128:(tt + 1) * 128, :]), w=["xf%d" % b])
            rms_rstd_g(junk8v, ss8v, rs8v, xb8v[b][:], "xf%d" % b, tt, EPS6, D)
            V(lambda e, b=b, tt=tt: e.scalar_tensor_tensor(out=ob[b][:], in0=xb8v[b][:], scalar=rs8v[:, tt:tt + 1], in1=gfb[:],
                                                           op0=ALU.mult, op1=ALU.mult), r=["xf%d" % b, "rs", "gfb"], w=["ob%d" % b])
            fin.append(DS(lambda e, b=b, tt=tt: e.dma_start(out=out_d[tt * 128:(tt + 1) * 128, :], in_=ob[b][:]), r=["ob%d" % b]))
        P.emit(fin)
        return nc, dbg_outs


def host_consts():
    c = {}
    I = np.eye(128, dtype=np.float32)
    perm = np.zeros((128, 128), np.float32)
    for i in range(128):
        perm[(i + 64) % 128, i] = 1.0
    onesdiv = np.full((128, 128), 1.0 / 128, np.float32)
    tri = np.triu(np.ones((128, 128), np.float32), k=1)
    ones = np.ones((128, 128), np.float32)
    c["cst32"] = np.ascontiguousarray(np.stack([I, perm, onesdiv, tri, ones], axis=1))
    half = 64
    inv_freq = 1.0 / (10000.0 ** (np.arange(half, dtype=np.float32) * 2.0 / 128))
    ang = np.arange(L, dtype=np.float32)[:, None] * inv_freq[None, :]
    cos = np.cos(ang).astype(np.float32).T
    sin = np.sin(ang).astype(np.float32).T
    cosT = np.concatenate([cos, cos], axis=0)
    sinT = np.concatenate([-sin, sin], axis=0)
    c["rope"] = np.ascontiguousarray(np.stack([cosT, sinT], axis=1).astype(np.float32))
    H = 8
    gamma = 1.0 - np.exp2(-5.0 - np.arange(H, dtype=np.float32))
    log_g = np.log(gamma).astype(np.float32)
    idx = np.arange(128, dtype=np.float32)
    rel = idx[:, None] - idx[None, :]
    decay = np.where(rel >= 0, np.exp(log_g[:, None, None] * np.maximum(rel, 0.0)), 0.0)
    sc = 128.0 ** -0.5
    decT = (decay.transpose(2, 0, 1) * sc).astype(np.float32)
    c["decayT"] = np.ascontiguousarray(decT)
    zeta = np.exp(log_g[:, None] * (127.0 - idx)[None, :]).astype(np.float32)
    xi = np.exp(log_g[None, :] * (idx + 1.0)[:, None]).astype(np.float32)
    xiT = np.broadcast_to(xi.T[None, :, :], (128, 8, 128)).astype(np.float32)
    c["xiT"] = np.ascontiguousarray(xiT)
    col = np.zeros((128, 24), np.float32)
    p = np.arange(128)
    col[:, 0] = np.where(p < 64, -1.0, 1.0)
    col[:, 1] = np.where(p < 64, 1.0, -1.0)
    col[:, 2] = (p // 16) % 2
    col[:, 3] = 1.0 - col[:, 2]
    col[:, 4] = 1e-6
    col[:, 5] = 1e-5
    col[:, 6] = p
    col[:, 7] = math.pi / 2
    col[:, 8:16] = (zeta.T * sc)
    col[:, 16] = (p < 64)
    col[:, 17] = (p >= 64)
    col[:, 18] = -col[:, 16]
    col[:, 19] = -col[:, 17]
    c["colc"] = col
    q = np.arange(128)[:, None]
    cc = np.arange(32)[None, :]
    m32 = (((q % 32) // 16) == (cc // 16)).astype(np.float32)
    m4 = np.zeros((128, 4, 32), np.float32)
    for pr in range(4):
        m4[32 * pr:32 * pr + 32, pr, :] = m32[32 * pr:32 * pr + 32]
    c["mask4"] = m4
    rt = np.zeros((128, 16 + 64 + 32), np.float32)
    rt[:, 0:16] = np.arange(16)[None, :] * 128 + p[:, None]
    rt[:, 16:80] = (np.arange(64) * 128)[None, :]
    rt[:, 80:112] = np.arange(32)[None, :]
    c["rtc"] = rt
    return c


def host_layout(inp):
    f = lambda a: np.ascontiguousarray(np.asarray(a, dtype=np.float32))
    o = {}
    o["w_ada"] = f(inp["w_ada"][0])
    o["b_ada"] = f(inp["b_ada"][0][None, :])
    o["g_norm1"] = f(inp["g_norm1"][0][None, :])
    o["g_norm2"] = f(inp["g_norm2"][0][None, :])
    o["g_final"] = f(inp["g_final"][None, :])
    w_in = np.asarray(inp["w_in"][0])
    order = list(range(8))
    for s in range(4):
        for h in range(8):
            order.append(8 + s * 8 + h)
    wt = w_in.reshape(16, 128, 40, 128).transpose(2, 1, 0, 3)
    o["w_in_t"] = f(wt)
    o["w_glu"] = f(inp["w_glu"][0])
    o["w_out"] = f(inp["w_out"][0])
    beta = np.concatenate([np.asarray(inp["beta_ssm"][0]), np.asarray(inp["beta_ret"][0])])
    o["betaT"] = f(beta.reshape(16, 128).T)
    o["dT"] = f(np.asarray(inp["ssm_d"][0]).reshape(8, 128).T)
    o["wr"] = f(np.concatenate([np.asarray(inp["w_router_group"][0]), np.asarray(inp["w_router_expert"][0])], axis=1))
    o["br"] = f(np.concatenate([np.asarray(inp["b_router_group"][0]), np.asarray(inp["b_router_expert"][0])])[None, :])
    o["w_gate"] = f(inp["w_gate"][0]).reshape(32 * 1024, 2048)
    o["w_up"] = f(inp["w_up"][0]).reshape(32 * 1024, 2048)
    o["w_down"] = f(inp["w_down"][0]).reshape(32 * 1024, 2048)
    a_re = np.asarray(inp["ssm_a_re"][0]); a_im = np.asarray(inp["ssm_a_im"][0])
    ldt = np.asarray(inp["ssm_log_dt"][0])
    B_re = np.asarray(inp["ssm_b_re"][0]); B_im = np.asarray(inp["ssm_b_im"][0])
    C_re = np.asarray(inp["ssm_c_re"][0]); C_im = np.asarray(inp["ssm_c_im"][0])
    par = np.stack([a_re.T, a_im.T, np.broadcast_to(ldt[None, :], (64, 64))], axis=1)
    o["sl_par"] = f(np.concatenate([par, par], axis=0))
    Bre_p = B_re.transpose(1, 0, 2); Bim_p = B_im.transpose(1, 0, 2)
    V1 = np.concatenate([Bre_p, Bim_p], axis=0); V2 = np.concatenate([Bim_p, Bre_p], axis=0)
    o["sl_V"] = f(np.stack([V1, V2], axis=1))
    o["sl_C"] = f(np.concatenate([C_re.transpose(2, 0, 1), C_im.transpose(2, 0, 1)], axis=0))
    def pl(arr_gp):
        return arr_gp.reshape(32, 2, 64).transpose(1, 2, 0).reshape(128, 32)
    ldt_gp = np.broadcast_to(ldt[:, None], (64, 64))
    o["pl_par"] = f(np.stack([pl(a_re), pl(a_im), pl(ldt_gp)], axis=1))
    def plC(c_ghp):
        return c_ghp.reshape(32, 2, 16, 64).transpose(1, 3, 0, 2).reshape(128, 32, 16)
    o["pl_C"] = f(np.stack([plC(C_re), plC(C_im)], axis=1))
    def tl_gp(arr_gp):
        a = arr_gp.reshape(8, 8, 64)
        a = np.broadcast_to(a[:, :, None, :], (8, 8, 16, 64)).transpose(1, 2, 0, 3).reshape(128, 8 * 64)
        return a
    o["tl_par"] = f(np.stack([tl_gp(a_re), tl_gp(a_im), tl_gp(ldt_gp)], axis=1))
    def tl_B(b_gph):
        a = b_gph.reshape(8, 8, 64, 16).transpose(1, 3, 0, 2).reshape(128, 8 * 64)
        return a
    o["tl_B"] = f(np.stack([tl_B(B_re), tl_B(B_im)], axis=1))
    return o


_CACHE = {}


def kernel(**inputs):
    x = np.asarray(inputs["x"], dtype=np.float32)
    c = np.asarray(inputs["c"], dtype=np.float32)
    shared = host_layout(inputs)
    shared.update(host_consts())
    if "nc" not in _CACHE:
        _CACHE["nc"] = build_nc()[0]
    nc = _CACHE["nc"]
    in_maps = []
    for b in range(NCORES):
        m = dict(shared)
        m["x"] = np.ascontiguousarray(x[b])
        m["cT"] = np.ascontiguousarray(c[b].reshape(16, 128).T)
        in_maps.append(m)
    res = run_bass_kernel_spmd(nc, in_maps, core_ids=list(range(NCORES)))
    return np.stack([np.asarray(r["out"], dtype=np.float32) for r in res.results], axis=0)
```
